# Optimizing a Trainium2 kernel written in Bass

```python
import math
import jax, jax.numpy as jnp
from jax import lax
import numpy as np

D_MODEL = 4096
BATCH = 2
SEQ = 4096
DEPTH = 1

ROPE_THETA = 10000.0
LN_EPS = 1e-5
NEG_INF = -1e30
Q_BLOCK = 128

A_HEADS = 64
A_KV_HEADS = 8
A_HEAD_DIM = D_MODEL // A_HEADS
A_WINDOW = 128

B_HEADS = 32
B_KV_HEADS = 4
B_HEAD_DIM = D_MODEL // B_HEADS
CMP_LEN = 32
CMP_STRIDE = 16
CMP_HIDDEN = 256
SLC_LEN = 64
SLC_TOPK = 16
SLC_Q_BLOCK = 64
WIN_LEN = 512

N_EXPERTS = 64
TOP_K = 8
EXPERT_FF = 512
SHARED_FF = 512
ROUTED_SCALE = 2.5
MOE_BLOCK = 256

DN_ALPHA = (2.0 * DEPTH) ** 0.25
DN_BETA = (8.0 * DEPTH) ** -0.25

kernel_name = "hybrid_swa_sink_nsa_moe_deepnorm"


def _in_layout():
    qa, kva = A_HEADS * A_HEAD_DIM, A_KV_HEADS * A_HEAD_DIM
    qb, kvb = B_HEADS * B_HEAD_DIM, B_KV_HEADS * B_HEAD_DIM
    return [(qa, False), (kva, False), (kva, True),
            (qb, False),
            (kvb, False), (kvb, True),
            (kvb, False), (kvb, True),
            (kvb, False), (kvb, True),
            (3 * B_HEADS, False),
            (D_MODEL, False), (D_MODEL, False)]


def layer_norm(x, g, b):
    xf = x.astype(jnp.float32)
    mu = xf.mean(-1, keepdims=True)
    var = jnp.square(xf - mu).mean(-1, keepdims=True)
    y = (xf - mu) * lax.rsqrt(var + LN_EPS) * g.astype(jnp.float32) + b.astype(jnp.float32)
    return y.astype(x.dtype)


def rope(x, positions):
    dh = x.shape[-1]
    half = dh // 2
    inv = ROPE_THETA ** (-jnp.arange(half, dtype=jnp.float32) * 2.0 / dh)
    ang = positions.astype(jnp.float32)[:, :, None] * inv
    cos, sin = jnp.cos(ang)[:, :, None, :], jnp.sin(ang)[:, :, None, :]
    x1, x2 = x[..., :half].astype(jnp.float32), x[..., half:].astype(jnp.float32)
    return jnp.concatenate([x1 * cos - x2 * sin, x2 * cos + x1 * sin], -1).astype(x.dtype)


def banded_attention(q, k, v, window, sinks):
    B, S, H, dh = q.shape
    G = k.shape[2]
    rep = H // G
    blk = Q_BLOCK
    nb = S // blk
    nprev = -(-(window - 1) // blk)
    pad = ((0, 0), (nprev * blk, 0), (0, 0), (0, 0))
    kb = jnp.pad(k, pad).reshape(B, nb + nprev, blk, G, dh)
    vb = jnp.pad(v, pad).reshape(B, nb + nprev, blk, G, dh)
    kw = jnp.concatenate([kb[:, i:i + nb] for i in range(nprev + 1)], axis=2)
    vw = jnp.concatenate([vb[:, i:i + nb] for i in range(nprev + 1)], axis=2)
    qb = q.reshape(B, nb, blk, G, rep, dh)
    s = jnp.einsum('bnqgrd,bnkgd->bngrqk', qb, kw).astype(jnp.float32) * (dh ** -0.5)
    nidx = jnp.arange(nb)[:, None, None]
    qpos = nidx * blk + jnp.arange(blk)[None, :, None]
    kpos = (nidx - nprev) * blk + jnp.arange((nprev + 1) * blk)[None, None, :]
    delta = qpos - kpos
    mask = (kpos >= 0) & (delta >= 0) & (delta < window)
    s = jnp.where(mask[None, :, None, None], s, NEG_INF)
    if sinks is None:
        p = jax.nn.softmax(s, axis=-1)
    else:
        sk = sinks.astype(jnp.float32).reshape(G, rep)[None, None, :, :, None, None]
        m = jnp.maximum(s.max(-1, keepdims=True), sk)
        e = jnp.exp(s - m)
        p = e / (e.sum(-1, keepdims=True) + jnp.exp(sk - m))
    o = jnp.einsum('bngrqk,bnkgd->bnqgrd', p.astype(v.dtype), vw)
    return o.reshape(B, S, H, dh)


def compress(t, pos_emb, w1, w2):
    B, S, G, dh = t.shape
    nshift = CMP_LEN // CMP_STRIDE
    c = t.reshape(B, S // CMP_STRIDE, CMP_STRIDE, G, dh)
    nc = S // CMP_STRIDE - nshift + 1
    blocks = jnp.concatenate([c[:, i:i + nc] for i in range(nshift)], axis=2)
    blocks = blocks + pos_emb[None, None, :, None, :]
    flat = blocks.transpose(0, 1, 3, 2, 4).reshape(B, nc, G, CMP_LEN * dh)
    return jax.nn.gelu(flat @ w1) @ w2


def nsa_compressed(q, kc, vc, kpe, kw1, kw2, vpe, vw1, vw2):
    B, S, H, dh = q.shape
    G = kc.shape[2]
    rep = H // G
    kcmp = compress(kc, kpe, kw1, kw2)
    vcmp = compress(vc, vpe, vw1, vw2)
    nc = kcmp.shape[1]
    qg = q.reshape(B, S, G, rep, dh)
    s = jnp.einsum('bsgrd,bcgd->bgrsc', qg, kcmp).astype(jnp.float32) * (dh ** -0.5)
    c_end = jnp.arange(nc) * CMP_STRIDE + CMP_LEN - 1
    valid = c_end[None, :] <= jnp.arange(S)[:, None]
    s = jnp.where(valid, s, NEG_INF)
    m = s.max(-1, keepdims=True)
    e = jnp.where(valid, jnp.exp(s - m), 0.0)
    den = e.sum(-1, keepdims=True)
    p = e / jnp.where(den > 0, den, 1.0)
    o = jnp.einsum('bgrsc,bcgd->bsgrd', p.astype(vcmp.dtype), vcmp).reshape(B, S, H, dh)
    return o, p.sum(axis=2)


def nsa_selected(q, ks, vs, p_grp):
    B, S, H, dh = q.shape
    G = ks.shape[2]
    rep = H // G
    ns = S // SLC_LEN
    r1, r2 = SLC_LEN // CMP_STRIDE, CMP_LEN // CMP_STRIDE
    pp = jnp.pad(p_grp, ((0, 0), (0, 0), (0, 0), (r2 - 1, r1)))
    p_slc = jnp.zeros(p_grp.shape[:3] + (ns,), jnp.float32)
    for mm in range(r1):
        for nn in range(r2):
            p_slc = p_slc + pp[..., mm - nn + r2 - 1::r1][..., :ns]
    t = jnp.arange(S)[:, None]
    j = jnp.arange(ns)[None, :]
    cur = t // SLC_LEN
    valid = j * SLC_LEN <= t
    forced = (j == 0) | (j == cur) | (j == cur - 1)
    score = jnp.where(forced, 1e9, jnp.where(valid, p_slc, NEG_INF))
    n_sel = min(SLC_TOPK, ns)
    _, idx = lax.top_k(score, n_sel)
    kblk = ks.reshape(B, ns, SLC_LEN, G, dh).transpose(0, 3, 1, 2, 4)
    vblk = vs.reshape(B, ns, SLC_LEN, G, dh).transpose(0, 3, 1, 2, 4)
    tq = SLC_Q_BLOCK
    nq = S // tq
    q_ch = q.reshape(B, nq, tq, G, rep, dh).transpose(1, 0, 2, 3, 4, 5)
    i_ch = idx.reshape(B, G, nq, tq, n_sel).transpose(2, 0, 1, 3, 4)
    bi = jnp.arange(B)[:, None, None, None]
    gi = jnp.arange(G)[None, :, None, None]
    scale = dh ** -0.5

    def chunk(args):
        n, qc, ic = args
        kg = kblk[bi, gi, ic]
        vg = vblk[bi, gi, ic]
        s = jnp.einsum('bqgrd,bgqnkd->bgrqnk', qc, kg).astype(jnp.float32) * scale
        qpos = n * tq + jnp.arange(tq)
        kpos = ic[..., None] * SLC_LEN + jnp.arange(SLC_LEN)
        mask = kpos <= qpos[None, None, :, None, None]
        s = jnp.where(mask[:, :, None], s, NEG_INF)
        p = jax.nn.softmax(s.reshape(s.shape[:4] + (-1,)), axis=-1).reshape(s.shape)
        return jnp.einsum('bgrqnk,bgqnkd->bqgrd', p.astype(vg.dtype), vg)

    o = lax.map(chunk, (jnp.arange(nq), q_ch, i_ch))
    return o.transpose(1, 0, 2, 3, 4, 5).reshape(B, S, H, dh)


def routed_experts(xf, w_gate, w_up, w_down, idx, gates):
    N, D = xf.shape
    E = w_gate.shape[0]
    A = N * TOP_K
    flat_e = idx.reshape(-1)
    flat_tok = jnp.arange(A, dtype=jnp.int32) // TOP_K
    flat_w = gates.reshape(-1)
    order = jnp.argsort(flat_e)
    e_s, tok_s, w_s = flat_e[order], flat_tok[order], flat_w[order]
    counts = jnp.bincount(flat_e, length=E)
    starts = jnp.cumsum(counts) - counts
    padded = (counts + MOE_BLOCK - 1) // MOE_BLOCK * MOE_BLOCK
    pends = jnp.cumsum(padded)
    pstarts = pends - padded
    dest = pstarts[e_s] + (jnp.arange(A, dtype=jnp.int32) - starts[e_s])
    n_blocks = -(-A // MOE_BLOCK) + E
    P = n_blocks * MOE_BLOCK
    row_tok = jnp.full((P,), N, jnp.int32).at[dest].set(tok_s)
    row_w = jnp.zeros((P,), xf.dtype).at[dest].set(w_s)
    block_e = jnp.minimum(jnp.searchsorted(pends, jnp.arange(n_blocks, dtype=jnp.int32) * MOE_BLOCK,
                                           side='right'), E - 1)
    x_pad = jnp.concatenate([xf, jnp.zeros((1, D), xf.dtype)], axis=0)

    def body(acc, blk):
        rows, wts, e = blk
        xb = x_pad[rows]
        h = jax.nn.silu(xb @ w_gate[e]) * (xb @ w_up[e])
        yb = (h @ w_down[e]) * wts[:, None]
        return acc.at[rows].add(yb), None

    acc, _ = lax.scan(body, jnp.zeros((N + 1, D), xf.dtype),
                      (row_tok.reshape(n_blocks, MOE_BLOCK), row_w.reshape(n_blocks, MOE_BLOCK), block_e))
    return acc[:N]


def hybrid_layer(x, positions, w_in, a_sinks, cmp_k_pos, cmp_k_w1, cmp_k_w2, cmp_v_pos, cmp_v_w1,
                 cmp_v_w2, w_o, ln1_g, ln1_b, w_router, router_bias, exp_w_gate, exp_w_up, exp_w_down,
                 sh_w_gate, sh_w_up, sh_w_down, ln2_g, ln2_b):
    B, S, D = x.shape
    h = x @ w_in
    cuts = np.cumsum([w for w, _ in _in_layout()])[:-1].tolist()
    qa, ka, va, qb, kc, vc, ks, vs, kwn, vwn, g_nsa, g_a, g_b = jnp.split(h, cuts, axis=-1)

    qa = rope(qa.reshape(B, S, A_HEADS, A_HEAD_DIM), positions)
    ka = rope(ka.reshape(B, S, A_KV_HEADS, A_HEAD_DIM), positions)
    va = va.reshape(B, S, A_KV_HEADS, A_HEAD_DIM)
    o_a = banded_attention(qa, ka, va, A_WINDOW, a_sinks).reshape(B, S, A_HEADS * A_HEAD_DIM)

    kvshape = (B, S, B_KV_HEADS, B_HEAD_DIM)
    qb = rope(qb.reshape(B, S, B_HEADS, B_HEAD_DIM), positions)
    kc, ks, kwn = (rope(t.reshape(kvshape), positions) for t in (kc, ks, kwn))
    vc, vs, vwn = (t.reshape(kvshape) for t in (vc, vs, vwn))
    o_cmp, p_grp = nsa_compressed(qb, kc, vc, cmp_k_pos, cmp_k_w1, cmp_k_w2, cmp_v_pos, cmp_v_w1, cmp_v_w2)
    o_slc = nsa_selected(qb, ks, vs, p_grp)
    o_win = banded_attention(qb, kwn, vwn, WIN_LEN, None)
    gb = jax.nn.sigmoid(g_nsa.astype(jnp.float32)).astype(x.dtype).reshape(B, S, 3, B_HEADS, 1)
    o_b = (gb[:, :, 0] * o_cmp + gb[:, :, 1] * o_slc + gb[:, :, 2] * o_win).reshape(B, S, B_HEADS * B_HEAD_DIM)

    y = jax.nn.sigmoid(g_a) * o_a + jax.nn.sigmoid(g_b) * o_b
    x1 = layer_norm(DN_ALPHA * x + y @ w_o, ln1_g, ln1_b)

    xf = x1.reshape(B * S, D)
    scores = jax.nn.sigmoid((xf @ w_router).astype(jnp.float32))
    _, idx = lax.top_k(scores + router_bias.astype(jnp.float32), TOP_K)
    sv = jnp.take_along_axis(scores, idx, axis=-1)
    gates = (sv / sv.sum(-1, keepdims=True) * ROUTED_SCALE).astype(x.dtype)
    routed = routed_experts(xf, exp_w_gate, exp_w_up, exp_w_down, idx, gates)
    shared = (jax.nn.silu(xf @ sh_w_gate) * (xf @ sh_w_up)) @ sh_w_down
    return layer_norm(DN_ALPHA * x1 + (routed + shared).reshape(B, S, D), ln2_g, ln2_b)


def setup_inputs(seed: int = 0) -> dict:
    key = jax.random.key(seed)
    ks = jax.random.split(key, 24)

    def nrm(k, shape, scale):
        return jax.random.normal(k, shape, jnp.float32) * scale

    L, D = DEPTH, D_MODEL
    layout = _in_layout()
    col_scale = jnp.concatenate([jnp.full((w,), DN_BETA if isv else 1.0, jnp.float32) for w, isv in layout])
    total = sum(w for w, _ in layout)
    cin = CMP_LEN * B_HEAD_DIM
    return {
        "x": nrm(ks[0], (BATCH, SEQ, D), 1.0),
        "positions": jnp.broadcast_to(jnp.arange(SEQ, dtype=jnp.int32)[None, :], (BATCH, SEQ)),
        "w_in": nrm(ks[1], (L, D, total), D ** -0.5) * col_scale,
        "a_sinks": nrm(ks[2], (L, A_HEADS), 0.5),
        "cmp_k_pos": nrm(ks[3], (L, CMP_LEN, B_HEAD_DIM), 0.1),
        "cmp_k_w1": nrm(ks[4], (L, cin, CMP_HIDDEN), cin ** -0.5),
        "cmp_k_w2": nrm(ks[5], (L, CMP_HIDDEN, B_HEAD_DIM), (CMP_HIDDEN / 2.0) ** -0.5),
        "cmp_v_pos": nrm(ks[6], (L, CMP_LEN, B_HEAD_DIM), 0.1),
        "cmp_v_w1": nrm(ks[7], (L, cin, CMP_HIDDEN), cin ** -0.5),
        "cmp_v_w2": nrm(ks[8], (L, CMP_HIDDEN, B_HEAD_DIM), (CMP_HIDDEN / 2.0) ** -0.5 * DN_BETA),
        "w_o": nrm(ks[9], (L, D, D), D ** -0.5 * DN_BETA),
        "ln1_g": 1.0 + nrm(ks[10], (L, D), 0.02),
        "ln1_b": nrm(ks[11], (L, D), 0.02),
        "w_router": nrm(ks[12], (L, D, N_EXPERTS), D ** -0.5),
        "router_bias": nrm(ks[13], (L, N_EXPERTS), 0.01),
        "exp_w_gate": nrm(ks[14], (L, N_EXPERTS, D, EXPERT_FF), D ** -0.5),
        "exp_w_up": nrm(ks[15], (L, N_EXPERTS, D, EXPERT_FF), D ** -0.5),
        "exp_w_down": nrm(ks[16], (L, N_EXPERTS, EXPERT_FF, D), EXPERT_FF ** -0.5 * DN_BETA),
        "sh_w_gate": nrm(ks[17], (L, D, SHARED_FF), D ** -0.5),
        "sh_w_up": nrm(ks[18], (L, D, SHARED_FF), D ** -0.5),
        "sh_w_down": nrm(ks[19], (L, SHARED_FF, D), SHARED_FF ** -0.5 * DN_BETA),
        "ln2_g": 1.0 + nrm(ks[20], (L, D), 0.02),
        "ln2_b": nrm(ks[21], (L, D), 0.02),
    }


def reference(x, positions, w_in, a_sinks, cmp_k_pos, cmp_k_w1, cmp_k_w2, cmp_v_pos, cmp_v_w1, cmp_v_w2,
              w_o, ln1_g, ln1_b, w_router, router_bias, exp_w_gate, exp_w_up, exp_w_down,
              sh_w_gate, sh_w_up, sh_w_down, ln2_g, ln2_b):
    for l in range(DEPTH):
        x = hybrid_layer(x, positions, w_in[l], a_sinks[l], cmp_k_pos[l], cmp_k_w1[l], cmp_k_w2[l],
                         cmp_v_pos[l], cmp_v_w1[l], cmp_v_w2[l], w_o[l], ln1_g[l], ln1_b[l],
                         w_router[l], router_bias[l], exp_w_gate[l], exp_w_up[l], exp_w_down[l],
                         sh_w_gate[l], sh_w_up[l], sh_w_down[l], ln2_g[l], ln2_b[l])
    return x
```

```python
import math
import contextlib
import numpy as np
import concourse.bass as bass
import concourse.mybir as mybir
from concourse.bass_utils import run_bass_kernel_spmd

F32 = mybir.dt.float32
BF16 = mybir.dt.bfloat16
I32 = mybir.dt.int32
AF = mybir.ActivationFunctionType
ALU = mybir.AluOpType
AX = mybir.AxisListType

COMPUTE = ('pe', 'act', 'dve', 'pool')
NCORES = 2
S_ = 4096
D_ = 4096
NT = S_ // 128
ALPHA = 2.0 ** 0.25
EPS = 1e-5
TWO_PI = 2.0 * math.pi
C1 = 6.28125
C2 = TWO_PI - C1


class Sched:
    def __init__(self, nc, n_dma_sems=8):
        self.nc = nc
        self.streams = {e: [] for e in ('pe', 'act', 'dve', 'pool', 'sp')}
        self._sem_ctx = []
        self.sems = {e: self._mksem('c_' + e) for e in COMPUTE}
        self.seq = {e: 0 for e in COMPUTE}
        self.dma_sems, self.dma_val, self.dma_rr = {}, {}, {}
        for q in ('sp', 'act', 'pool'):
            self.dma_sems[q] = [self._mksem('d_%s%d' % (q, i)) for i in range(n_dma_sems)]
            self.dma_rr[q] = 0
            for s in self.dma_sems[q]:
                self.dma_val[id(s)] = 0
        self.observed = {e: {} for e in self.streams}
        self.last_w = {}
        self.readers = {}
        self.n_ins = 0

    def _mksem(self, name):
        ctx = self.nc.semaphore(name)
        h = ctx.__enter__()
        self._sem_ctx.append(ctx)
        return h

    def close(self):
        for c in reversed(self._sem_ctx):
            c.__exit__(None, None, None)

    def _need(self, eng, dep, waits):
        if dep is None:
            return
        sem, val, deng = dep
        if deng == eng and eng == 'pe':
            return
        ob = self.observed[eng]
        if ob.get(id(sem), 0) >= val:
            return
        ob[id(sem)] = val
        for i, (s, v) in enumerate(waits):
            if s is sem:
                waits[i] = (s, max(v, val))
                return
        waits.append((sem, val))

    def op(self, eng, fn, reads=(), writes=(), dma=False):
        waits = []
        for k in reads:
            self._need(eng, self.last_w.get(k), waits)
        for k in writes:
            self._need(eng, self.last_w.get(k), waits)
            for r in list(self.readers.get(k, {}).values()):
                self._need(eng, r, waits)
        if dma:
            lst = self.dma_sems[eng]
            s = lst[self.dma_rr[eng] % len(lst)]
            self.dma_rr[eng] += 1
            old = self.dma_val[id(s)]
            if old > 0:
                self._need(eng, (s, old, 'dma'), waits)
            val = old + 16
            self.dma_val[id(s)] = val
            comp = (s, val, 'dma')
            inc = (s, 16)
        else:
            self.seq[eng] += 1
            comp = (self.sems[eng], self.seq[eng], eng)
            inc = (self.sems[eng], 1)
        for k in reads:
            self.readers.setdefault(k, {})[id(comp[0])] = comp
        for k in writes:
            self.last_w[k] = comp
            self.readers[k] = {}
        self.streams[eng].append((waits, fn, inc))
        self.n_ins += 1
        return comp

    def wait_all(self, eng, comps):
        waits = []
        for c in comps:
            self._need(eng, c, waits)
        self.streams[eng].append((waits, None, None))

    def barrier(self):
        comps = [(self.sems[e], self.seq[e], e) for e in COMPUTE if self.seq[e] > 0]
        for q in self.dma_sems:
            for sm in self.dma_sems[q]:
                if self.dma_val[id(sm)] > 0:
                    comps.append((sm, self.dma_val[id(sm)], 'dma'))
        for e in self.streams:
            waits = []
            for c in comps:
                sem, val, deng = c
                ob = self.observed[e]
                if ob.get(id(sem), 0) >= val:
                    continue
                ob[id(sem)] = val
                waits.append((sem, val))
            self.streams[e].append((waits, None, None))

    def flush(self):
        self.barrier()
        self.emit()
        self.streams = {e: [] for e in self.streams}

    def emit(self):
        engobj = {'pe': 'tensor', 'act': 'scalar', 'dve': 'vector', 'pool': 'gpsimd', 'sp': 'sync'}
        with self.nc.Block() as block:
            for e, attr in engobj.items():
                stream = self.streams[e]

                def body(engine, stream=stream):
                    for waits, fn, inc in stream:
                        for (s, v) in waits:
                            engine.wait_ge(s, v)
                        if fn is not None:
                            fn(engine).then_inc(inc[0], inc[1])
                getattr(block, attr)(body)


class Ring:
    def __init__(self, es, nc, name, shape, dt, n, psum=False):
        mk = nc.psum_tensor if psum else nc.sbuf_tensor
        self.bufs = [es.enter_context(mk("%s%d" % (name, i), shape, dt)) for i in range(n)]
        self.name = name
        self.i = 0

    def get(self):
        j = self.i % len(self.bufs)
        self.i += 1
        return self.bufs[j], (self.name, j)


QA0, KA0, VA0, QB0, KC0, VC0, KS0, VS0, KW0, VW0, GN0, GA0, GB0 = (
    0, 4096, 4608, 5120, 9216, 9728, 10240, 10752, 11264, 11776, 12288, 12384, 16480)
WTOT = 20576


DECLARED = []
DBG_NAMES = ("X1", "Gt", "out")
USED_INPUTS = ("x", "pos", "w_in", "inva", "invb", "ident", "sinks_b", "m_swa", "m_win", "kpeT", "vpeT", "kw1", "vw1", "kw2", "vw2", "m_cmpT", "m_cmp", "sl_vb", "sl_add", "exmat",
               "w_o", "ln1g", "ln1b", "ln2g", "ln2b", "w_r", "rbias", "eg", "eu", "ed")


def build_nc(stages=99, dbg=False):
    del DECLARED[:]
    nc = bass.Bass("TRN2", target_bir_lowering=False)
    S = Sched(nc)

    def din(name, shape, dt=F32):
        return nc.dram_tensor(name, list(shape), dt, kind="ExternalInput").ap()

    DBG_OUT = DBG_NAMES

    def dscr(name, shape, dt=BF16):
        if dbg and name in DBG_OUT:
            return nc.dram_tensor(name, list(shape), dt, kind="ExternalOutput").ap()
        return nc.dram_tensor(name, list(shape), dt).ap()

    _din = din

    def din(name, shape, dt=F32):
        if name not in USED_INPUTS or (name in ("eg", "eu", "ed") and stages < 8):
            return None
        DECLARED.append(name)
        return _din(name, shape, dt)

    x = din("x", [S_, D_])
    pos = din("pos", [128, NT], I32)
    w_in = din("w_in", [D_, WTOT])
    esink_in = din("sinks_b", [128, 64])
    inva = din("inva", [128, 32])
    invb = din("invb", [128, 64])
    ident_in = din("ident", [128, 128])
    kpeT = din("kpeT", [128, 32])
    vpeT = din("vpeT", [128, 32])
    kw1 = din("kw1", [4096, 256])
    vw1 = din("vw1", [4096, 256])
    kw2 = din("kw2", [256, 128])
    vw2 = din("vw2", [256, 128])
    w_o = din("w_o", [D_, D_])
    ln1g = din("ln1g", [128, D_])
    ln1b = din("ln1b", [128, D_])
    ln2g = din("ln2g", [128, D_])
    ln2b = din("ln2b", [128, D_])
    w_r = din("w_r", [D_, 64])
    rbias = din("rbias", [128, 64])
    eg = din("eg", [65, 2, 128, 32 * 256])
    eu = din("eu", [65, 2, 128, 32 * 256])
    ed = din("ed", [65, 2, 128, 2 * D_])
    m_swa = din("m_swa", [5, 128, 512])
    m_win = din("m_win", [8, 128, 512])
    m_cmpT = din("m_cmpT", [2, 128, S_])
    m_cmp = din("m_cmp", [S_, 256])
    sl_vb = din("sl_vb", [S_, 64])
    sl_add = din("sl_add", [S_, 64])
    exmat = din("exmat", [64, 32 * 128])
    out = nc.dram_tensor("out", [S_, D_], F32, kind="ExternalOutput").ap()

    xT_d = dscr("xT_d", [NT, 128, 32 * 128])
    QaT = dscr("QaT", [32, 128, S_])
    KaT2 = dscr("KaT2", [8, 128, S_])
    Va = dscr("Va", [S_, 8 * 65])
    QbT = dscr("QbT", [32, 128, S_])
    kcT = dscr("kcT", [4, 128, S_])
    vcT = dscr("vcT", [4, 128, S_])
    ksT = dscr("ksT", [4, 128, S_])
    kwT = dscr("kwT", [4, 128, S_])
    Vs = dscr("Vs", [S_, 4 * 129])
    Vw = dscr("Vw", [S_, 4 * 129])
    Gn = dscr("Gn", [S_, 96], F32)
    Ga = dscr("Ga", [S_, D_], F32)
    Gb = dscr("Gb", [S_, D_], F32)
    Oa = dscr("Oa", [S_, D_], F32)
    Ocmp = dscr("Ocmp", [S_, D_], F32)
    Oslc = dscr("Oslc", [S_, D_], F32)
    Owin = dscr("Owin", [S_, D_], F32)
    yT_d = dscr("yT_d", [NT, 128, 32 * 128])
    R1 = dscr("R1", [S_, D_], F32)
    X1 = dscr("X1", [S_, D_], F32)
    x1T_d = dscr("x1T_d", [NT, 128, 32 * 128])
    Gt = dscr("Gt", [S_, 65], F32)
    class _Split:
        def __init__(self, name):
            self.t = [dscr(name + "0", [33, 2, 128, 8192]), dscr(name + "1", [32, 2, 128, 8192])]

        def __getitem__(self, idx):
            ex_, fh_ = idx
            return self.t[ex_ // 33][ex_ % 33, fh_]
    eg_b = _Split("eg_b")
    eu_b = _Split("eu_b")
    ed_b = _Split("ed_b")
    kcmpT_d = dscr("kcmpT_d", [4, 128, S_])
    vcmp_d = dscr("vcmp_d", [S_, 4 * 129])
    selT_d = dscr("selT_d", [4, 64, S_])

    es = contextlib.ExitStack()
    with es:
        es.enter_context(nc.allow_low_precision("bf16 matmul operands, fp32 accumulation"))

        def sb(name, shape, dt):
            return es.enter_context(nc.sbuf_tensor(name, shape, dt))

        def op(eng, fn, r=(), w=(), dma=False):
            return S.op(eng, fn, reads=r, writes=w, dma=dma)

        idf = sb("idf", [128, 128], F32)
        idb = sb("idb", [128, 128], BF16)
        op('sp', lambda e: e.dma_start(out=idf[:], in_=ident_in), w=['idf'], dma=True)
        op('dve', lambda e: e.tensor_copy(out=idb[:], in_=idf[:]), r=['idf'], w=['idb'])

        ps_acc = Ring(es, nc, "psacc", [128, 512], F32, 2, psum=True)
        ps_t = Ring(es, nc, "pst", [128, 8, 128], BF16, 2, psum=True)
        ps_o = Ring(es, nc, "pso", [128, 512], F32, 4, psum=True)

        def transpose_blocks(src_fn, nblk, dst_fn, rkeys, wkey_fn):
            for j0 in range(0, nblk, 8):
                n = min(8, nblk - j0)
                pt, pk = ps_t.get()
                for j in range(n):
                    op('pe', lambda e, j=j, pt=pt, j0=j0: e.transpose(out=pt[:, j, :], in_=src_fn(j0 + j), identity=idb[:]),
                       r=list(rkeys) + ['idb'], w=[pk])
                op('act', lambda e, pt=pt, n=n, j0=j0: e.copy(out=dst_fn(j0, n), in_=pt[:, 0:n, :]), r=[pk], w=[wkey_fn(j0)])

        pes = contextlib.ExitStack()
        pes.__enter__()
        xf_r = Ring(pes, nc, "xf", [128, D_], F32, 2)
        xb_r = Ring(pes, nc, "xb", [128, D_], BF16, 2)
        xt_r = Ring(pes, nc, "xTt", [128, 32, 128], BF16, 2)
        for tt in range(NT):
            xf, kf = xf_r.get()
            xb, kb = xb_r.get()
            xt, kt = xt_r.get()
            op('sp', lambda e, xf=xf, tt=tt: e.dma_start(out=xf[:], in_=x[tt * 128:(tt + 1) * 128, :]), w=[kf], dma=True)
            op('dve', lambda e, xf=xf, xb=xb: e.tensor_copy(out=xb[:], in_=xf[:]), r=[kf], w=[kb])
            transpose_blocks(lambda j, xb=xb: xb[:, j * 128:(j + 1) * 128], 32,
                             lambda j0, n, xt=xt: xt[:, j0:j0 + n, :], [kb], lambda j0, kt=kt: kt)
            op('act', lambda e, xt=xt, tt=tt: e.dma_start(out=xT_d[tt].rearrange("p (k t) -> p k t", k=32), in_=xt[:]),
               r=[kt], w=[('xT_d', tt)], dma=True)

        S.flush()
        pes.__exit__(None, None, None)
        tes = contextlib.ExitStack()
        tes.__enter__()
        sbt = lambda n, shp, dt: tes.enter_context(nc.sbuf_tensor(n, shp, dt))
        cosA = sbt("cosA", [128, NT, 32], F32)
        sinA = sbt("sinA", [128, NT, 32], F32)
        cosB = sbt("cosB", [128, NT, 64], F32)
        sinB = sbt("sinB", [128, NT, 64], F32)
        inv_sb = sbt("inv_sb", [128, 96], F32)
        posi = sbt("posi", [128, NT], I32)
        posf = sbt("posf", [128, NT], F32)
        op('sp', lambda e: e.dma_start(out=inv_sb[:, 0:32], in_=inva), w=['inva'], dma=True)
        op('sp', lambda e: e.dma_start(out=inv_sb[:, 32:96], in_=invb), w=['invb'], dma=True)
        op('sp', lambda e: e.dma_start(out=posi[:], in_=pos), w=['posi'], dma=True)
        op('dve', lambda e: e.tensor_copy(out=posf[:], in_=posi[:]), r=['posi'], w=['posf'])
        ang = sbt("ang", [128, 96], F32)
        kq_i = sbt("kq_i", [128, 96], I32)
        kq = sbt("kq", [128, 96], F32)
        rr = sbt("rr", [128, 96], F32)
        mm = sbt("mm", [128, 96], F32)
        r2 = sbt("r2", [128, 96], F32)

        def wrap(v):
            op('dve', lambda e: e.tensor_single_scalar(out=mm[:], in_=v[:], scalar=math.pi, op=ALU.is_gt), r=['rp'], w=['mm'])
            op('dve', lambda e: e.scalar_tensor_tensor(out=v[:], in0=mm[:], scalar=-TWO_PI, in1=v[:], op0=ALU.mult, op1=ALU.add), r=['mm', 'rp'], w=['rp'])
            op('dve', lambda e: e.tensor_single_scalar(out=mm[:], in_=v[:], scalar=-math.pi, op=ALU.is_lt), r=['rp'], w=['mm'])
            op('dve', lambda e: e.scalar_tensor_tensor(out=v[:], in0=mm[:], scalar=TWO_PI, in1=v[:], op0=ALU.mult, op1=ALU.add), r=['mm', 'rp'], w=['rp'])

        for tt in range(NT):
            op('dve', lambda e, tt=tt: e.tensor_scalar(out=ang[:], in0=inv_sb[:], scalar1=posf[:, tt:tt + 1], scalar2=None, op0=ALU.mult),
               r=['inva', 'invb', 'posf'], w=['rp'])
            op('dve', lambda e: e.tensor_single_scalar(out=kq[:], in_=ang[:], scalar=1.0 / TWO_PI, op=ALU.mult), r=['rp'], w=['kq'])
            op('dve', lambda e: e.tensor_copy(out=kq_i[:], in_=kq[:]), r=['kq'], w=['kqi'])
            op('dve', lambda e: e.tensor_copy(out=kq[:], in_=kq_i[:]), r=['kqi'], w=['kq'])
            op('dve', lambda e: e.scalar_tensor_tensor(out=rr[:], in0=kq[:], scalar=-C1, in1=ang[:], op0=ALU.mult, op1=ALU.add), r=['kq', 'rp'], w=['rp'])
            op('dve', lambda e: e.scalar_tensor_tensor(out=rr[:], in0=kq[:], scalar=-C2, in1=rr[:], op0=ALU.mult, op1=ALU.add), r=['kq', 'rp'], w=['rp'])
            wrap(rr)
            op('act', lambda e, tt=tt: e.activation(out=sinA[:, tt, :], in_=rr[:, 0:32], func=AF.Sin), r=['rp'], w=['sinA'])
            op('act', lambda e, tt=tt: e.activation(out=sinB[:, tt, :], in_=rr[:, 32:96], func=AF.Sin), r=['rp'], w=['sinB'])
            op('dve', lambda e: e.tensor_single_scalar(out=r2[:], in_=rr[:], scalar=math.pi / 2, op=ALU.add), r=['rp'], w=['rp'])
            wrap(r2)
            op('act', lambda e, tt=tt: e.activation(out=cosA[:, tt, :], in_=r2[:, 0:32], func=AF.Sin), r=['rp'], w=['cosA'])
            op('act', lambda e, tt=tt: e.activation(out=cosB[:, tt, :], in_=r2[:, 32:96], func=AF.Sin), r=['rp'], w=['cosB'])

        pes = contextlib.ExitStack()
        pes.__enter__()
        wb_r = Ring(pes, nc, "wb", [128, 32, 512], BF16, 2)
        xl_r = Ring(pes, nc, "xl", [128, 32, 128], BF16, 3)
        ep_f = Ring(pes, nc, "epf", [128, 512], F32, 2)
        ep_b = Ring(pes, nc, "epb", [128, 1024], BF16, 3)
        tr_b = Ring(pes, nc, "trb", [128, 8, 128], BF16, 2)
        t1 = pes.enter_context(nc.sbuf_tensor("t1", [128, 256], F32))
        t2 = pes.enter_context(nc.sbuf_tensor("t2", [128, 256], F32))
        ones_col = pes.enter_context(nc.sbuf_tensor("ones_col", [128, 8], BF16))
        op('dve', lambda e: e.memset(ones_col[:], 1.0), w=['ones_col'])
        w_in_v = w_in.rearrange("(k p) c -> p k c", p=128)

        chunks = []
        for c in range(8):
            chunks.append((QA0 + c * 512, 512, 'ropeA_q', c))
        chunks.append((KA0, 512, 'ropeA_k', 0))
        chunks.append((VA0, 512, 'v_a', 0))
        for c in range(8):
            chunks.append((QB0 + c * 512, 512, 'ropeB_q', c))
        chunks.append((KC0, 512, 'ropeB_k', kcT))
        chunks.append((VC0, 512, 'plainT', vcT))
        chunks.append((KS0, 512, 'ropeB_k', ksT))
        chunks.append((VS0, 512, 'v_b', Vs))
        chunks.append((KW0, 512, 'ropeB_k', kwT))
        chunks.append((VW0, 512, 'v_b', Vw))
        chunks.append((GN0, 96, 'sig', Gn))
        for c in range(8):
            chunks.append((GA0 + c * 512, 512, 'sig', (Ga, c)))
        for c in range(8):
            chunks.append((GB0 + c * 512, 512, 'sig', (Gb, c)))

        def rope(ps, pk, H, hd, cos_t, sin_t, tt, ob, obk):
            v = ps[:, :].rearrange("p (h t d) -> p h t d", h=H, t=2)
            o = ob[:, 0:512].rearrange("p (h t d) -> p h t d", h=H, t=2)
            cb = cos_t[:, tt, :].unsqueeze(1).broadcast_to([128, H, hd])
            sn = sin_t[:, tt, :].unsqueeze(1).broadcast_to([128, H, hd])
            a = t1[:, :].rearrange("p (h d) -> p h d", h=H)
            b = t2[:, :].rearrange("p (h d) -> p h d", h=H)
            tabs = ['cosA', 'sinA', 'cosB', 'sinB']
            op('dve', lambda e: e.tensor_tensor(out=a, in0=v[:, :, 0, :], in1=cb, op=ALU.mult), r=[pk] + tabs, w=['t1'])
            op('dve', lambda e: e.tensor_tensor(out=b, in0=v[:, :, 1, :], in1=sn, op=ALU.mult), r=[pk] + tabs, w=['t2'])
            op('dve', lambda e: e.tensor_tensor(out=o[:, :, 0, :], in0=a, in1=b, op=ALU.subtract), r=['t1', 't2'], w=[obk])
            op('dve', lambda e: e.tensor_tensor(out=a, in0=v[:, :, 1, :], in1=cb, op=ALU.mult), r=[pk] + tabs, w=['t1'])
            op('dve', lambda e: e.tensor_tensor(out=b, in0=v[:, :, 0, :], in1=sn, op=ALU.mult), r=[pk] + tabs, w=['t2'])
            op('dve', lambda e: e.tensor_tensor(out=o[:, :, 1, :], in0=a, in1=b, op=ALU.add), r=['t1', 't2'], w=[obk])

        def to_featmajor(ob, obk, nblk, dst, tt, idx0):
            tb, tk = tr_b.get()
            transpose_blocks(lambda j: ob[:, j * 128:(j + 1) * 128], nblk, lambda j0, n: tb[:, j0:j0 + n, :], [obk], lambda j0: tk)
            op('act', lambda e: e.dma_start(out=dst[idx0:idx0 + nblk, :, tt * 128:(tt + 1) * 128].rearrange("j p t -> p j t"), in_=tb[:, 0:nblk, :]),
               r=[tk], w=[('featT', id(dst), tt)], dma=True)

        cast_items = []
        if stages >= 8:
            for ex_ in range(65):
                for fh_ in range(2):
                    cast_items += [(eg, eg_b, 'g', ex_, fh_), (eu, eu_b, 'u', ex_, fh_), (ed, ed_b, 'd', ex_, fh_)]
        cast_pos = [0]

        def issue_casts(n):
            for _ in range(n):
                if cast_pos[0] >= len(cast_items):
                    return
                src_, dst_, wh_, ex_, fh_ = cast_items[cast_pos[0]]
                cast_pos[0] += 1
                op('pool', lambda e, src_=src_, dst_=dst_, ex_=ex_, fh_=fh_: e.dma_start(out=dst_[ex_, fh_], in_=src_[ex_, fh_], max_dma_last_dim=4096),
                   w=[('ecast', wh_, ex_, fh_)], dma=True)

        for (c0, ncol, kind, arg) in (chunks if stages >= 1 else []):
            wb, wk = wb_r.get()
            for k0 in range(0, 32, 8):
                op('pool', lambda e, wb=wb, k0=k0, c0=c0, ncol=ncol: e.dma_start(out=wb[:, k0:k0 + 8, 0:ncol], in_=w_in_v[:, k0:k0 + 8, c0:c0 + ncol]),
                   w=[(wk, k0)], dma=True)
            issue_casts(10)
            for tt in range(NT):
                xl, xk = xl_r.get()
                op('sp', lambda e, xl=xl, tt=tt: e.dma_start(out=xl[:], in_=xT_d[tt].rearrange("p (k t) -> p k t", k=32)),
                   r=[('xT_d', tt)], w=[xk], dma=True)
                ps, pk = ps_acc.get()
                for k in range(32):
                    op('pe', lambda e, ps=ps, xl=xl, wb=wb, k=k, ncol=ncol: e.matmul(ps[:, 0:ncol], lhsT=xl[:, k, :], rhs=wb[:, k, 0:ncol], start=(k == 0), stop=(k == 31)),
                       r=[xk, (wk, (k // 8) * 8)], w=[pk])
                tsl = slice(tt * 128, (tt + 1) * 128)
                if kind == 'sig':
                    of, ofk = ep_f.get()
                    op('act', lambda e, of=of, ps=ps, ncol=ncol: e.activation(out=of[:, 0:ncol], in_=ps[:, 0:ncol], func=AF.Sigmoid), r=[pk], w=[ofk])
                    if ncol == 96:
                        op('act', lambda e, of=of, tsl=tsl: e.dma_start(out=Gn[tsl, :], in_=of[:, 0:96]), r=[ofk], w=[('Gn', tt)], dma=True)
                    else:
                        dst, cc = arg
                        op('act', lambda e, of=of, tsl=tsl, dst=dst, cc=cc: e.dma_start(out=dst[tsl, cc * 512:(cc + 1) * 512], in_=of[:]), r=[ofk], w=[('G', id(dst), tt, cc)], dma=True)
                elif kind == 'v_a':
                    ob, obk = ep_b.get()
                    o3 = ob[:, 0:520].rearrange("p (g d) -> p g d", g=8)
                    op('act', lambda e, o3=o3, ps=ps: e.copy(out=o3[:, :, 0:64], in_=ps[:, :].rearrange("p (g d) -> p g d", g=8)), r=[pk], w=[obk])
                    op('dve', lambda e, o3=o3: e.tensor_copy(out=o3[:, :, 64:65], in_=ones_col[:, 0:8].unsqueeze(2)), r=['ones_col'], w=[obk])
                    op('act', lambda e, ob=ob, tsl=tsl: e.dma_start(out=Va[tsl, :], in_=ob[:, 0:520]), r=[obk], w=[('Va', tt)], dma=True)
                elif kind == 'v_b':
                    ob, obk = ep_b.get()
                    o3 = ob[:, 0:516].rearrange("p (g d) -> p g d", g=4)
                    op('act', lambda e, o3=o3, ps=ps: e.copy(out=o3[:, :, 0:128], in_=ps[:, :].rearrange("p (g d) -> p g d", g=4)), r=[pk], w=[obk])
                    op('dve', lambda e, o3=o3: e.tensor_copy(out=o3[:, :, 128:129], in_=ones_col[:, 0:4].unsqueeze(2)), r=['ones_col'], w=[obk])
                    op('act', lambda e, ob=ob, tsl=tsl, arg=arg: e.dma_start(out=arg[tsl, :], in_=ob[:, 0:516]), r=[obk], w=[('Vb', id(arg), tt)], dma=True)
                elif kind == 'plainT':
                    ob, obk = ep_b.get()
                    op('act', lambda e, ob=ob, ps=ps: e.copy(out=ob[:, 0:512], in_=ps[:, :]), r=[pk], w=[obk])
                    to_featmajor(ob, obk, 4, arg, tt, 0)
                elif kind == 'ropeB_k':
                    ob, obk = ep_b.get()
                    rope(ps, pk, 4, 64, cosB, sinB, tt, ob, obk)
                    to_featmajor(ob, obk, 4, arg, tt, 0)
                elif kind == 'ropeB_q':
                    ob, obk = ep_b.get()
                    rope(ps, pk, 4, 64, cosB, sinB, tt, ob, obk)
                    to_featmajor(ob, obk, 4, QbT, tt, arg * 4)
                elif kind == 'ropeA_q':
                    ob, obk = ep_b.get()
                    rope(ps, pk, 8, 32, cosA, sinA, tt, ob, obk)
                    to_featmajor(ob, obk, 4, QaT, tt, arg * 4)
                elif kind == 'ropeA_k':
                    ob, obk = ep_b.get()
                    rope(ps, pk, 8, 32, cosA, sinA, tt, ob, obk)
                    ob2, ob2k = ep_b.get()
                    src = ob[:, 0:512].rearrange("p (g d) -> p g d", g=8)
                    d4 = ob2[:, 0:1024].rearrange("p (g t d) -> p g t d", g=8, t=2)
                    op('dve', lambda e, d4=d4, src=src: e.tensor_copy(out=d4[:, :, 0, :], in_=src), r=[obk], w=[ob2k])
                    op('dve', lambda e, d4=d4, src=src: e.tensor_copy(out=d4[:, :, 1, :], in_=src), r=[obk], w=[ob2k])
                    to_featmajor(ob2, ob2k, 8, KaT2, tt, 0)

        S.flush()
        pes.__exit__(None, None, None)
        tes.__exit__(None, None, None)
        def attention_phase(tag, nheads, hpg, dh, dv, QT_src, KT_src, V_src, vstride, plan, Odst, mask_setup, sink_tab=None):
            pes = contextlib.ExitStack()
            pes.__enter__()
            scale = float(dh) ** -0.5
            pre_g, pre_gc, mask_fn = mask_setup(pes)
            kt_r = Ring(pes, nc, tag + "K", [128, S_], BF16, 2)
            v_r = Ring(pes, nc, tag + "V", [128, NT, dv + 1], BF16, 2)
            nq = hpg if dh == 128 else hpg // 2
            q_r = Ring(pes, nc, tag + "Q", [128, S_], BF16, nq + 1)
            e_r = Ring(pes, nc, tag + "E", [128, 512], BF16, 4)
            os_r = Ring(pes, nc, tag + "O", [128, 2, dv], F32, 3)
            rec_r = Ring(pes, nc, tag + "R", [128, 2], F32, 3)
            G = nheads // hpg
            cnt = [0]
            for g in range(G):
                ksb, kk = kt_r.get()
                vsb, vk = v_r.get()
                op('sp', lambda e, ksb=ksb, g=g: e.dma_start(out=ksb[:], in_=KT_src(g)), w=[kk], dma=True)
                op('sp', lambda e, vsb=vsb, g=g: e.dma_start(out=vsb[:], in_=V_src[:, g * vstride:(g + 1) * vstride].rearrange("(kt p) d -> p kt d", p=128)),
                   w=[vk], dma=True)
                qtiles = {}
                for hh in range(hpg):
                    qsrc, pbase, qid = QT_src(g * hpg + hh)
                    if qid not in qtiles:
                        qsb, qk = q_r.get()
                        op('sp', lambda e, qsb=qsb, qsrc=qsrc: e.dma_start(out=qsb[:], in_=qsrc), w=[qk], dma=True)
                        qtiles[qid] = (qsb, qk)
                if pre_g is not None:
                    pre_g(g)
                for c in range(8):
                    kts = plan(c)
                    if pre_gc is not None:
                        pre_gc(g, c)
                    for hh in range(hpg):
                        h = g * hpg + hh
                        qsrc, pbase, qid = QT_src(h)
                        qsb, qk = qtiles[qid]
                        pr = slice(pbase, pbase + dh)
                        ob = [ps_o.get(), ps_o.get()]
                        first = [True, True]
                        for ki, kt in enumerate(kts):
                            st, stk = ps_acc.get()
                            op('pe', lambda e, st=st, ksb=ksb, qsb=qsb, pr=pr, kt=kt, c=c: e.matmul(st[:, :], lhsT=ksb[pr, kt * 128:(kt + 1) * 128], rhs=qsb[pr, c * 512:(c + 1) * 512], start=True, stop=True),
                               r=[kk, qk], w=[stk])
                            eb, ek = e_r.get()
                            op('act', lambda e, eb=eb, st=st: e.activation(out=eb[:], in_=st[:, :], func=AF.Exp, scale=scale), r=[stk], w=[ek])
                            mk = mask_fn(g, c, kt)
                            if mk is not None:
                                cnt[0] += 1
                                eng = 'dve' if cnt[0] % 2 == 0 else 'pool'
                                op(eng, lambda e, eb=eb, mk=mk: e.tensor_tensor(out=eb[:], in0=eb[:], in1=mk[0], op=ALU.mult), r=[ek, mk[1]], w=[ek])
                            for qi in range(4):
                                (ot, otk) = ob[qi // 2]
                                j = qi % 2
                                stf = first[qi // 2]
                                first[qi // 2] = False
                                op('pe', lambda e, ot=ot, eb=eb, vsb=vsb, kt=kt, qi=qi, j=j, stf=stf, last=(ki == len(kts) - 1):
                                   e.matmul(ot[:, j * (dv + 1):(j + 1) * (dv + 1)], lhsT=eb[:, qi * 128:(qi + 1) * 128], rhs=vsb[:, kt, :], start=stf, stop=last),
                                   r=[ek, vk], w=[otk])
                        for b2 in range(2):
                            ot, otk = ob[b2]
                            o3 = ot[:, 0:2 * (dv + 1)].rearrange("p (j d) -> p j d", j=2)
                            rec, rk = rec_r.get()
                            osb, osk = os_r.get()
                            s1 = sink_tab[:, h:h + 1] if sink_tab is not None else 0.0
                            op('dve', lambda e, rec=rec, o3=o3, s1=s1: e.tensor_scalar(out=rec[:].unsqueeze(2), in0=o3[:, :, dv:dv + 1], scalar1=s1, scalar2=1e-30, op0=ALU.add, op1=ALU.max),
                               r=[otk, 'esink'], w=[rk])
                            op('dve', lambda e, rec=rec: e.reciprocal(out=rec[:], in_=rec[:]), r=[rk], w=[rk])
                            op('dve', lambda e, osb=osb, o3=o3, rec=rec: e.tensor_tensor(out=osb[:], in0=o3[:, :, 0:dv], in1=rec[:].unsqueeze(2).broadcast_to([128, 2, dv]), op=ALU.mult),
                               r=[otk, rk], w=[osk])
                            t0 = c * 512 + b2 * 256
                            op('act', lambda e, osb=osb, t0=t0, h=h: e.dma_start(out=Odst[t0:t0 + 256, h * dv:(h + 1) * dv].rearrange("(j p) d -> p j d", p=128), in_=osb[:]),
                               r=[osk], w=[('O', tag, h, c, b2)], dma=True)
            S.flush()
            pes.__exit__(None, None, None)

        def static_masks(tag, masks_in, idx_fn):
            def setup(pes):
                n = len(masks_in)
                msk = pes.enter_context(nc.sbuf_tensor(tag + "msk", [128, n, 512], BF16))
                for r in range(n):
                    op('pool', lambda e, r=r: e.dma_start(out=msk[:, r, :], in_=masks_in[r]), w=[(tag, 'msk')], dma=True)
                return None, None, (lambda g, c, kt: (msk[:, idx_fn(c, kt), :], (tag, 'msk')))
            return setup

        if stages >= 2:
            esink = sb("esink", [128, 64], F32)
            op('sp', lambda e: e.dma_start(out=esink[:], in_=esink_in), w=['esink'], dma=True)
            op('act', lambda e: e.activation(out=esink[:], in_=esink[:], func=AF.Exp), r=['esink'], w=['esink'])
            attention_phase("swa", 64, 8, 64, 64,
                            lambda h: (QaT[h // 2], 64 * (h % 2), h // 2), lambda g: KaT2[g], Va, 65,
                            lambda c: [kt for kt in range(4 * c - 1, 4 * c + 4) if kt >= 0], Oa,
                            static_masks("swa", [m_swa[r] for r in range(5)], lambda c, kt: kt - 4 * c + 1), sink_tab=esink)
        if stages >= 3:
            attention_phase("win", 32, 8, 128, 128,
                            lambda h: (QbT[h], 0, h), lambda g: kwT[g], Vw, 129,
                            lambda c: [kt for kt in range(4 * c - 4, 4 * c + 4) if kt >= 0], Owin,
                            static_masks("win", [m_win[r] for r in range(8)], lambda c, kt: kt - 4 * c + 4))

        if stages >= 4:
            pes = contextlib.ExitStack()
            pes.__enter__()
            A_ = lambda n, shp, dt: pes.enter_context(nc.sbuf_tensor(n, shp, dt))
            w1k = A_("w1k", [128, 32, 256], BF16)
            w1v = A_("w1v", [128, 32, 256], BF16)
            w2k = A_("w2k", [128, 2, 128], BF16)
            w2v = A_("w2v", [128, 2, 128], BF16)
            pek = A_("pek", [128, 32], F32)
            pev = A_("pev", [128, 32], F32)
            op('pool', lambda e: e.dma_start(out=w1k[:], in_=kw1.rearrange("(l d) h -> d l h", d=128)), w=['w1k'], dma=True)
            op('pool', lambda e: e.dma_start(out=w1v[:], in_=vw1.rearrange("(l d) h -> d l h", d=128)), w=['w1v'], dma=True)
            op('pool', lambda e: e.dma_start(out=w2k[:], in_=kw2.rearrange("(hh p) d -> p hh d", p=128)), w=['w2k'], dma=True)
            op('pool', lambda e: e.dma_start(out=w2v[:], in_=vw2.rearrange("(hh p) d -> p hh d", p=128)), w=['w2v'], dma=True)
            op('sp', lambda e: e.dma_start(out=pek[:], in_=kpeT), w=['pek'], dma=True)
            op('sp', lambda e: e.dma_start(out=pev[:], in_=vpeT), w=['pev'], dma=True)
            kcmp_sb = A_("kcmp_sb", [128, 4, 256], BF16)
            vcmp_sb = A_("vcmp_sb", [128, 2, 4 * 129], BF16)
            op('dve', lambda e: e.memset(kcmp_sb[:], 0.0), w=['kcmp_sb'])
            op('dve', lambda e: e.memset(vcmp_sb[:], 0.0), w=['vcmp_sb'])
            src_r = Ring(pes, nc, "csrc", [128, S_], BF16, 2)
            tmp_r = Ring(pes, nc, "ctmp", [128, 256], BF16, 4)
            xs = A_("cxs", [128, 256], F32)
            g1 = A_("cg1", [128, 256], F32)
            g2 = A_("cg2", [128, 256], F32)
            hg = A_("chg", [128, 2, 256], BF16)
            op('dve', lambda e: e.memset(hg[:], 0.0), w=['chg'])
            for which in ('k', 'v'):
                srcT, w1, w2, pe_, = (kcT, w1k, w2k, pek) if which == 'k' else (vcT, w1v, w2v, pev)
                for g in range(4):
                    sT, sk = src_r.get()
                    op('sp', lambda e, sT=sT, g=g, srcT=srcT: e.dma_start(out=sT[:], in_=srcT[g]), w=[sk], dma=True)
                    pa = [ps_acc.get(), ps_acc.get()]
                    for l in range(32):
                        tm, tk = tmp_r.get()
                        op('dve', lambda e, tm=tm, sT=sT, l=l, pe_=pe_: e.tensor_scalar(out=tm[:, 0:255], in0=sT[:, l:l + 16 * 254 + 1:16], scalar1=pe_[:, l:l + 1], scalar2=None, op0=ALU.add),
                           r=[sk, 'pek', 'pev'], w=[tk])
                        for hh in range(2):
                            op('pe', lambda e, hh=hh, tm=tm, l=l, w1=w1, pa=pa: e.matmul(pa[hh][0][:, 0:255], lhsT=w1[:, l, hh * 128:(hh + 1) * 128], rhs=tm[:, 0:255], start=(l == 0), stop=(l == 31)),
                               r=[tk, 'w1k', 'w1v'], w=[pa[hh][1]])
                    for hh in range(2):
                        pt_, pk_ = pa[hh]
                        op('act', lambda e, pt_=pt_: e.copy(out=xs[:, 0:255], in_=pt_[:, 0:255]), r=[pk_], w=['cxs'])
                        op('dve', lambda e: e.tensor_tensor(out=g1[:, 0:255], in0=xs[:, 0:255], in1=xs[:, 0:255], op=ALU.mult), r=['cxs'], w=['cg1'])
                        op('dve', lambda e: e.tensor_scalar(out=g1[:, 0:255], in0=g1[:, 0:255], scalar1=0.044715, scalar2=1.0, op0=ALU.mult, op1=ALU.add), r=['cg1'], w=['cg1'])
                        op('dve', lambda e: e.tensor_tensor(out=g1[:, 0:255], in0=g1[:, 0:255], in1=xs[:, 0:255], op=ALU.mult), r=['cg1', 'cxs'], w=['cg1'])
                        op('act', lambda e: e.activation(out=g2[:, 0:255], in_=g1[:, 0:255], func=AF.Sigmoid, scale=1.5957691216057308), r=['cg1'], w=['cg2'])
                        op('dve', lambda e, hh=hh: e.tensor_tensor(out=hg[:, hh, 0:255], in0=xs[:, 0:255], in1=g2[:, 0:255], op=ALU.mult), r=['cxs', 'cg2'], w=['chg'])
                    if which == 'k':
                        pt_, pk_ = ps_acc.get()
                        for hh in range(2):
                            op('pe', lambda e, hh=hh, pt_=pt_: e.matmul(pt_[:, 0:256], lhsT=w2k[:, hh, :], rhs=hg[:, hh, :], start=(hh == 0), stop=(hh == 1)), r=['chg', 'w2k'], w=[pk_])
                        op('act', lambda e, pt_=pt_, g=g: e.copy(out=kcmp_sb[:, g, 0:255], in_=pt_[:, 0:255]), r=[pk_], w=['kcmp_sb'])
                    else:
                        for bt in range(2):
                            pt_, pk_ = ps_acc.get()
                            for hh in range(2):
                                op('pe', lambda e, hh=hh, pt_=pt_, bt=bt: e.matmul(pt_[:, 0:128], lhsT=hg[:, hh, bt * 128:(bt + 1) * 128], rhs=w2v[:, hh, :], start=(hh == 0), stop=(hh == 1)), r=['chg', 'w2v'], w=[pk_])
                            op('act', lambda e, pt_=pt_, g=g, bt=bt: e.copy(out=vcmp_sb[:, bt, g * 129:g * 129 + 128], in_=pt_[:, 0:128]), r=[pk_], w=['vcmp_sb'])
                            op('dve', lambda e, g=g, bt=bt: e.memset(vcmp_sb[:, bt, g * 129 + 128:g * 129 + 129], 1.0), r=[], w=['vcmp_sb'])
            for g in range(4):
                op('act', lambda e, g=g: e.dma_start(out=kcmpT_d[g][:, 0:256], in_=kcmp_sb[:, g, :]), r=['kcmp_sb'], w=[('kcmpT_d', g)], dma=True)
            op('act', lambda e: e.dma_start(out=vcmp_d[0:256, :].rearrange("(bt p) d -> p bt d", p=128), in_=vcmp_sb[:]), r=['vcmp_sb'], w=['vcmp_d'], dma=True)

            qg_r = Ring(pes, nc, "pq", [128, S_], BF16, 9)
            mc_r = Ring(pes, nc, "pmc", [128, 256], F32, 2)
            vb_r = Ring(pes, nc, "pvb", [128, 64], F32, 2)
            ad_r = Ring(pes, nc, "pad", [128, 64], F32, 2)
            ee = A_("pee", [128, 256], F32)
            den = A_("pden", [128, 1], F32)
            pg = A_("ppg", [128, 256], F32)
            s3 = A_("ps3", [128, 64], F32)
            psl = A_("ppsl", [128, 64], F32)
            sc = A_("psc", [128, 64], F32)
            sc2 = A_("psc2", [128, 64], F32)
            m8 = A_("pm8", [128, 8], F32)
            selb = A_("pselb", [128, 64], BF16)
            selT_sb = A_("pselT", [64, S_], BF16)
            for g in range(4):
                qs = []
                for hh in range(8):
                    qsb, qk = qg_r.get()
                    op('sp', lambda e, qsb=qsb, h=g * 8 + hh: e.dma_start(out=qsb[:], in_=QbT[h]), w=[qk], dma=True)
                    qs.append((qsb, qk))
                for tt in range(NT):
                    mc, mck = mc_r.get()
                    vb, vbk = vb_r.get()
                    ad, adk = ad_r.get()
                    tsl = slice(tt * 128, (tt + 1) * 128)
                    op('sp', lambda e, mc=mc, tsl=tsl: e.dma_start(out=mc[:], in_=m_cmp[tsl, :]), w=[mck], dma=True)
                    op('sp', lambda e, vb=vb, tsl=tsl: e.dma_start(out=vb[:], in_=sl_vb[tsl, :]), w=[vbk], dma=True)
                    op('sp', lambda e, ad=ad, tsl=tsl: e.dma_start(out=ad[:], in_=sl_add[tsl, :]), w=[adk], dma=True)
                    for hh in range(8):
                        qsb, qk = qs[hh]
                        pt_, pk_ = ps_acc.get()
                        op('pe', lambda e, pt_=pt_, qsb=qsb, tsl=tsl, g=g: e.matmul(pt_[:, 0:256], lhsT=qsb[:, tsl], rhs=kcmp_sb[:, g, :], start=True, stop=True), r=[qk, 'kcmp_sb'], w=[pk_])
                        op('act', lambda e, pt_=pt_: e.activation(out=ee[:], in_=pt_[:, 0:256], func=AF.Exp, scale=128.0 ** -0.5), r=[pk_], w=['pee'])
                        op('dve', lambda e, mc=mc: e.tensor_tensor(out=ee[:], in0=ee[:], in1=mc[:], op=ALU.mult), r=['pee', mck], w=['pee'])
                        op('dve', lambda e: e.reduce_sum(out=den[:], in_=ee[:], axis=AX.X), r=['pee'], w=['pden'])
                        op('dve', lambda e: e.tensor_scalar(out=den[:], in0=den[:], scalar1=1e-30, scalar2=None, op0=ALU.max), r=['pden'], w=['pden'])
                        op('dve', lambda e: e.reciprocal(out=den[:], in_=den[:]), r=['pden'], w=['pden'])
                        if hh == 0:
                            op('dve', lambda e: e.tensor_scalar(out=pg[:], in0=ee[:], scalar1=den[:, 0:1], scalar2=None, op0=ALU.mult), r=['pee', 'pden'], w=['ppg'])
                        else:
                            op('dve', lambda e: e.scalar_tensor_tensor(out=pg[:], in0=ee[:], scalar=den[:, 0:1], in1=pg[:], op0=ALU.mult, op1=ALU.add), r=['pee', 'pden', 'ppg'], w=['ppg'])
                    pgv = pg[:, :].rearrange("p (j m) -> p j m", m=4)
                    op('dve', lambda e, pgv=pgv: e.tensor_tensor(out=s3[:], in0=pgv[:, :, 0], in1=pgv[:, :, 1], op=ALU.add), r=['ppg'], w=['ps3'])
                    op('dve', lambda e, pgv=pgv: e.tensor_tensor(out=s3[:], in0=s3[:], in1=pgv[:, :, 2], op=ALU.add), r=['ppg', 'ps3'], w=['ps3'])
                    op('dve', lambda e, pgv=pgv: e.scalar_tensor_tensor(out=psl[:], in0=s3[:], scalar=2.0, in1=pgv[:, :, 3], op0=ALU.mult, op1=ALU.add), r=['ppg', 'ps3'], w=['ppsl'])
                    op('dve', lambda e, pgv=pgv: e.tensor_tensor(out=psl[:, 1:64], in0=psl[:, 1:64], in1=pgv[:, 0:63, 3], op=ALU.add), r=['ppg', 'ppsl'], w=['ppsl'])
                    op('dve', lambda e, vb=vb: e.tensor_tensor(out=sc[:], in0=psl[:], in1=vb[:], op=ALU.mult), r=['ppsl', vbk], w=['psc'])
                    op('dve', lambda e, ad=ad: e.tensor_tensor(out=sc[:], in0=sc[:], in1=ad[:], op=ALU.add), r=['psc', adk], w=['psc'])
                    op('dve', lambda e: e.max(out=m8[:], in_=sc[:]), r=['psc'], w=['pm8'])
                    op('dve', lambda e: e.match_replace(out=sc2[:], in_to_replace=m8[:], in_values=sc[:], imm_value=-1e30), r=['psc', 'pm8'], w=['psc2'])
                    op('dve', lambda e: e.max(out=m8[:], in_=sc2[:]), r=['psc2'], w=['pm8'])
                    op('dve', lambda e: e.tensor_scalar(out=sc2[:], in0=sc[:], scalar1=m8[:, 7:8], scalar2=None, op0=ALU.is_ge), r=['psc', 'pm8'], w=['psc2'])
                    op('dve', lambda e, vb=vb: e.tensor_tensor(out=selb[:], in0=sc2[:], in1=vb[:], op=ALU.mult), r=['psc2', vbk], w=['pselb'])
                    ptt, ptk = ps_t.get()
                    op('pe', lambda e, ptt=ptt: e.transpose(out=ptt[0:64, 0, :], in_=selb[:, 0:64], identity=idb[:]), r=['pselb', 'idb'], w=[ptk])
                    op('act', lambda e, ptt=ptt, tsl=tsl: e.copy(out=selT_sb[:, tsl], in_=ptt[0:64, 0, :]), r=[ptk], w=['pselT'])
                op('act', lambda e, g=g: e.dma_start(out=selT_d[g], in_=selT_sb[:]), r=['pselT'], w=[('selT_d', g)], dma=True)
            S.flush()
            pes.__exit__(None, None, None)

            def cmp_masks(pes):
                msk = pes.enter_context(nc.sbuf_tensor("cmpmsk", [128, 2, S_], BF16))
                for bt in range(2):
                    op('pool', lambda e, bt=bt: e.dma_start(out=msk[:, bt, :], in_=m_cmpT[bt]), w=[('cmp', 'msk')], dma=True)
                return None, None, (lambda g, c, kt: (msk[:, kt, c * 512:(c + 1) * 512], ('cmp', 'msk')))
            attention_phase("cmp", 32, 8, 128, 128, lambda h: (QbT[h], 0, h), lambda g: kcmpT_d[g], vcmp_d, 129,
                            lambda c: [0, 1], Ocmp, cmp_masks)

        if stages >= 5:
            def slc_masks(pes):
                exm = pes.enter_context(nc.sbuf_tensor("slcex", [64, 32, 128], BF16))
                mwin = pes.enter_context(nc.sbuf_tensor("slcmw", [128, 4, 512], BF16))
                mk_all = pes.enter_context(nc.sbuf_tensor("slcmk", [128, 32, 512], BF16))
                selr = Ring(pes, nc, "slcsel", [64, S_], BF16, 2)
                op('pool', lambda e: e.dma_start(out=exm[:], in_=exmat.rearrange("b (k q) -> b k q", k=32)), w=['slcex'], dma=True)
                for r in range(4):
                    op('pool', lambda e, r=r: e.dma_start(out=mwin[:, r, :], in_=m_win[4 + r]), w=['slcmw'], dma=True)
                cur = {}

                def pre_g(g):
                    sb_, sk_ = selr.get()
                    op('sp', lambda e, sb_=sb_, g=g: e.dma_start(out=sb_[:], in_=selT_d[g]), w=[sk_], dma=True)
                    cur['sel'] = (sb_, sk_)

                def pre_gc(g, c):
                    sb_, sk_ = cur['sel']
                    for kt in range(4 * c + 4):
                        pt_, pk_ = ps_acc.get()
                        op('pe', lambda e, pt_=pt_, kt=kt, sb_=sb_, c=c: e.matmul(pt_[:, :], lhsT=exm[:, kt, :], rhs=sb_[:, c * 512:(c + 1) * 512], start=True, stop=True),
                           r=['slcex', sk_], w=[pk_])
                        if kt >= 4 * c:
                            op('dve', lambda e, pt_=pt_, kt=kt, c=c: e.tensor_tensor(out=mk_all[:, kt, :], in0=pt_[:, :], in1=mwin[:, kt - 4 * c, :], op=ALU.mult),
                               r=[pk_, 'slcmw'], w=[('slcmk', kt)])
                        else:
                            op('act', lambda e, pt_=pt_, kt=kt: e.copy(out=mk_all[:, kt, :], in_=pt_[:, :]), r=[pk_], w=[('slcmk', kt)])
                return pre_g, pre_gc, (lambda g, c, kt: (mk_all[:, kt, :], ('slcmk', kt)))
            attention_phase("slc", 32, 8, 128, 128, lambda h: (QbT[h], 0, h), lambda g: ksT[g], Vs, 129,
                            lambda c: list(range(4 * c + 4)), Oslc, slc_masks)

        fin = []
        if stages >= 6:
            pes = contextlib.ExitStack()
            pes.__enter__()
            HW = 2048
            r_oa = Ring(pes, nc, "m_oa", [128, HW], F32, 2)
            r_oc = Ring(pes, nc, "m_oc", [128, HW], F32, 2)
            r_os = Ring(pes, nc, "m_os", [128, HW], F32, 2)
            r_ow = Ring(pes, nc, "m_ow", [128, HW], F32, 2)
            r_ga = Ring(pes, nc, "m_ga", [128, HW], F32, 2)
            r_gb = Ring(pes, nc, "m_gb", [128, HW], F32, 2)
            r_gn = Ring(pes, nc, "m_gn", [128, 96], F32, 2)
            r_acc = Ring(pes, nc, "m_acc", [128, HW], F32, 2)
            r_tmp = Ring(pes, nc, "m_tmp", [128, HW], F32, 4)
            r_y = Ring(pes, nc, "m_y", [128, D_], BF16, 2)
            r_yT = Ring(pes, nc, "m_yT", [128, 32, 128], BF16, 2)
            for tt in range(NT):
                tsl = slice(tt * 128, (tt + 1) * 128)
                gn, gnk = r_gn.get()
                op('sp', lambda e, gn=gn, tsl=tsl: e.dma_start(out=gn[:], in_=Gn[tsl, :]), w=[gnk], dma=True)
                yb, ybk = r_y.get()
                for hf in range(2):
                    cs = slice(hf * HW, (hf + 1) * HW)
                    tiles = []
                    for ring, src in ((r_oa, Oa), (r_oc, Ocmp), (r_os, Oslc), (r_ow, Owin), (r_ga, Ga), (r_gb, Gb)):
                        t_, k_ = ring.get()
                        op('sp', lambda e, t_=t_, src=src, tsl=tsl, cs=cs: e.dma_start(out=t_[:], in_=src[tsl, cs]), w=[k_], dma=True)
                        tiles.append((t_, k_))
                    (oa, oak), (oc, ock), (os_, osk), (ow, owk), (ga, gak), (gb_, gbk) = tiles
                    acc, ack = r_acc.get()
                    tmp, tmk = r_tmp.get()
                    v3 = lambda t_: t_[:, :].rearrange("p (h d) -> p h d", h=16)
                    gsl_ = [gn[:, j * 32 + hf * 16: j * 32 + hf * 16 + 16].unsqueeze(2).broadcast_to([128, 16, 128]) for j in range(3)]
                    gsl = lambda j, gsl_=gsl_: gsl_[j]
                    tmp2, tm2k = r_tmp.get()
                    op('dve', lambda e, acc=acc, oc=oc, g0=gsl_[0]: e.tensor_tensor(out=v3(acc), in0=v3(oc), in1=g0, op=ALU.mult), r=[ock, gnk], w=[ack])
                    op('dve', lambda e, tmp=tmp, os_=os_, g1_=gsl_[1]: e.tensor_tensor(out=v3(tmp), in0=v3(os_), in1=g1_, op=ALU.mult), r=[osk, gnk], w=[tmk])
                    op('pool', lambda e, acc=acc, tmp=tmp: e.tensor_tensor(out=acc[:], in0=acc[:], in1=tmp[:], op=ALU.add), r=[ack, tmk], w=[ack])
                    op('dve', lambda e, tmp2=tmp2, ow=ow, g2_=gsl_[2]: e.tensor_tensor(out=v3(tmp2), in0=v3(ow), in1=g2_, op=ALU.mult), r=[owk, gnk], w=[tm2k])
                    op('pool', lambda e, acc=acc, tmp2=tmp2: e.tensor_tensor(out=acc[:], in0=acc[:], in1=tmp2[:], op=ALU.add), r=[ack, tm2k], w=[ack])
                    op('pool', lambda e, acc=acc, gb_=gb_: e.tensor_tensor(out=acc[:], in0=acc[:], in1=gb_[:], op=ALU.mult), r=[ack, gbk], w=[ack])
                    op('pool', lambda e, tmp=tmp, oa=oa, ga=ga: e.tensor_tensor(out=tmp[:], in0=oa[:], in1=ga[:], op=ALU.mult), r=[oak, gak], w=[tmk])
                    op('dve', lambda e, acc=acc, tmp=tmp, yb=yb, cs=cs: e.tensor_tensor(out=yb[:, cs], in0=acc[:], in1=tmp[:], op=ALU.add), r=[ack, tmk], w=[ybk])
                yT, yTk = r_yT.get()
                transpose_blocks(lambda j, yb=yb: yb[:, j * 128:(j + 1) * 128], 32, lambda j0, n, yT=yT: yT[:, j0:j0 + n, :], [ybk], lambda j0, yTk=yTk: yTk)
                op('act', lambda e, yT=yT, tt=tt: e.dma_start(out=yT_d[tt].rearrange("p (k t) -> p k t", k=32), in_=yT[:]), r=[yTk], w=[('yT_d', tt)], dma=True)
            S.flush()
            pes.__exit__(None, None, None)

            pes = contextlib.ExitStack()
            pes.__enter__()
            wb_r = Ring(pes, nc, "wob", [128, 32, 512], BF16, 2)
            xl_r = Ring(pes, nc, "oyl", [128, 32, 128], BF16, 3)
            xs_r = Ring(pes, nc, "oxs", [128, 512], F32, 3)
            rs_r = Ring(pes, nc, "ors", [128, 512], F32, 3)
            w_o_v = w_o.rearrange("(k p) c -> p k c", p=128)
            for cc in range(8):
                wb, wk = wb_r.get()
                for k0 in range(0, 32, 8):
                    op('pool', lambda e, wb=wb, k0=k0, cc=cc: e.dma_start(out=wb[:, k0:k0 + 8, :], in_=w_o_v[:, k0:k0 + 8, cc * 512:(cc + 1) * 512]), w=[(wk, k0)], dma=True)
                for tt in range(NT):
                    tsl = slice(tt * 128, (tt + 1) * 128)
                    xl, xk = xl_r.get()
                    op('sp', lambda e, xl=xl, tt=tt: e.dma_start(out=xl[:], in_=yT_d[tt].rearrange("p (k t) -> p k t", k=32)), w=[xk], dma=True)
                    xs, xsk = xs_r.get()
                    op('sp', lambda e, xs=xs, tsl=tsl, cc=cc: e.dma_start(out=xs[:], in_=x[tsl, cc * 512:(cc + 1) * 512]), w=[xsk], dma=True)
                    ps, pk = ps_acc.get()
                    for k in range(32):
                        op('pe', lambda e, ps=ps, xl=xl, wb=wb, k=k: e.matmul(ps[:, :], lhsT=xl[:, k, :], rhs=wb[:, k, :], start=(k == 0), stop=(k == 31)),
                           r=[xk, (wk, (k // 8) * 8)], w=[pk])
                    rs, rsk = rs_r.get()
                    op('dve', lambda e, rs=rs, xs=xs, ps=ps: e.scalar_tensor_tensor(out=rs[:], in0=xs[:], scalar=ALPHA, in1=ps[:, :], op0=ALU.mult, op1=ALU.add), r=[xsk, pk], w=[rsk])
                    op('act', lambda e, rs=rs, tsl=tsl, cc=cc: e.dma_start(out=R1[tsl, cc * 512:(cc + 1) * 512], in_=rs[:]), r=[rsk], w=[('R1', tt, cc)], dma=True)
            S.flush()
            pes.__exit__(None, None, None)

        def layer_norm_tile(rt, rtk, gsrc, bsrc, g_r, b_r, st, mv, rsd, tagk):
            for c8 in range(8):
                op('dve', lambda e, c8=c8: e.bn_stats(out=st[:, c8, :], in_=rt[:, c8 * 512:(c8 + 1) * 512]), r=[rtk], w=[tagk + 'st'])
            op('dve', lambda e: e.bn_aggr(out=mv[:], in_=st[:].rearrange("p a b -> p (a b)")), r=[tagk + 'st'], w=[tagk + 'mv'])
            op('dve', lambda e: e.tensor_scalar(out=rsd[:], in0=mv[:, 1:2], scalar1=EPS, scalar2=None, op0=ALU.add), r=[tagk + 'mv'], w=[tagk + 'rs'])
            op('act', lambda e: e.activation(out=rsd[:], in_=rsd[:], func=AF.Sqrt), r=[tagk + 'rs'], w=[tagk + 'rs'])
            op('dve', lambda e: e.reciprocal(out=rsd[:], in_=rsd[:]), r=[tagk + 'rs'], w=[tagk + 'rs'])
            op('dve', lambda e: e.tensor_scalar(out=rt, in0=rt, scalar1=mv[:, 0:1], scalar2=rsd[:, 0:1], op0=ALU.subtract, op1=ALU.mult), r=[rtk, tagk + 'mv', tagk + 'rs'], w=[rtk])
            for c8 in range(8):
                cs = slice(c8 * 512, (c8 + 1) * 512)
                gt_, gk_ = g_r.get()
                bt_, bk_ = b_r.get()
                op('sp', lambda e, gt_=gt_, cs=cs: e.dma_start(out=gt_[:], in_=gsrc[:, cs]), w=[gk_], dma=True)
                op('sp', lambda e, bt_=bt_, cs=cs: e.dma_start(out=bt_[:], in_=bsrc[:, cs]), w=[bk_], dma=True)
                op('dve', lambda e, gt_=gt_, cs=cs: e.tensor_tensor(out=rt[:, cs], in0=rt[:, cs], in1=gt_[:], op=ALU.mult), r=[rtk, gk_], w=[rtk])
                op('pool', lambda e, bt_=bt_, cs=cs: e.tensor_tensor(out=rt[:, cs], in0=rt[:, cs], in1=bt_[:], op=ALU.add), r=[rtk, bk_], w=[rtk])

        if stages >= 7:
            pes = contextlib.ExitStack()
            pes.__enter__()
            A_ = lambda n, shp, dt: pes.enter_context(nc.sbuf_tensor(n, shp, dt))
            r_r = Ring(pes, nc, "l_r", [128, D_], F32, 2)
            g_r = Ring(pes, nc, "l_g", [128, 512], F32, 3)
            b_r = Ring(pes, nc, "l_b", [128, 512], F32, 3)
            st = A_("l_st", [128, 8, 6], F32)
            mv = A_("l_mv", [128, 2], F32)
            rsd = A_("l_rsd", [128, 1], F32)
            xb_r2 = Ring(pes, nc, "l_xb", [128, D_], BF16, 2)
            xT_r2 = Ring(pes, nc, "l_xT", [128, 32, 128], BF16, 2)
            xTf = A_("l_xTf", [128, 32, 128], F32)
            wr_sb = A_("l_wr", [128, 32, 64], F32)
            rb_sb = A_("l_rb", [128, 64], F32)
            scs = A_("l_sc", [128, 64], F32)
            sel = A_("l_sel", [128, 64], F32)
            m8r = A_("l_m8", [128, 8], F32)
            ssum = A_("l_ss", [128, 1], F32)
            gt_r = Ring(pes, nc, "l_gt", [128, 65], F32, 2)
            op('sp', lambda e: e.dma_start(out=wr_sb[:], in_=w_r.rearrange("(k p) c -> p k c", p=128)), w=['l_wr'], dma=True)
            op('sp', lambda e: e.dma_start(out=rb_sb[:], in_=rbias), w=['l_rb'], dma=True)
            for tt in range(NT):
                tsl = slice(tt * 128, (tt + 1) * 128)
                rt, rtk = r_r.get()
                op('sp', lambda e, rt=rt, tsl=tsl: e.dma_start(out=rt[:], in_=R1[tsl, :]), w=[rtk], dma=True)
                layer_norm_tile(rt[:, :], rtk, ln1g, ln1b, g_r, b_r, st, mv, rsd, 'l1')
                op('act', lambda e, rt=rt, tsl=tsl: e.dma_start(out=X1[tsl, :], in_=rt[:]), r=[rtk], w=[('X1', tt)], dma=True)
                xb, xbk = xb_r2.get()
                op('act', lambda e, xb=xb, rt=rt: e.copy(out=xb[:], in_=rt[:]), r=[rtk], w=[xbk])
                xT, xTk = xT_r2.get()
                transpose_blocks(lambda j, xb=xb: xb[:, j * 128:(j + 1) * 128], 32, lambda j0, n, xT=xT: xT[:, j0:j0 + n, :], [xbk], lambda j0, xTk=xTk: xTk)
                op('act', lambda e, xT=xT, tt=tt: e.dma_start(out=x1T_d[tt].rearrange("p (k t) -> p k t", k=32), in_=xT[:]), r=[xTk], w=[('x1T_d', tt)], dma=True)
                for j0 in range(0, 32, 4):
                    pf, pfk = ps_o.get()
                    for j in range(4):
                        op('pe', lambda e, pf=pf, j=j, j0=j0, rt=rt: e.transpose(out=pf[:, j * 128:(j + 1) * 128], in_=rt[:, (j0 + j) * 128:(j0 + j + 1) * 128], identity=idf[:]),
                           r=[rtk, 'idf'], w=[pfk])
                    op('act', lambda e, pf=pf, j0=j0: e.copy(out=xTf[:, j0:j0 + 4, :], in_=pf[:, :].rearrange("p (j t) -> p j t", j=4)), r=[pfk], w=[('l_xTf', j0)])
                pr_, prk = ps_acc.get()
                for k in range(32):
                    op('pe', lambda e, pr_=pr_, k=k: e.matmul(pr_[:, 0:64], lhsT=xTf[:, k, :], rhs=wr_sb[:, k, :], start=(k == 0), stop=(k == 31)),
                       r=[('l_xTf', (k // 4) * 4), 'l_wr'], w=[prk])
                gt, gtk = gt_r.get()
                op('act', lambda e, pr_=pr_: e.activation(out=scs[:], in_=pr_[:, 0:64], func=AF.Sigmoid), r=[prk], w=['l_sc'])
                op('dve', lambda e: e.tensor_tensor(out=sel[:], in0=scs[:], in1=rb_sb[:], op=ALU.add), r=['l_sc', 'l_rb'], w=['l_sel'])
                op('dve', lambda e: e.max(out=m8r[:], in_=sel[:]), r=['l_sel'], w=['l_m8'])
                op('dve', lambda e: e.tensor_scalar(out=sel[:], in0=sel[:], scalar1=m8r[:, 7:8], scalar2=None, op0=ALU.is_ge), r=['l_sel', 'l_m8'], w=['l_sel'])
                op('dve', lambda e: e.tensor_tensor(out=sel[:], in0=sel[:], in1=scs[:], op=ALU.mult), r=['l_sel', 'l_sc'], w=['l_sel'])
                op('dve', lambda e: e.reduce_sum(out=ssum[:], in_=sel[:], axis=AX.X), r=['l_sel'], w=['l_ss'])
                op('dve', lambda e: e.reciprocal(out=ssum[:], in_=ssum[:]), r=['l_ss'], w=['l_ss'])
                op('dve', lambda e, gt=gt: e.tensor_scalar(out=gt[:, 0:64], in0=sel[:], scalar1=ssum[:, 0:1], scalar2=2.5, op0=ALU.mult, op1=ALU.mult), r=['l_sel', 'l_ss'], w=[gtk])
                op('dve', lambda e, gt=gt: e.memset(gt[:, 64:65], 1.0), w=[gtk])
                op('act', lambda e, gt=gt, tsl=tsl: e.dma_start(out=Gt[tsl, :], in_=gt[:]), r=[gtk], w=[('Gt', tt)], dma=True)
            S.flush()
            pes.__exit__(None, None, None)

        if stages >= 8:
            pes = contextlib.ExitStack()
            pes.__enter__()
            A_ = lambda n, shp, dt: pes.enter_context(nc.sbuf_tensor(n, shp, dt))
            acc = A_("e_acc", [128, 4, D_], F32)
            x1c = A_("e_x1c", [128, 32, 512], BF16)
            gts = A_("e_gt", [128, 4, 65], F32)
            wg_r = Ring(pes, nc, "e_wg", [128, 32, 256], BF16, 2)
            wu_r = Ring(pes, nc, "e_wu", [128, 32, 256], BF16, 2)
            wd_r = Ring(pes, nc, "e_wd", [128, 2, D_], BF16, 2)
            sg_r = Ring(pes, nc, "e_sg", [128, 512], F32, 2)
            h_r = Ring(pes, nc, "e_h", [128, 512], BF16, 4)
            g_r = Ring(pes, nc, "e_g", [128, 512], F32, 1)
            b_r = Ring(pes, nc, "e_b", [128, 512], F32, 1)
            st = A_("e_st", [128, 8, 6], F32)
            mv = A_("e_mv", [128, 2], F32)
            rsd = A_("e_rsd", [128, 1], F32)
            NE = 65
            for tc in range(8):
                for j in range(4):
                    tt = tc * 4 + j
                    tsl = slice(tt * 128, (tt + 1) * 128)
                    op('sp', lambda e, j=j, tsl=tsl: e.dma_start(out=acc[:, j, :], in_=X1[tsl, :]), w=[('e_acc', j)], dma=True)
                    op('act', lambda e, j=j: e.activation(out=acc[:, j, :], in_=acc[:, j, :], func=AF.Copy, scale=ALPHA), r=[('e_acc', j)], w=[('e_acc', j)])
                    op('sp', lambda e, j=j, tt=tt: e.dma_start(out=x1c[:, :, j * 128:(j + 1) * 128], in_=x1T_d[tt].rearrange("p (k t) -> p k t", k=32)), w=[('e_x1c', j)], dma=True)
                    op('sp', lambda e, j=j, tsl=tsl: e.dma_start(out=gts[:, j, :], in_=Gt[tsl, :]), w=[('e_gt', j)], dma=True)
                xkeys = [('e_x1c', j) for j in range(4)]
                steps = [(ex, fh) for ex in range(NE) for fh in range(2)]
                nst = len(steps)
                gu_banks = [(ps_acc.bufs[0], ('psacc', 0)), (ps_acc.bufs[1], ('psacc', 1)), (ps_o.bufs[0], ('pso', 0)), (ps_o.bufs[1], ('pso', 1))]
                dn_banks = [(ps_o.bufs[2], ('pso', 2)), (ps_o.bufs[3], ('pso', 3))]
                wts = {}
                hts_of = {}

                def load_gu(i):
                    ex, fh = steps[i]
                    wg, wgk = wg_r.get()
                    wu, wuk = wu_r.get()
                    op('sp', lambda e, wg=wg, ex=ex, fh=fh: e.dma_start(out=wg[:], in_=eg_b[ex, fh].rearrange("p (k f) -> p k f", k=32)), r=[('ecast', 'g', ex, fh)], w=[wgk], dma=True)
                    op('sp', lambda e, wu=wu, ex=ex, fh=fh: e.dma_start(out=wu[:], in_=eu_b[ex, fh].rearrange("p (k f) -> p k f", k=32)), r=[('ecast', 'u', ex, fh)], w=[wuk], dma=True)
                    wts[('gu', i)] = (wg, wgk, wu, wuk)

                def load_d(i):
                    ex, fh = steps[i]
                    wd, wdk = wd_r.get()
                    op('sp', lambda e, wd=wd, ex=ex, fh=fh: e.dma_start(out=wd[:], in_=ed_b[ex, fh].rearrange("p (t c) -> p t c", t=2)), r=[('ecast', 'd', ex, fh)], w=[wdk], dma=True)
                    wts[('d', i)] = (wd, wdk)

                def gu_group(i, q):
                    wg, wgk, wu, wuk = wts[('gu', i)]
                    for m in range(q * 4, q * 4 + 4):
                        ft, rem = divmod(m, 64)
                        isu, k = divmod(rem, 32)
                        bank, bk = gu_banks[ft * 2 + isu]
                        w_, wk_ = (wu, wuk) if isu else (wg, wgk)
                        op('pe', lambda e, bank=bank, w_=w_, k=k, ft=ft: e.matmul(bank[:, :], lhsT=w_[:, k, ft * 128:(ft + 1) * 128], rhs=x1c[:, k, :], start=(k == 0), stop=(k == 31)),
                           r=[wk_] + xkeys, w=[bk])
                    if q in (15, 31):
                        ft = 0 if q == 15 else 1
                        (pg_, pgk), (pu_, puk) = gu_banks[ft * 2], gu_banks[ft * 2 + 1]
                        sg, sgk = sg_r.get()
                        op('act', lambda e, sg=sg, pg_=pg_: e.activation(out=sg[:], in_=pg_[:, :], func=AF.Silu), r=[pgk], w=[sgk])
                        hT, hk = h_r.get()
                        op('dve', lambda e, hT=hT, sg=sg, pu_=pu_: e.tensor_tensor(out=hT[:], in0=sg[:], in1=pu_[:, :], op=ALU.mult), r=[sgk, puk], w=[hk])
                        hts_of.setdefault(i, []).append((hT, hk))

                def dn_group(i, q):
                    ex, fh = steps[i]
                    wd, wdk = wts[('d', i)]
                    tq, cc = divmod(q, 8)
                    po, pok = dn_banks[q % 2]
                    for ft in range(2):
                        hT, hk = hts_of[i][ft]
                        op('pe', lambda e, po=po, hT=hT, wd=wd, ft=ft, tq=tq, cc=cc: e.matmul(po[:, :], lhsT=hT[:, tq * 128:(tq + 1) * 128], rhs=wd[:, ft, cc * 512:(cc + 1) * 512], start=(ft == 0), stop=(ft == 1)),
                           r=[hk, wdk], w=[pok])
                    op('dve', lambda e, po=po, tq=tq, cc=cc, ex=ex: e.scalar_tensor_tensor(out=acc[:, tq, cc * 512:(cc + 1) * 512], in0=po[:, :], scalar=gts[:, tq, ex:ex + 1], in1=acc[:, tq, cc * 512:(cc + 1) * 512], op0=ALU.mult, op1=ALU.add),
                       r=[pok, ('e_gt', tq), ('e_acc', tq)], w=[('e_acc', tq)])

                load_gu(0)
                load_d(0)
                load_gu(1)
                for q in range(32):
                    gu_group(0, q)
                for i in range(nst):
                    if i + 2 < nst:
                        load_gu(i + 2)
                    if i + 1 < nst:
                        load_d(i + 1)
                    for q in range(32):
                        if i + 1 < nst:
                            gu_group(i + 1, q)
                        dn_group(i, q)
                    hts_of.pop(i, None)
                    wts.pop(('gu', i), None)
                    wts.pop(('d', i), None)
                for j in range(4):
                    tt = tc * 4 + j
                    tsl = slice(tt * 128, (tt + 1) * 128)
                    layer_norm_tile(acc[:, j, :], ('e_acc', j), ln2g, ln2b, g_r, b_r, st, mv, rsd, 'l2')
                    fin.append(op('act', lambda e, j=j, tsl=tsl: e.dma_start(out=out[tsl, :], in_=acc[:, j, :]), r=[('e_acc', j)], w=[('out', tt)], dma=True))
            S.wait_all('act', fin)
            S.flush()
            pes.__exit__(None, None, None)
        else:
            zt = sb("zt", [128, D_], F32)
            op('dve', lambda e: e.memset(zt[:], 0.0), w=['zt'])
            for tt in range(NT):
                fin.append(op('act', lambda e, tt=tt: e.dma_start(out=out[tt * 128:(tt + 1) * 128, :], in_=zt[:]), r=['zt'], w=[('out', tt)], dma=True))
            S.wait_all('act', fin)
            S.flush()
        S.close()
    return nc


def host_constants():
    c = {}
    c["ident"] = np.eye(128, dtype=np.float32)
    inva = (10000.0 ** (-np.arange(32, dtype=np.float32) * 2.0 / 64)).astype(np.float32)
    invb = (10000.0 ** (-np.arange(64, dtype=np.float32) * 2.0 / 128)).astype(np.float32)
    c["inva"] = np.ascontiguousarray(np.broadcast_to(inva, (128, 32)))
    c["invb"] = np.ascontiguousarray(np.broadcast_to(invb, (128, 64)))
    k = np.arange(128)[:, None]
    q = np.arange(512)[None, :]

    def band(r, W):
        d = q - (128 * r + k)
        return ((d >= 0) & (d < W)).astype(np.float32)
    c["m_swa"] = np.stack([band(r, 128) for r in range(-1, 4)])
    c["m_win"] = np.stack([band(r, 512) for r in range(-4, 4)])
    t = np.arange(S_)[:, None]
    blk = np.arange(256)[None, :]
    mc = ((16 * blk + 31 <= t) & (blk < 255)).astype(np.float32)
    c["m_cmp"] = mc
    c["m_cmpT"] = np.ascontiguousarray(mc.T.reshape(2, 128, S_))
    j = np.arange(64)[None, :]
    cur = t // 64
    valid = (j * 64 <= t)
    forced = (j == 0) | (j == cur) | (j == cur - 1)
    c["sl_vb"] = valid.astype(np.float32)
    c["sl_add"] = (1e9 * forced - (~valid)).astype(np.float32)
    ex = np.zeros((64, 32, 128), np.float32)
    for kt in range(32):
        for kk in range(128):
            ex[2 * kt + kk // 64, kt, kk] = 1.0
    c["exmat"] = ex.reshape(64, 32 * 128)
    return c


def lay_gu(w):
    E = w.shape[0]
    return np.ascontiguousarray(w.reshape(E, 32, 128, 2, 256).transpose(0, 3, 2, 1, 4)).reshape(E, 2, 128, 32 * 256)


def lay_d(w):
    E = w.shape[0]
    return np.ascontiguousarray(w.reshape(E, 2, 2, 128, 4096).transpose(0, 1, 3, 2, 4)).reshape(E, 2, 128, 2 * 4096)


def kernel(x, positions, w_in, a_sinks, cmp_k_pos, cmp_k_w1, cmp_k_w2, cmp_v_pos, cmp_v_w1, cmp_v_w2,
           w_o, ln1_g, ln1_b, w_router, router_bias, exp_w_gate, exp_w_up, exp_w_down,
           sh_w_gate, sh_w_up, sh_w_down, ln2_g, ln2_b):
    f = lambda a: np.ascontiguousarray(np.asarray(a))
    bc = lambda v, n: np.ascontiguousarray(np.broadcast_to(np.asarray(v).reshape(1, -1), (128, n)))
    nc = build_nc()
    shared = host_constants()
    shared.update({
        "w_in": f(w_in[0]), "sinks_b": bc(a_sinks[0], 64),
        "kpeT": f(np.asarray(cmp_k_pos[0]).T), "vpeT": f(np.asarray(cmp_v_pos[0]).T),
        "kw1": f(cmp_k_w1[0]), "vw1": f(cmp_v_w1[0]), "kw2": f(cmp_k_w2[0]), "vw2": f(cmp_v_w2[0]),
        "w_o": f(w_o[0]), "ln1g": bc(ln1_g[0], D_), "ln1b": bc(ln1_b[0], D_), "ln2g": bc(ln2_g[0], D_), "ln2b": bc(ln2_b[0], D_),
        "w_r": f(w_router[0]), "rbias": bc(router_bias[0], 64),
        "eg": lay_gu(np.concatenate([np.asarray(exp_w_gate[0]), np.asarray(sh_w_gate[0])[None]], 0)),
        "eu": lay_gu(np.concatenate([np.asarray(exp_w_up[0]), np.asarray(sh_w_up[0])[None]], 0)),
        "ed": lay_d(np.concatenate([np.asarray(exp_w_down[0]), np.asarray(sh_w_down[0])[None]], 0)),
    })
    in_maps = []
    for b in range(NCORES):
        m = dict(shared)
        m["x"] = f(np.asarray(x)[b])
        m["pos"] = f(np.asarray(positions)[b].astype(np.int32).reshape(NT, 128).T)
        in_maps.append({k: v for k, v in m.items() if k in DECLARED})
    res = run_bass_kernel_spmd(nc, in_maps, core_ids=list(range(NCORES)))
    return np.stack([np.asarray(res.results[b]["out"]) for b in range(NCORES)], 0).astype(np.float32)
```

```python
import math
import contextlib
import numpy as np
import concourse.bass as bass
import concourse.mybir as mybir
from concourse.bass_utils import run_bass_kernel_spmd

F32 = mybir.dt.float32
BF16 = mybir.dt.bfloat16
I32 = mybir.dt.int32
AF = mybir.ActivationFunctionType
ALU = mybir.AluOpType
AX = mybir.AxisListType

COMPUTE = ('pe', 'act', 'dve', 'pool')
NCORES = 2
S_ = 4096
D_ = 4096
NT = S_ // 128
ALPHA = 2.0 ** 0.25
EPS = 1e-5
TWO_PI = 2.0 * math.pi
C1 = 6.28125
C2 = TWO_PI - C1


class Sched:
    def __init__(self, nc, n_dma_sems=8):
        self.nc = nc
        self.streams = {e: [] for e in ('pe', 'act', 'dve', 'pool', 'sp')}
        self._sem_ctx = []
        self.sems = {e: self._mksem('c_' + e) for e in COMPUTE}
        self.seq = {e: 0 for e in COMPUTE}
        self.dma_sems, self.dma_val, self.dma_rr = {}, {}, {}
        for q in ('sp', 'act', 'pool'):
            self.dma_sems[q] = [self._mksem('d_%s%d' % (q, i)) for i in range(n_dma_sems)]
            self.dma_rr[q] = 0
            for s in self.dma_sems[q]:
                self.dma_val[id(s)] = 0
        self.observed = {e: {} for e in self.streams}
        self.last_w = {}
        self.readers = {}
        self.n_ins = 0

    def _mksem(self, name):
        ctx = self.nc.semaphore(name)
        h = ctx.__enter__()
        self._sem_ctx.append(ctx)
        return h

    def close(self):
        for c in reversed(self._sem_ctx):
            c.__exit__(None, None, None)

    def _need(self, eng, dep, waits):
        if dep is None:
            return
        sem, val, deng = dep
        if deng == eng and eng == 'pe':
            return
        ob = self.observed[eng]
        if ob.get(id(sem), 0) >= val:
            return
        ob[id(sem)] = val
        for i, (s, v) in enumerate(waits):
            if s is sem:
                waits[i] = (s, max(v, val))
                return
        waits.append((sem, val))

    def op(self, eng, fn, reads=(), writes=(), dma=False):
        waits = []
        for k in reads:
            self._need(eng, self.last_w.get(k), waits)
        for k in writes:
            self._need(eng, self.last_w.get(k), waits)
            for r in list(self.readers.get(k, {}).values()):
                self._need(eng, r, waits)
        if dma:
            lst = self.dma_sems[eng]
            s = lst[self.dma_rr[eng] % len(lst)]
            self.dma_rr[eng] += 1
            old = self.dma_val[id(s)]
            if old > 0:
                self._need(eng, (s, old, 'dma'), waits)
            val = old + 16
            self.dma_val[id(s)] = val
            comp = (s, val, 'dma')
            inc = (s, 16)
        else:
            self.seq[eng] += 1
            comp = (self.sems[eng], self.seq[eng], eng)
            inc = (self.sems[eng], 1)
        for k in reads:
            self.readers.setdefault(k, {})[id(comp[0])] = comp
        for k in writes:
            self.last_w[k] = comp
            self.readers[k] = {}
        self.streams[eng].append((waits, fn, inc))
        self.n_ins += 1
        return comp

    def wait_all(self, eng, comps):
        waits = []
        for c in comps:
            self._need(eng, c, waits)
        self.streams[eng].append((waits, None, None))

    def barrier(self):
        comps = [(self.sems[e], self.seq[e], e) for e in COMPUTE if self.seq[e] > 0]
        for q in self.dma_sems:
            for sm in self.dma_sems[q]:
                if self.dma_val[id(sm)] > 0:
                    comps.append((sm, self.dma_val[id(sm)], 'dma'))
        for e in self.streams:
            waits = []
            for c in comps:
                sem, val, deng = c
                ob = self.observed[e]
                if ob.get(id(sem), 0) >= val:
                    continue
                ob[id(sem)] = val
                waits.append((sem, val))
            self.streams[e].append((waits, None, None))

    def flush(self):
        self.barrier()
        self.emit()
        self.streams = {e: [] for e in self.streams}

    def emit(self):
        engobj = {'pe': 'tensor', 'act': 'scalar', 'dve': 'vector', 'pool': 'gpsimd', 'sp': 'sync'}
        with self.nc.Block() as block:
            for e, attr in engobj.items():
                stream = self.streams[e]

                def body(engine, stream=stream):
                    for waits, fn, inc in stream:
                        for (s, v) in waits:
                            engine.wait_ge(s, v)
                        if fn is not None:
                            fn(engine).then_inc(inc[0], inc[1])
                getattr(block, attr)(body)


class Ring:
    def __init__(self, es, nc, name, shape, dt, n, psum=False):
        mk = nc.psum_tensor if psum else nc.sbuf_tensor
        self.bufs = [es.enter_context(mk("%s%d" % (name, i), shape, dt)) for i in range(n)]
        self.name = name
        self.i = 0

    def get(self):
        j = self.i % len(self.bufs)
        self.i += 1
        return self.bufs[j], (self.name, j)


QA0, KA0, VA0, QB0, KC0, VC0, KS0, VS0, KW0, VW0, GN0, GA0, GB0 = (
    0, 4096, 4608, 5120, 9216, 9728, 10240, 10752, 11264, 11776, 12288, 12384, 16480)
WTOT = 20576


DECLARED = []
DBG_NAMES = ("X1", "Gt", "out")
USED_INPUTS = ("x", "pos", "w_in", "inva", "invb", "ident", "sinks_b", "m_swa", "m_win", "kpeT", "vpeT", "kw1", "vw1", "kw2", "vw2", "m_cmpT", "m_cmp", "sl_vb", "sl_add", "exmat",
               "w_o", "ln1g", "ln1b", "ln2g", "ln2b", "w_r", "rbias", "eg", "eu", "ed")


def build_nc(stages=99, dbg=False):
    del DECLARED[:]
    nc = bass.Bass("TRN2", target_bir_lowering=False)
    S = Sched(nc)

    def din(name, shape, dt=F32):
        return nc.dram_tensor(name, list(shape), dt, kind="ExternalInput").ap()

    DBG_OUT = DBG_NAMES

    def dscr(name, shape, dt=BF16):
        if dbg and name in DBG_OUT:
            return nc.dram_tensor(name, list(shape), dt, kind="ExternalOutput").ap()
        return nc.dram_tensor(name, list(shape), dt).ap()

    _din = din

    def din(name, shape, dt=F32):
        if name not in USED_INPUTS or (name in ("eg", "eu", "ed") and stages < 8):
            return None
        DECLARED.append(name)
        return _din(name, shape, dt)

    x = din("x", [S_, D_])
    pos = din("pos", [128, NT], I32)
    w_in = din("w_in", [D_, WTOT])
    esink_in = din("sinks_b", [128, 64])
    inva = din("inva", [128, 32])
    invb = din("invb", [128, 64])
    ident_in = din("ident", [128, 128])
    kpeT = din("kpeT", [128, 32])
    vpeT = din("vpeT", [128, 32])
    kw1 = din("kw1", [4096, 256])
    vw1 = din("vw1", [4096, 256])
    kw2 = din("kw2", [256, 128])
    vw2 = din("vw2", [256, 128])
    w_o = din("w_o", [D_, D_])
    ln1g = din("ln1g", [128, D_])
    ln1b = din("ln1b", [128, D_])
    ln2g = din("ln2g", [128, D_])
    ln2b = din("ln2b", [128, D_])
    w_r = din("w_r", [D_, 64])
    rbias = din("rbias", [128, 64])
    eg = din("eg", [65, 2, 128, 32 * 256])
    eu = din("eu", [65, 2, 128, 32 * 256])
    ed = din("ed", [65, 2, 128, 2 * D_])
    m_swa = din("m_swa", [5, 128, 512])
    m_win = din("m_win", [8, 128, 512])
    m_cmpT = din("m_cmpT", [2, 128, S_])
    m_cmp = din("m_cmp", [S_, 256])
    sl_vb = din("sl_vb", [S_, 64])
    sl_add = din("sl_add", [S_, 64])
    exmat = din("exmat", [64, 32 * 128])
    out = nc.dram_tensor("out", [S_, D_], F32, kind="ExternalOutput").ap()

    xT_d = dscr("xT_d", [NT, 128, 32 * 128])
    QaT = dscr("QaT", [32, 128, S_])
    KaT2 = dscr("KaT2", [8, 128, S_])
    Va = dscr("Va", [S_, 8 * 65])
    QbT = dscr("QbT", [32, 128, S_])
    kcT = dscr("kcT", [4, 128, S_])
    vcT = dscr("vcT", [4, 128, S_])
    ksT = dscr("ksT", [4, 128, S_])
    kwT = dscr("kwT", [4, 128, S_])
    Vs = dscr("Vs", [S_, 4 * 129])
    Vw = dscr("Vw", [S_, 4 * 129])
    Gn = dscr("Gn", [S_, 96], F32)
    Ga = dscr("Ga", [S_, D_], F32)
    Gb = dscr("Gb", [S_, D_], F32)
    Oa = dscr("Oa", [S_, D_], F32)
    Ocmp = dscr("Ocmp", [S_, D_], F32)
    Oslc = dscr("Oslc", [S_, D_], F32)
    Owin = dscr("Owin", [S_, D_], F32)
    yT_d = dscr("yT_d", [NT, 128, 32 * 128])
    R1 = dscr("R1", [S_, D_], F32)
    X1 = dscr("X1", [S_, D_], F32)
    x1T_d = dscr("x1T_d", [NT, 128, 32 * 128])
    Gt = dscr("Gt", [S_, 65], F32)
    kcmpT_d = dscr("kcmpT_d", [4, 128, S_])
    vcmp_d = dscr("vcmp_d", [S_, 4 * 129])
    selT_d = dscr("selT_d", [4, 64, S_])

    es = contextlib.ExitStack()
    with es:
        es.enter_context(nc.allow_low_precision("bf16 matmul operands, fp32 accumulation"))

        def sb(name, shape, dt):
            return es.enter_context(nc.sbuf_tensor(name, shape, dt))

        def op(eng, fn, r=(), w=(), dma=False):
            return S.op(eng, fn, reads=r, writes=w, dma=dma)

        idf = sb("idf", [128, 128], F32)
        idb = sb("idb", [128, 128], BF16)
        op('sp', lambda e: e.dma_start(out=idf[:], in_=ident_in), w=['idf'], dma=True)
        op('dve', lambda e: e.tensor_copy(out=idb[:], in_=idf[:]), r=['idf'], w=['idb'])

        ps_acc = Ring(es, nc, "psacc", [128, 512], F32, 2, psum=True)
        ps_t = Ring(es, nc, "pst", [128, 8, 128], BF16, 2, psum=True)
        ps_o = Ring(es, nc, "pso", [128, 512], F32, 4, psum=True)

        def transpose_blocks(src_fn, nblk, dst_fn, rkeys, wkey_fn):
            for j0 in range(0, nblk, 8):
                n = min(8, nblk - j0)
                pt, pk = ps_t.get()
                for j in range(n):
                    op('pe', lambda e, j=j, pt=pt, j0=j0: e.transpose(out=pt[:, j, :], in_=src_fn(j0 + j), identity=idb[:]),
                       r=list(rkeys) + ['idb'], w=[pk])
                op('act', lambda e, pt=pt, n=n, j0=j0: e.copy(out=dst_fn(j0, n), in_=pt[:, 0:n, :]), r=[pk], w=[wkey_fn(j0)])

        pes = contextlib.ExitStack()
        pes.__enter__()
        xf_r = Ring(pes, nc, "xf", [128, D_], F32, 2)
        xb_r = Ring(pes, nc, "xb", [128, D_], BF16, 2)
        xt_r = Ring(pes, nc, "xTt", [128, 32, 128], BF16, 2)
        for tt in range(NT):
            xf, kf = xf_r.get()
            xb, kb = xb_r.get()
            xt, kt = xt_r.get()
            op('sp', lambda e, xf=xf, tt=tt: e.dma_start(out=xf[:], in_=x[tt * 128:(tt + 1) * 128, :]), w=[kf], dma=True)
            op('dve', lambda e, xf=xf, xb=xb: e.tensor_copy(out=xb[:], in_=xf[:]), r=[kf], w=[kb])
            transpose_blocks(lambda j, xb=xb: xb[:, j * 128:(j + 1) * 128], 32,
                             lambda j0, n, xt=xt: xt[:, j0:j0 + n, :], [kb], lambda j0, kt=kt: kt)
            op('act', lambda e, xt=xt, tt=tt: e.dma_start(out=xT_d[tt].rearrange("p (k t) -> p k t", k=32), in_=xt[:]),
               r=[kt], w=[('xT_d', tt)], dma=True)

        S.flush()
        pes.__exit__(None, None, None)
        tes = contextlib.ExitStack()
        tes.__enter__()
        sbt = lambda n, shp, dt: tes.enter_context(nc.sbuf_tensor(n, shp, dt))
        cosA = sbt("cosA", [128, NT, 32], F32)
        sinA = sbt("sinA", [128, NT, 32], F32)
        cosB = sbt("cosB", [128, NT, 64], F32)
        sinB = sbt("sinB", [128, NT, 64], F32)
        inv_sb = sbt("inv_sb", [128, 96], F32)
        posi = sbt("posi", [128, NT], I32)
        posf = sbt("posf", [128, NT], F32)
        op('sp', lambda e: e.dma_start(out=inv_sb[:, 0:32], in_=inva), w=['inva'], dma=True)
        op('sp', lambda e: e.dma_start(out=inv_sb[:, 32:96], in_=invb), w=['invb'], dma=True)
        op('sp', lambda e: e.dma_start(out=posi[:], in_=pos), w=['posi'], dma=True)
        op('dve', lambda e: e.tensor_copy(out=posf[:], in_=posi[:]), r=['posi'], w=['posf'])
        ang = sbt("ang", [128, 96], F32)
        kq_i = sbt("kq_i", [128, 96], I32)
        kq = sbt("kq", [128, 96], F32)
        rr = sbt("rr", [128, 96], F32)
        mm = sbt("mm", [128, 96], F32)
        r2 = sbt("r2", [128, 96], F32)

        def wrap(v):
            op('dve', lambda e: e.tensor_single_scalar(out=mm[:], in_=v[:], scalar=math.pi, op=ALU.is_gt), r=['rp'], w=['mm'])
            op('dve', lambda e: e.scalar_tensor_tensor(out=v[:], in0=mm[:], scalar=-TWO_PI, in1=v[:], op0=ALU.mult, op1=ALU.add), r=['mm', 'rp'], w=['rp'])
            op('dve', lambda e: e.tensor_single_scalar(out=mm[:], in_=v[:], scalar=-math.pi, op=ALU.is_lt), r=['rp'], w=['mm'])
            op('dve', lambda e: e.scalar_tensor_tensor(out=v[:], in0=mm[:], scalar=TWO_PI, in1=v[:], op0=ALU.mult, op1=ALU.add), r=['mm', 'rp'], w=['rp'])

        for tt in range(NT):
            op('dve', lambda e, tt=tt: e.tensor_scalar(out=ang[:], in0=inv_sb[:], scalar1=posf[:, tt:tt + 1], scalar2=None, op0=ALU.mult),
               r=['inva', 'invb', 'posf'], w=['rp'])
            op('dve', lambda e: e.tensor_single_scalar(out=kq[:], in_=ang[:], scalar=1.0 / TWO_PI, op=ALU.mult), r=['rp'], w=['kq'])
            op('dve', lambda e: e.tensor_copy(out=kq_i[:], in_=kq[:]), r=['kq'], w=['kqi'])
            op('dve', lambda e: e.tensor_copy(out=kq[:], in_=kq_i[:]), r=['kqi'], w=['kq'])
            op('dve', lambda e: e.scalar_tensor_tensor(out=rr[:], in0=kq[:], scalar=-C1, in1=ang[:], op0=ALU.mult, op1=ALU.add), r=['kq', 'rp'], w=['rp'])
            op('dve', lambda e: e.scalar_tensor_tensor(out=rr[:], in0=kq[:], scalar=-C2, in1=rr[:], op0=ALU.mult, op1=ALU.add), r=['kq', 'rp'], w=['rp'])
            wrap(rr)
            op('act', lambda e, tt=tt: e.activation(out=sinA[:, tt, :], in_=rr[:, 0:32], func=AF.Sin), r=['rp'], w=['sinA'])
            op('act', lambda e, tt=tt: e.activation(out=sinB[:, tt, :], in_=rr[:, 32:96], func=AF.Sin), r=['rp'], w=['sinB'])
            op('dve', lambda e: e.tensor_single_scalar(out=r2[:], in_=rr[:], scalar=math.pi / 2, op=ALU.add), r=['rp'], w=['rp'])
            wrap(r2)
            op('act', lambda e, tt=tt: e.activation(out=cosA[:, tt, :], in_=r2[:, 0:32], func=AF.Sin), r=['rp'], w=['cosA'])
            op('act', lambda e, tt=tt: e.activation(out=cosB[:, tt, :], in_=r2[:, 32:96], func=AF.Sin), r=['rp'], w=['cosB'])

        pes = contextlib.ExitStack()
        pes.__enter__()
        wb_r = Ring(pes, nc, "wb", [128, 32, 512], BF16, 2)
        xl_r = Ring(pes, nc, "xl", [128, 32, 128], BF16, 3)
        ep_f = Ring(pes, nc, "epf", [128, 512], F32, 2)
        ep_b = Ring(pes, nc, "epb", [128, 1024], BF16, 3)
        tr_b = Ring(pes, nc, "trb", [128, 8, 128], BF16, 2)
        t1 = pes.enter_context(nc.sbuf_tensor("t1", [128, 256], F32))
        t2 = pes.enter_context(nc.sbuf_tensor("t2", [128, 256], F32))
        ones_col = pes.enter_context(nc.sbuf_tensor("ones_col", [128, 8], BF16))
        op('dve', lambda e: e.memset(ones_col[:], 1.0), w=['ones_col'])
        w_in_v = w_in.rearrange("(k p) c -> p k c", p=128)

        chunks = []
        for c in range(8):
            chunks.append((QA0 + c * 512, 512, 'ropeA_q', c))
        chunks.append((KA0, 512, 'ropeA_k', 0))
        chunks.append((VA0, 512, 'v_a', 0))
        for c in range(8):
            chunks.append((QB0 + c * 512, 512, 'ropeB_q', c))
        chunks.append((KC0, 512, 'ropeB_k', kcT))
        chunks.append((VC0, 512, 'plainT', vcT))
        chunks.append((KS0, 512, 'ropeB_k', ksT))
        chunks.append((VS0, 512, 'v_b', Vs))
        chunks.append((KW0, 512, 'ropeB_k', kwT))
        chunks.append((VW0, 512, 'v_b', Vw))
        chunks.append((GN0, 96, 'sig', Gn))
        for c in range(8):
            chunks.append((GA0 + c * 512, 512, 'sig', (Ga, c)))
        for c in range(8):
            chunks.append((GB0 + c * 512, 512, 'sig', (Gb, c)))

        def rope(ps, pk, H, hd, cos_t, sin_t, tt, ob, obk):
            v = ps[:, :].rearrange("p (h t d) -> p h t d", h=H, t=2)
            o = ob[:, 0:512].rearrange("p (h t d) -> p h t d", h=H, t=2)
            cb = cos_t[:, tt, :].unsqueeze(1).broadcast_to([128, H, hd])
            sn = sin_t[:, tt, :].unsqueeze(1).broadcast_to([128, H, hd])
            a = t1[:, :].rearrange("p (h d) -> p h d", h=H)
            b = t2[:, :].rearrange("p (h d) -> p h d", h=H)
            tabs = ['cosA', 'sinA', 'cosB', 'sinB']
            op('dve', lambda e: e.tensor_tensor(out=a, in0=v[:, :, 0, :], in1=cb, op=ALU.mult), r=[pk] + tabs, w=['t1'])
            op('dve', lambda e: e.tensor_tensor(out=b, in0=v[:, :, 1, :], in1=sn, op=ALU.mult), r=[pk] + tabs, w=['t2'])
            op('dve', lambda e: e.tensor_tensor(out=o[:, :, 0, :], in0=a, in1=b, op=ALU.subtract), r=['t1', 't2'], w=[obk])
            op('dve', lambda e: e.tensor_tensor(out=a, in0=v[:, :, 1, :], in1=cb, op=ALU.mult), r=[pk] + tabs, w=['t1'])
            op('dve', lambda e: e.tensor_tensor(out=b, in0=v[:, :, 0, :], in1=sn, op=ALU.mult), r=[pk] + tabs, w=['t2'])
            op('dve', lambda e: e.tensor_tensor(out=o[:, :, 1, :], in0=a, in1=b, op=ALU.add), r=['t1', 't2'], w=[obk])

        def to_featmajor(ob, obk, nblk, dst, tt, idx0):
            tb, tk = tr_b.get()
            transpose_blocks(lambda j: ob[:, j * 128:(j + 1) * 128], nblk, lambda j0, n: tb[:, j0:j0 + n, :], [obk], lambda j0: tk)
            op('act', lambda e: e.dma_start(out=dst[idx0:idx0 + nblk, :, tt * 128:(tt + 1) * 128].rearrange("j p t -> p j t"), in_=tb[:, 0:nblk, :]),
               r=[tk], w=[('featT', id(dst), tt)], dma=True)

        for (c0, ncol, kind, arg) in (chunks if stages >= 1 else []):
            wb, wk = wb_r.get()
            for k0 in range(0, 32, 8):
                op('pool', lambda e, wb=wb, k0=k0, c0=c0, ncol=ncol: e.dma_start(out=wb[:, k0:k0 + 8, 0:ncol], in_=w_in_v[:, k0:k0 + 8, c0:c0 + ncol]),
                   w=[(wk, k0)], dma=True)
            for tt in range(NT):
                xl, xk = xl_r.get()
                op('sp', lambda e, xl=xl, tt=tt: e.dma_start(out=xl[:], in_=xT_d[tt].rearrange("p (k t) -> p k t", k=32)),
                   r=[('xT_d', tt)], w=[xk], dma=True)
                ps, pk = ps_acc.get()
                for k in range(32):
                    op('pe', lambda e, ps=ps, xl=xl, wb=wb, k=k, ncol=ncol: e.matmul(ps[:, 0:ncol], lhsT=xl[:, k, :], rhs=wb[:, k, 0:ncol], start=(k == 0), stop=(k == 31)),
                       r=[xk, (wk, (k // 8) * 8)], w=[pk])
                tsl = slice(tt * 128, (tt + 1) * 128)
                if kind == 'sig':
                    of, ofk = ep_f.get()
                    op('act', lambda e, of=of, ps=ps, ncol=ncol: e.activation(out=of[:, 0:ncol], in_=ps[:, 0:ncol], func=AF.Sigmoid), r=[pk], w=[ofk])
                    if ncol == 96:
                        op('act', lambda e, of=of, tsl=tsl: e.dma_start(out=Gn[tsl, :], in_=of[:, 0:96]), r=[ofk], w=[('Gn', tt)], dma=True)
                    else:
                        dst, cc = arg
                        op('act', lambda e, of=of, tsl=tsl, dst=dst, cc=cc: e.dma_start(out=dst[tsl, cc * 512:(cc + 1) * 512], in_=of[:]), r=[ofk], w=[('G', id(dst), tt, cc)], dma=True)
                elif kind == 'v_a':
                    ob, obk = ep_b.get()
                    o3 = ob[:, 0:520].rearrange("p (g d) -> p g d", g=8)
                    op('act', lambda e, o3=o3, ps=ps: e.copy(out=o3[:, :, 0:64], in_=ps[:, :].rearrange("p (g d) -> p g d", g=8)), r=[pk], w=[obk])
                    op('dve', lambda e, o3=o3: e.tensor_copy(out=o3[:, :, 64:65], in_=ones_col[:, 0:8].unsqueeze(2)), r=['ones_col'], w=[obk])
                    op('act', lambda e, ob=ob, tsl=tsl: e.dma_start(out=Va[tsl, :], in_=ob[:, 0:520]), r=[obk], w=[('Va', tt)], dma=True)
                elif kind == 'v_b':
                    ob, obk = ep_b.get()
                    o3 = ob[:, 0:516].rearrange("p (g d) -> p g d", g=4)
                    op('act', lambda e, o3=o3, ps=ps: e.copy(out=o3[:, :, 0:128], in_=ps[:, :].rearrange("p (g d) -> p g d", g=4)), r=[pk], w=[obk])
                    op('dve', lambda e, o3=o3: e.tensor_copy(out=o3[:, :, 128:129], in_=ones_col[:, 0:4].unsqueeze(2)), r=['ones_col'], w=[obk])
                    op('act', lambda e, ob=ob, tsl=tsl, arg=arg: e.dma_start(out=arg[tsl, :], in_=ob[:, 0:516]), r=[obk], w=[('Vb', id(arg), tt)], dma=True)
                elif kind == 'plainT':
                    ob, obk = ep_b.get()
                    op('act', lambda e, ob=ob, ps=ps: e.copy(out=ob[:, 0:512], in_=ps[:, :]), r=[pk], w=[obk])
                    to_featmajor(ob, obk, 4, arg, tt, 0)
                elif kind == 'ropeB_k':
                    ob, obk = ep_b.get()
                    rope(ps, pk, 4, 64, cosB, sinB, tt, ob, obk)
                    to_featmajor(ob, obk, 4, arg, tt, 0)
                elif kind == 'ropeB_q':
                    ob, obk = ep_b.get()
                    rope(ps, pk, 4, 64, cosB, sinB, tt, ob, obk)
                    to_featmajor(ob, obk, 4, QbT, tt, arg * 4)
                elif kind == 'ropeA_q':
                    ob, obk = ep_b.get()
                    rope(ps, pk, 8, 32, cosA, sinA, tt, ob, obk)
                    to_featmajor(ob, obk, 4, QaT, tt, arg * 4)
                elif kind == 'ropeA_k':
                    ob, obk = ep_b.get()
                    rope(ps, pk, 8, 32, cosA, sinA, tt, ob, obk)
                    ob2, ob2k = ep_b.get()
                    src = ob[:, 0:512].rearrange("p (g d) -> p g d", g=8)
                    d4 = ob2[:, 0:1024].rearrange("p (g t d) -> p g t d", g=8, t=2)
                    op('dve', lambda e, d4=d4, src=src: e.tensor_copy(out=d4[:, :, 0, :], in_=src), r=[obk], w=[ob2k])
                    op('dve', lambda e, d4=d4, src=src: e.tensor_copy(out=d4[:, :, 1, :], in_=src), r=[obk], w=[ob2k])
                    to_featmajor(ob2, ob2k, 8, KaT2, tt, 0)

        S.flush()
        pes.__exit__(None, None, None)
        tes.__exit__(None, None, None)
        def attention_phase(tag, nheads, hpg, dh, dv, QT_src, KT_src, V_src, vstride, plan, Odst, mask_setup, sink_tab=None):
            pes = contextlib.ExitStack()
            pes.__enter__()
            scale = float(dh) ** -0.5
            pre_g, pre_gc, mask_fn = mask_setup(pes)
            kt_r = Ring(pes, nc, tag + "K", [128, S_], BF16, 2)
            v_r = Ring(pes, nc, tag + "V", [128, NT, dv + 1], BF16, 2)
            nq = hpg if dh == 128 else hpg // 2
            q_r = Ring(pes, nc, tag + "Q", [128, S_], BF16, nq + 1)
            e_r = Ring(pes, nc, tag + "E", [128, 512], BF16, 4)
            os_r = Ring(pes, nc, tag + "O", [128, 2, dv], F32, 3)
            rec_r = Ring(pes, nc, tag + "R", [128, 2], F32, 3)
            G = nheads // hpg
            cnt = [0]
            for g in range(G):
                ksb, kk = kt_r.get()
                vsb, vk = v_r.get()
                op('sp', lambda e, ksb=ksb, g=g: e.dma_start(out=ksb[:], in_=KT_src(g)), w=[kk], dma=True)
                op('sp', lambda e, vsb=vsb, g=g: e.dma_start(out=vsb[:], in_=V_src[:, g * vstride:(g + 1) * vstride].rearrange("(kt p) d -> p kt d", p=128)),
                   w=[vk], dma=True)
                qtiles = {}
                for hh in range(hpg):
                    qsrc, pbase, qid = QT_src(g * hpg + hh)
                    if qid not in qtiles:
                        qsb, qk = q_r.get()
                        op('sp', lambda e, qsb=qsb, qsrc=qsrc: e.dma_start(out=qsb[:], in_=qsrc), w=[qk], dma=True)
                        qtiles[qid] = (qsb, qk)
                if pre_g is not None:
                    pre_g(g)
                for c in range(8):
                    kts = plan(c)
                    if pre_gc is not None:
                        pre_gc(g, c)
                    for hh in range(hpg):
                        h = g * hpg + hh
                        qsrc, pbase, qid = QT_src(h)
                        qsb, qk = qtiles[qid]
                        pr = slice(pbase, pbase + dh)
                        ob = [ps_o.get(), ps_o.get()]
                        first = [True, True]
                        def emit_pv(eb, ek, kt, ki):
                            for qi in range(4):
                                (ot, otk) = ob[qi // 2]
                                j = qi % 2
                                stf = first[qi // 2]
                                first[qi // 2] = False
                                op('pe', lambda e, ot=ot, eb=eb, vsb=vsb, kt=kt, qi=qi, j=j, stf=stf, last=(ki == len(kts) - 1):
                                   e.matmul(ot[:, j * (dv + 1):(j + 1) * (dv + 1)], lhsT=eb[:, qi * 128:(qi + 1) * 128], rhs=vsb[:, kt, :], start=stf, stop=last),
                                   r=[ek, vk], w=[otk])
                        pend = None
                        for ki, kt in enumerate(kts):
                            st, stk = ps_acc.get()
                            op('pe', lambda e, st=st, ksb=ksb, qsb=qsb, pr=pr, kt=kt, c=c: e.matmul(st[:, :], lhsT=ksb[pr, kt * 128:(kt + 1) * 128], rhs=qsb[pr, c * 512:(c + 1) * 512], start=True, stop=True),
                               r=[kk, qk], w=[stk])
                            eb, ek = e_r.get()
                            op('act', lambda e, eb=eb, st=st: e.activation(out=eb[:], in_=st[:, :], func=AF.Exp, scale=scale), r=[stk], w=[ek])
                            mk = mask_fn(g, c, kt)
                            if mk is not None:
                                cnt[0] += 1
                                eng = 'dve' if cnt[0] % 2 == 0 else 'pool'
                                op(eng, lambda e, eb=eb, mk=mk: e.tensor_tensor(out=eb[:], in0=eb[:], in1=mk[0], op=ALU.mult), r=[ek, mk[1]], w=[ek])
                            if pend is not None:
                                emit_pv(*pend)
                            pend = (eb, ek, kt, ki)
                        emit_pv(*pend)
                        for b2 in range(2):
                            ot, otk = ob[b2]
                            o3 = ot[:, 0:2 * (dv + 1)].rearrange("p (j d) -> p j d", j=2)
                            rec, rk = rec_r.get()
                            osb, osk = os_r.get()
                            s1 = sink_tab[:, h:h + 1] if sink_tab is not None else 0.0
                            op('dve', lambda e, rec=rec, o3=o3, s1=s1: e.tensor_scalar(out=rec[:].unsqueeze(2), in0=o3[:, :, dv:dv + 1], scalar1=s1, scalar2=1e-30, op0=ALU.add, op1=ALU.max),
                               r=[otk, 'esink'], w=[rk])
                            op('dve', lambda e, rec=rec: e.reciprocal(out=rec[:], in_=rec[:]), r=[rk], w=[rk])
                            op('dve', lambda e, osb=osb, o3=o3, rec=rec: e.tensor_tensor(out=osb[:], in0=o3[:, :, 0:dv], in1=rec[:].unsqueeze(2).broadcast_to([128, 2, dv]), op=ALU.mult),
                               r=[otk, rk], w=[osk])
                            t0 = c * 512 + b2 * 256
                            op('act', lambda e, osb=osb, t0=t0, h=h: e.dma_start(out=Odst[t0:t0 + 256, h * dv:(h + 1) * dv].rearrange("(j p) d -> p j d", p=128), in_=osb[:]),
                               r=[osk], w=[('O', tag, h, c, b2)], dma=True)
            S.flush()
            pes.__exit__(None, None, None)

        def static_masks(tag, masks_in, idx_fn):
            def setup(pes):
                n = len(masks_in)
                msk = pes.enter_context(nc.sbuf_tensor(tag + "msk", [128, n, 512], BF16))
                for r in range(n):
                    op('pool', lambda e, r=r: e.dma_start(out=msk[:, r, :], in_=masks_in[r]), w=[(tag, 'msk')], dma=True)
                return None, None, (lambda g, c, kt: (msk[:, idx_fn(c, kt), :], (tag, 'msk')))
            return setup

        if stages >= 2:
            esink = sb("esink", [128, 64], F32)
            op('sp', lambda e: e.dma_start(out=esink[:], in_=esink_in), w=['esink'], dma=True)
            op('act', lambda e: e.activation(out=esink[:], in_=esink[:], func=AF.Exp), r=['esink'], w=['esink'])
            attention_phase("swa", 64, 8, 64, 64,
                            lambda h: (QaT[h // 2], 64 * (h % 2), h // 2), lambda g: KaT2[g], Va, 65,
                            lambda c: [kt for kt in range(4 * c - 1, 4 * c + 4) if kt >= 0], Oa,
                            static_masks("swa", [m_swa[r] for r in range(5)], lambda c, kt: kt - 4 * c + 1), sink_tab=esink)
        if stages >= 3:
            attention_phase("win", 32, 8, 128, 128,
                            lambda h: (QbT[h], 0, h), lambda g: kwT[g], Vw, 129,
                            lambda c: [kt for kt in range(4 * c - 4, 4 * c + 4) if kt >= 0], Owin,
                            static_masks("win", [m_win[r] for r in range(8)], lambda c, kt: kt - 4 * c + 4))

        if stages >= 4:
            pes = contextlib.ExitStack()
            pes.__enter__()
            A_ = lambda n, shp, dt: pes.enter_context(nc.sbuf_tensor(n, shp, dt))
            w1k = A_("w1k", [128, 32, 256], BF16)
            w1v = A_("w1v", [128, 32, 256], BF16)
            w2k = A_("w2k", [128, 2, 128], BF16)
            w2v = A_("w2v", [128, 2, 128], BF16)
            pek = A_("pek", [128, 32], F32)
            pev = A_("pev", [128, 32], F32)
            op('pool', lambda e: e.dma_start(out=w1k[:], in_=kw1.rearrange("(l d) h -> d l h", d=128)), w=['w1k'], dma=True)
            op('pool', lambda e: e.dma_start(out=w1v[:], in_=vw1.rearrange("(l d) h -> d l h", d=128)), w=['w1v'], dma=True)
            op('pool', lambda e: e.dma_start(out=w2k[:], in_=kw2.rearrange("(hh p) d -> p hh d", p=128)), w=['w2k'], dma=True)
            op('pool', lambda e: e.dma_start(out=w2v[:], in_=vw2.rearrange("(hh p) d -> p hh d", p=128)), w=['w2v'], dma=True)
            op('sp', lambda e: e.dma_start(out=pek[:], in_=kpeT), w=['pek'], dma=True)
            op('sp', lambda e: e.dma_start(out=pev[:], in_=vpeT), w=['pev'], dma=True)
            kcmp_sb = A_("kcmp_sb", [128, 4, 256], BF16)
            vcmp_sb = A_("vcmp_sb", [128, 2, 4 * 129], BF16)
            op('dve', lambda e: e.memset(kcmp_sb[:], 0.0), w=['kcmp_sb'])
            op('dve', lambda e: e.memset(vcmp_sb[:], 0.0), w=['vcmp_sb'])
            src_r = Ring(pes, nc, "csrc", [128, S_], BF16, 2)
            tmp_r = Ring(pes, nc, "ctmp", [128, 256], BF16, 4)
            xs = A_("cxs", [128, 256], F32)
            g1 = A_("cg1", [128, 256], F32)
            g2 = A_("cg2", [128, 256], F32)
            hg = A_("chg", [128, 2, 256], BF16)
            op('dve', lambda e: e.memset(hg[:], 0.0), w=['chg'])
            for which in ('k', 'v'):
                srcT, w1, w2, pe_, = (kcT, w1k, w2k, pek) if which == 'k' else (vcT, w1v, w2v, pev)
                for g in range(4):
                    sT, sk = src_r.get()
                    op('sp', lambda e, sT=sT, g=g, srcT=srcT: e.dma_start(out=sT[:], in_=srcT[g]), w=[sk], dma=True)
                    pa = [ps_acc.get(), ps_acc.get()]
                    for l in range(32):
                        tm, tk = tmp_r.get()
                        op('dve', lambda e, tm=tm, sT=sT, l=l, pe_=pe_: e.tensor_scalar(out=tm[:, 0:255], in0=sT[:, l:l + 16 * 254 + 1:16], scalar1=pe_[:, l:l + 1], scalar2=None, op0=ALU.add),
                           r=[sk, 'pek', 'pev'], w=[tk])
                        for hh in range(2):
                            op('pe', lambda e, hh=hh, tm=tm, l=l, w1=w1, pa=pa: e.matmul(pa[hh][0][:, 0:255], lhsT=w1[:, l, hh * 128:(hh + 1) * 128], rhs=tm[:, 0:255], start=(l == 0), stop=(l == 31)),
                               r=[tk, 'w1k', 'w1v'], w=[pa[hh][1]])
                    for hh in range(2):
                        pt_, pk_ = pa[hh]
                        op('act', lambda e, pt_=pt_: e.copy(out=xs[:, 0:255], in_=pt_[:, 0:255]), r=[pk_], w=['cxs'])
                        op('dve', lambda e: e.tensor_tensor(out=g1[:, 0:255], in0=xs[:, 0:255], in1=xs[:, 0:255], op=ALU.mult), r=['cxs'], w=['cg1'])
                        op('dve', lambda e: e.tensor_scalar(out=g1[:, 0:255], in0=g1[:, 0:255], scalar1=0.044715, scalar2=1.0, op0=ALU.mult, op1=ALU.add), r=['cg1'], w=['cg1'])
                        op('dve', lambda e: e.tensor_tensor(out=g1[:, 0:255], in0=g1[:, 0:255], in1=xs[:, 0:255], op=ALU.mult), r=['cg1', 'cxs'], w=['cg1'])
                        op('act', lambda e: e.activation(out=g2[:, 0:255], in_=g1[:, 0:255], func=AF.Sigmoid, scale=1.5957691216057308), r=['cg1'], w=['cg2'])
                        op('dve', lambda e, hh=hh: e.tensor_tensor(out=hg[:, hh, 0:255], in0=xs[:, 0:255], in1=g2[:, 0:255], op=ALU.mult), r=['cxs', 'cg2'], w=['chg'])
                    if which == 'k':
                        pt_, pk_ = ps_acc.get()
                        for hh in range(2):
                            op('pe', lambda e, hh=hh, pt_=pt_: e.matmul(pt_[:, 0:256], lhsT=w2k[:, hh, :], rhs=hg[:, hh, :], start=(hh == 0), stop=(hh == 1)), r=['chg', 'w2k'], w=[pk_])
                        op('act', lambda e, pt_=pt_, g=g: e.copy(out=kcmp_sb[:, g, 0:255], in_=pt_[:, 0:255]), r=[pk_], w=['kcmp_sb'])
                    else:
                        for bt in range(2):
                            pt_, pk_ = ps_acc.get()
                            for hh in range(2):
                                op('pe', lambda e, hh=hh, pt_=pt_, bt=bt: e.matmul(pt_[:, 0:128], lhsT=hg[:, hh, bt * 128:(bt + 1) * 128], rhs=w2v[:, hh, :], start=(hh == 0), stop=(hh == 1)), r=['chg', 'w2v'], w=[pk_])
                            op('act', lambda e, pt_=pt_, g=g, bt=bt: e.copy(out=vcmp_sb[:, bt, g * 129:g * 129 + 128], in_=pt_[:, 0:128]), r=[pk_], w=['vcmp_sb'])
                            op('dve', lambda e, g=g, bt=bt: e.memset(vcmp_sb[:, bt, g * 129 + 128:g * 129 + 129], 1.0), r=[], w=['vcmp_sb'])
            for g in range(4):
                op('act', lambda e, g=g: e.dma_start(out=kcmpT_d[g][:, 0:256], in_=kcmp_sb[:, g, :]), r=['kcmp_sb'], w=[('kcmpT_d', g)], dma=True)
            op('act', lambda e: e.dma_start(out=vcmp_d[0:256, :].rearrange("(bt p) d -> p bt d", p=128), in_=vcmp_sb[:]), r=['vcmp_sb'], w=['vcmp_d'], dma=True)

            qg_r = Ring(pes, nc, "pq", [128, S_], BF16, 9)
            mc_r = Ring(pes, nc, "pmc", [128, 256], F32, 2)
            vb_r = Ring(pes, nc, "pvb", [128, 64], F32, 2)
            ad_r = Ring(pes, nc, "pad", [128, 64], F32, 2)
            ee = A_("pee", [128, 256], F32)
            den = A_("pden", [128, 1], F32)
            pg = A_("ppg", [128, 256], F32)
            s3 = A_("ps3", [128, 64], F32)
            psl = A_("ppsl", [128, 64], F32)
            sc = A_("psc", [128, 64], F32)
            sc2 = A_("psc2", [128, 64], F32)
            m8 = A_("pm8", [128, 8], F32)
            selb = A_("pselb", [128, 64], BF16)
            selT_sb = A_("pselT", [64, S_], BF16)
            for g in range(4):
                qs = []
                for hh in range(8):
                    qsb, qk = qg_r.get()
                    op('sp', lambda e, qsb=qsb, h=g * 8 + hh: e.dma_start(out=qsb[:], in_=QbT[h]), w=[qk], dma=True)
                    qs.append((qsb, qk))
                for tt in range(NT):
                    mc, mck = mc_r.get()
                    vb, vbk = vb_r.get()
                    ad, adk = ad_r.get()
                    tsl = slice(tt * 128, (tt + 1) * 128)
                    op('sp', lambda e, mc=mc, tsl=tsl: e.dma_start(out=mc[:], in_=m_cmp[tsl, :]), w=[mck], dma=True)
                    op('sp', lambda e, vb=vb, tsl=tsl: e.dma_start(out=vb[:], in_=sl_vb[tsl, :]), w=[vbk], dma=True)
                    op('sp', lambda e, ad=ad, tsl=tsl: e.dma_start(out=ad[:], in_=sl_add[tsl, :]), w=[adk], dma=True)
                    for hh in range(8):
                        qsb, qk = qs[hh]
                        pt_, pk_ = ps_acc.get()
                        op('pe', lambda e, pt_=pt_, qsb=qsb, tsl=tsl, g=g: e.matmul(pt_[:, 0:256], lhsT=qsb[:, tsl], rhs=kcmp_sb[:, g, :], start=True, stop=True), r=[qk, 'kcmp_sb'], w=[pk_])
                        op('act', lambda e, pt_=pt_: e.activation(out=ee[:], in_=pt_[:, 0:256], func=AF.Exp, scale=128.0 ** -0.5), r=[pk_], w=['pee'])
                        op('dve', lambda e, mc=mc: e.tensor_tensor(out=ee[:], in0=ee[:], in1=mc[:], op=ALU.mult), r=['pee', mck], w=['pee'])
                        op('dve', lambda e: e.reduce_sum(out=den[:], in_=ee[:], axis=AX.X), r=['pee'], w=['pden'])
                        op('dve', lambda e: e.tensor_scalar(out=den[:], in0=den[:], scalar1=1e-30, scalar2=None, op0=ALU.max), r=['pden'], w=['pden'])
                        op('dve', lambda e: e.reciprocal(out=den[:], in_=den[:]), r=['pden'], w=['pden'])
                        if hh == 0:
                            op('dve', lambda e: e.tensor_scalar(out=pg[:], in0=ee[:], scalar1=den[:, 0:1], scalar2=None, op0=ALU.mult), r=['pee', 'pden'], w=['ppg'])
                        else:
                            op('dve', lambda e: e.scalar_tensor_tensor(out=pg[:], in0=ee[:], scalar=den[:, 0:1], in1=pg[:], op0=ALU.mult, op1=ALU.add), r=['pee', 'pden', 'ppg'], w=['ppg'])
                    pgv = pg[:, :].rearrange("p (j m) -> p j m", m=4)
                    op('dve', lambda e, pgv=pgv: e.tensor_tensor(out=s3[:], in0=pgv[:, :, 0], in1=pgv[:, :, 1], op=ALU.add), r=['ppg'], w=['ps3'])
                    op('dve', lambda e, pgv=pgv: e.tensor_tensor(out=s3[:], in0=s3[:], in1=pgv[:, :, 2], op=ALU.add), r=['ppg', 'ps3'], w=['ps3'])
                    op('dve', lambda e, pgv=pgv: e.scalar_tensor_tensor(out=psl[:], in0=s3[:], scalar=2.0, in1=pgv[:, :, 3], op0=ALU.mult, op1=ALU.add), r=['ppg', 'ps3'], w=['ppsl'])
                    op('dve', lambda e, pgv=pgv: e.tensor_tensor(out=psl[:, 1:64], in0=psl[:, 1:64], in1=pgv[:, 0:63, 3], op=ALU.add), r=['ppg', 'ppsl'], w=['ppsl'])
                    op('dve', lambda e, vb=vb: e.tensor_tensor(out=sc[:], in0=psl[:], in1=vb[:], op=ALU.mult), r=['ppsl', vbk], w=['psc'])
                    op('dve', lambda e, ad=ad: e.tensor_tensor(out=sc[:], in0=sc[:], in1=ad[:], op=ALU.add), r=['psc', adk], w=['psc'])
                    op('dve', lambda e: e.max(out=m8[:], in_=sc[:]), r=['psc'], w=['pm8'])
                    op('dve', lambda e: e.match_replace(out=sc2[:], in_to_replace=m8[:], in_values=sc[:], imm_value=-1e30), r=['psc', 'pm8'], w=['psc2'])
                    op('dve', lambda e: e.max(out=m8[:], in_=sc2[:]), r=['psc2'], w=['pm8'])
                    op('dve', lambda e: e.tensor_scalar(out=sc2[:], in0=sc[:], scalar1=m8[:, 7:8], scalar2=None, op0=ALU.is_ge), r=['psc', 'pm8'], w=['psc2'])
                    op('dve', lambda e, vb=vb: e.tensor_tensor(out=selb[:], in0=sc2[:], in1=vb[:], op=ALU.mult), r=['psc2', vbk], w=['pselb'])
                    ptt, ptk = ps_t.get()
                    op('pe', lambda e, ptt=ptt: e.transpose(out=ptt[0:64, 0, :], in_=selb[:, 0:64], identity=idb[:]), r=['pselb', 'idb'], w=[ptk])
                    op('act', lambda e, ptt=ptt, tsl=tsl: e.copy(out=selT_sb[:, tsl], in_=ptt[0:64, 0, :]), r=[ptk], w=['pselT'])
                op('act', lambda e, g=g: e.dma_start(out=selT_d[g], in_=selT_sb[:]), r=['pselT'], w=[('selT_d', g)], dma=True)
            S.flush()
            pes.__exit__(None, None, None)

            def cmp_masks(pes):
                msk = pes.enter_context(nc.sbuf_tensor("cmpmsk", [128, 2, S_], BF16))
                for bt in range(2):
                    op('pool', lambda e, bt=bt: e.dma_start(out=msk[:, bt, :], in_=m_cmpT[bt]), w=[('cmp', 'msk')], dma=True)
                return None, None, (lambda g, c, kt: (msk[:, kt, c * 512:(c + 1) * 512], ('cmp', 'msk')))
            attention_phase("cmp", 32, 8, 128, 128, lambda h: (QbT[h], 0, h), lambda g: kcmpT_d[g], vcmp_d, 129,
                            lambda c: [0, 1], Ocmp, cmp_masks)

        if stages >= 5:
            def slc_masks(pes):
                exm = pes.enter_context(nc.sbuf_tensor("slcex", [64, 32, 128], BF16))
                mwin = pes.enter_context(nc.sbuf_tensor("slcmw", [128, 4, 512], BF16))
                mk_all = pes.enter_context(nc.sbuf_tensor("slcmk", [128, 32, 512], BF16))
                selr = Ring(pes, nc, "slcsel", [64, S_], BF16, 2)
                op('pool', lambda e: e.dma_start(out=exm[:], in_=exmat.rearrange("b (k q) -> b k q", k=32)), w=['slcex'], dma=True)
                for r in range(4):
                    op('pool', lambda e, r=r: e.dma_start(out=mwin[:, r, :], in_=m_win[4 + r]), w=['slcmw'], dma=True)
                cur = {}

                def pre_g(g):
                    sb_, sk_ = selr.get()
                    op('sp', lambda e, sb_=sb_, g=g: e.dma_start(out=sb_[:], in_=selT_d[g]), w=[sk_], dma=True)
                    cur['sel'] = (sb_, sk_)

                def pre_gc(g, c):
                    sb_, sk_ = cur['sel']
                    for kt in range(4 * c + 4):
                        pt_, pk_ = ps_acc.get()
                        op('pe', lambda e, pt_=pt_, kt=kt, sb_=sb_, c=c: e.matmul(pt_[:, :], lhsT=exm[:, kt, :], rhs=sb_[:, c * 512:(c + 1) * 512], start=True, stop=True),
                           r=['slcex', sk_], w=[pk_])
                        if kt >= 4 * c:
                            op('dve', lambda e, pt_=pt_, kt=kt, c=c: e.tensor_tensor(out=mk_all[:, kt, :], in0=pt_[:, :], in1=mwin[:, kt - 4 * c, :], op=ALU.mult),
                               r=[pk_, 'slcmw'], w=[('slcmk', kt)])
                        else:
                            op('act', lambda e, pt_=pt_, kt=kt: e.copy(out=mk_all[:, kt, :], in_=pt_[:, :]), r=[pk_], w=[('slcmk', kt)])
                return pre_g, pre_gc, (lambda g, c, kt: (mk_all[:, kt, :], ('slcmk', kt)))
            attention_phase("slc", 32, 8, 128, 128, lambda h: (QbT[h], 0, h), lambda g: ksT[g], Vs, 129,
                            lambda c: list(range(4 * c + 4)), Oslc, slc_masks)

        fin = []
        if stages >= 6:
            pes = contextlib.ExitStack()
            pes.__enter__()
            HW = 2048
            r_oa = Ring(pes, nc, "m_oa", [128, HW], F32, 2)
            r_oc = Ring(pes, nc, "m_oc", [128, HW], F32, 2)
            r_os = Ring(pes, nc, "m_os", [128, HW], F32, 2)
            r_ow = Ring(pes, nc, "m_ow", [128, HW], F32, 2)
            r_ga = Ring(pes, nc, "m_ga", [128, HW], F32, 2)
            r_gb = Ring(pes, nc, "m_gb", [128, HW], F32, 2)
            r_gn = Ring(pes, nc, "m_gn", [128, 96], F32, 2)
            r_acc = Ring(pes, nc, "m_acc", [128, HW], F32, 2)
            r_tmp = Ring(pes, nc, "m_tmp", [128, HW], F32, 4)
            r_y = Ring(pes, nc, "m_y", [128, D_], BF16, 2)
            r_yT = Ring(pes, nc, "m_yT", [128, 32, 128], BF16, 2)
            for tt in range(NT):
                tsl = slice(tt * 128, (tt + 1) * 128)
                gn, gnk = r_gn.get()
                op('sp', lambda e, gn=gn, tsl=tsl: e.dma_start(out=gn[:], in_=Gn[tsl, :]), w=[gnk], dma=True)
                yb, ybk = r_y.get()
                for hf in range(2):
                    cs = slice(hf * HW, (hf + 1) * HW)
                    tiles = []
                    for ring, src in ((r_oa, Oa), (r_oc, Ocmp), (r_os, Oslc), (r_ow, Owin), (r_ga, Ga), (r_gb, Gb)):
                        t_, k_ = ring.get()
                        op('sp', lambda e, t_=t_, src=src, tsl=tsl, cs=cs: e.dma_start(out=t_[:], in_=src[tsl, cs]), w=[k_], dma=True)
                        tiles.append((t_, k_))
                    (oa, oak), (oc, ock), (os_, osk), (ow, owk), (ga, gak), (gb_, gbk) = tiles
                    acc, ack = r_acc.get()
                    tmp, tmk = r_tmp.get()
                    v3 = lambda t_: t_[:, :].rearrange("p (h d) -> p h d", h=16)
                    gsl_ = [gn[:, j * 32 + hf * 16: j * 32 + hf * 16 + 16].unsqueeze(2).broadcast_to([128, 16, 128]) for j in range(3)]
                    gsl = lambda j, gsl_=gsl_: gsl_[j]
                    tmp2, tm2k = r_tmp.get()
                    op('dve', lambda e, acc=acc, oc=oc, g0=gsl_[0]: e.tensor_tensor(out=v3(acc), in0=v3(oc), in1=g0, op=ALU.mult), r=[ock, gnk], w=[ack])
                    op('dve', lambda e, tmp=tmp, os_=os_, g1_=gsl_[1]: e.tensor_tensor(out=v3(tmp), in0=v3(os_), in1=g1_, op=ALU.mult), r=[osk, gnk], w=[tmk])
                    op('pool', lambda e, acc=acc, tmp=tmp: e.tensor_tensor(out=acc[:], in0=acc[:], in1=tmp[:], op=ALU.add), r=[ack, tmk], w=[ack])
                    op('dve', lambda e, tmp2=tmp2, ow=ow, g2_=gsl_[2]: e.tensor_tensor(out=v3(tmp2), in0=v3(ow), in1=g2_, op=ALU.mult), r=[owk, gnk], w=[tm2k])
                    op('pool', lambda e, acc=acc, tmp2=tmp2: e.tensor_tensor(out=acc[:], in0=acc[:], in1=tmp2[:], op=ALU.add), r=[ack, tm2k], w=[ack])
                    op('pool', lambda e, acc=acc, gb_=gb_: e.tensor_tensor(out=acc[:], in0=acc[:], in1=gb_[:], op=ALU.mult), r=[ack, gbk], w=[ack])
                    op('pool', lambda e, tmp=tmp, oa=oa, ga=ga: e.tensor_tensor(out=tmp[:], in0=oa[:], in1=ga[:], op=ALU.mult), r=[oak, gak], w=[tmk])
                    op('dve', lambda e, acc=acc, tmp=tmp, yb=yb, cs=cs: e.tensor_tensor(out=yb[:, cs], in0=acc[:], in1=tmp[:], op=ALU.add), r=[ack, tmk], w=[ybk])
                yT, yTk = r_yT.get()
                transpose_blocks(lambda j, yb=yb: yb[:, j * 128:(j + 1) * 128], 32, lambda j0, n, yT=yT: yT[:, j0:j0 + n, :], [ybk], lambda j0, yTk=yTk: yTk)
                op('act', lambda e, yT=yT, tt=tt: e.dma_start(out=yT_d[tt].rearrange("p (k t) -> p k t", k=32), in_=yT[:]), r=[yTk], w=[('yT_d', tt)], dma=True)
            S.flush()
            pes.__exit__(None, None, None)

            pes = contextlib.ExitStack()
            pes.__enter__()
            wb_r = Ring(pes, nc, "wob", [128, 32, 512], BF16, 2)
            xl_r = Ring(pes, nc, "oyl", [128, 32, 128], BF16, 3)
            xs_r = Ring(pes, nc, "oxs", [128, 512], F32, 3)
            rs_r = Ring(pes, nc, "ors", [128, 512], F32, 3)
            w_o_v = w_o.rearrange("(k p) c -> p k c", p=128)
            for cc in range(8):
                wb, wk = wb_r.get()
                for k0 in range(0, 32, 8):
                    op('pool', lambda e, wb=wb, k0=k0, cc=cc: e.dma_start(out=wb[:, k0:k0 + 8, :], in_=w_o_v[:, k0:k0 + 8, cc * 512:(cc + 1) * 512]), w=[(wk, k0)], dma=True)
                for tt in range(NT):
                    tsl = slice(tt * 128, (tt + 1) * 128)
                    xl, xk = xl_r.get()
                    op('sp', lambda e, xl=xl, tt=tt: e.dma_start(out=xl[:], in_=yT_d[tt].rearrange("p (k t) -> p k t", k=32)), w=[xk], dma=True)
                    xs, xsk = xs_r.get()
                    op('sp', lambda e, xs=xs, tsl=tsl, cc=cc: e.dma_start(out=xs[:], in_=x[tsl, cc * 512:(cc + 1) * 512]), w=[xsk], dma=True)
                    ps, pk = ps_acc.get()
                    for k in range(32):
                        op('pe', lambda e, ps=ps, xl=xl, wb=wb, k=k: e.matmul(ps[:, :], lhsT=xl[:, k, :], rhs=wb[:, k, :], start=(k == 0), stop=(k == 31)),
                           r=[xk, (wk, (k // 8) * 8)], w=[pk])
                    rs, rsk = rs_r.get()
                    op('dve', lambda e, rs=rs, xs=xs, ps=ps: e.scalar_tensor_tensor(out=rs[:], in0=xs[:], scalar=ALPHA, in1=ps[:, :], op0=ALU.mult, op1=ALU.add), r=[xsk, pk], w=[rsk])
                    op('act', lambda e, rs=rs, tsl=tsl, cc=cc: e.dma_start(out=R1[tsl, cc * 512:(cc + 1) * 512], in_=rs[:]), r=[rsk], w=[('R1', tt, cc)], dma=True)
            S.flush()
            pes.__exit__(None, None, None)

        def layer_norm_tile(rt, rtk, gsrc, bsrc, g_r, b_r, st, mv, rsd, tagk):
            for c8 in range(8):
                op('dve', lambda e, c8=c8: e.bn_stats(out=st[:, c8, :], in_=rt[:, c8 * 512:(c8 + 1) * 512]), r=[rtk], w=[tagk + 'st'])
            op('dve', lambda e: e.bn_aggr(out=mv[:], in_=st[:].rearrange("p a b -> p (a b)")), r=[tagk + 'st'], w=[tagk + 'mv'])
            op('dve', lambda e: e.tensor_scalar(out=rsd[:], in0=mv[:, 1:2], scalar1=EPS, scalar2=None, op0=ALU.add), r=[tagk + 'mv'], w=[tagk + 'rs'])
            op('act', lambda e: e.activation(out=rsd[:], in_=rsd[:], func=AF.Sqrt), r=[tagk + 'rs'], w=[tagk + 'rs'])
            op('dve', lambda e: e.reciprocal(out=rsd[:], in_=rsd[:]), r=[tagk + 'rs'], w=[tagk + 'rs'])
            op('dve', lambda e: e.tensor_scalar(out=rt, in0=rt, scalar1=mv[:, 0:1], scalar2=rsd[:, 0:1], op0=ALU.subtract, op1=ALU.mult), r=[rtk, tagk + 'mv', tagk + 'rs'], w=[rtk])
            for c8 in range(8):
                cs = slice(c8 * 512, (c8 + 1) * 512)
                gt_, gk_ = g_r.get()
                bt_, bk_ = b_r.get()
                op('sp', lambda e, gt_=gt_, cs=cs: e.dma_start(out=gt_[:], in_=gsrc[:, cs]), w=[gk_], dma=True)
                op('sp', lambda e, bt_=bt_, cs=cs: e.dma_start(out=bt_[:], in_=bsrc[:, cs]), w=[bk_], dma=True)
                op('dve', lambda e, gt_=gt_, cs=cs: e.tensor_tensor(out=rt[:, cs], in0=rt[:, cs], in1=gt_[:], op=ALU.mult), r=[rtk, gk_], w=[rtk])
                op('pool', lambda e, bt_=bt_, cs=cs: e.tensor_tensor(out=rt[:, cs], in0=rt[:, cs], in1=bt_[:], op=ALU.add), r=[rtk, bk_], w=[rtk])

        if stages >= 7:
            pes = contextlib.ExitStack()
            pes.__enter__()
            A_ = lambda n, shp, dt: pes.enter_context(nc.sbuf_tensor(n, shp, dt))
            r_r = Ring(pes, nc, "l_r", [128, D_], F32, 2)
            g_r = Ring(pes, nc, "l_g", [128, 512], F32, 3)
            b_r = Ring(pes, nc, "l_b", [128, 512], F32, 3)
            st = A_("l_st", [128, 8, 6], F32)
            mv = A_("l_mv", [128, 2], F32)
            rsd = A_("l_rsd", [128, 1], F32)
            xb_r2 = Ring(pes, nc, "l_xb", [128, D_], BF16, 2)
            xT_r2 = Ring(pes, nc, "l_xT", [128, 32, 128], BF16, 2)
            xTf = A_("l_xTf", [128, 32, 128], F32)
            wr_sb = A_("l_wr", [128, 32, 64], F32)
            rb_sb = A_("l_rb", [128, 64], F32)
            scs = A_("l_sc", [128, 64], F32)
            sel = A_("l_sel", [128, 64], F32)
            m8r = A_("l_m8", [128, 8], F32)
            ssum = A_("l_ss", [128, 1], F32)
            gt_r = Ring(pes, nc, "l_gt", [128, 65], F32, 2)
            op('sp', lambda e: e.dma_start(out=wr_sb[:], in_=w_r.rearrange("(k p) c -> p k c", p=128)), w=['l_wr'], dma=True)
            op('sp', lambda e: e.dma_start(out=rb_sb[:], in_=rbias), w=['l_rb'], dma=True)
            for tt in range(NT):
                tsl = slice(tt * 128, (tt + 1) * 128)
                rt, rtk = r_r.get()
                op('sp', lambda e, rt=rt, tsl=tsl: e.dma_start(out=rt[:], in_=R1[tsl, :]), w=[rtk], dma=True)
                layer_norm_tile(rt[:, :], rtk, ln1g, ln1b, g_r, b_r, st, mv, rsd, 'l1')
                op('act', lambda e, rt=rt, tsl=tsl: e.dma_start(out=X1[tsl, :], in_=rt[:]), r=[rtk], w=[('X1', tt)], dma=True)
                xb, xbk = xb_r2.get()
                op('act', lambda e, xb=xb, rt=rt: e.copy(out=xb[:], in_=rt[:]), r=[rtk], w=[xbk])
                xT, xTk = xT_r2.get()
                transpose_blocks(lambda j, xb=xb: xb[:, j * 128:(j + 1) * 128], 32, lambda j0, n, xT=xT: xT[:, j0:j0 + n, :], [xbk], lambda j0, xTk=xTk: xTk)
                op('act', lambda e, xT=xT, tt=tt: e.dma_start(out=x1T_d[tt].rearrange("p (k t) -> p k t", k=32), in_=xT[:]), r=[xTk], w=[('x1T_d', tt)], dma=True)
                for j0 in range(0, 32, 4):
                    pf, pfk = ps_o.get()
                    for j in range(4):
                        op('pe', lambda e, pf=pf, j=j, j0=j0, rt=rt: e.transpose(out=pf[:, j * 128:(j + 1) * 128], in_=rt[:, (j0 + j) * 128:(j0 + j + 1) * 128], identity=idf[:]),
                           r=[rtk, 'idf'], w=[pfk])
                    op('act', lambda e, pf=pf, j0=j0: e.copy(out=xTf[:, j0:j0 + 4, :], in_=pf[:, :].rearrange("p (j t) -> p j t", j=4)), r=[pfk], w=[('l_xTf', j0)])
                pr_, prk = ps_acc.get()
                for k in range(32):
                    op('pe', lambda e, pr_=pr_, k=k: e.matmul(pr_[:, 0:64], lhsT=xTf[:, k, :], rhs=wr_sb[:, k, :], start=(k == 0), stop=(k == 31)),
                       r=[('l_xTf', (k // 4) * 4), 'l_wr'], w=[prk])
                gt, gtk = gt_r.get()
                op('act', lambda e, pr_=pr_: e.activation(out=scs[:], in_=pr_[:, 0:64], func=AF.Sigmoid), r=[prk], w=['l_sc'])
                op('dve', lambda e: e.tensor_tensor(out=sel[:], in0=scs[:], in1=rb_sb[:], op=ALU.add), r=['l_sc', 'l_rb'], w=['l_sel'])
                op('dve', lambda e: e.max(out=m8r[:], in_=sel[:]), r=['l_sel'], w=['l_m8'])
                op('dve', lambda e: e.tensor_scalar(out=sel[:], in0=sel[:], scalar1=m8r[:, 7:8], scalar2=None, op0=ALU.is_ge), r=['l_sel', 'l_m8'], w=['l_sel'])
                op('dve', lambda e: e.tensor_tensor(out=sel[:], in0=sel[:], in1=scs[:], op=ALU.mult), r=['l_sel', 'l_sc'], w=['l_sel'])
                op('dve', lambda e: e.reduce_sum(out=ssum[:], in_=sel[:], axis=AX.X), r=['l_sel'], w=['l_ss'])
                op('dve', lambda e: e.reciprocal(out=ssum[:], in_=ssum[:]), r=['l_ss'], w=['l_ss'])
                op('dve', lambda e, gt=gt: e.tensor_scalar(out=gt[:, 0:64], in0=sel[:], scalar1=ssum[:, 0:1], scalar2=2.5, op0=ALU.mult, op1=ALU.mult), r=['l_sel', 'l_ss'], w=[gtk])
                op('dve', lambda e, gt=gt: e.memset(gt[:, 64:65], 1.0), w=[gtk])
                op('act', lambda e, gt=gt, tsl=tsl: e.dma_start(out=Gt[tsl, :], in_=gt[:]), r=[gtk], w=[('Gt', tt)], dma=True)
            S.flush()
            pes.__exit__(None, None, None)

        if stages >= 8:
            pes = contextlib.ExitStack()
            pes.__enter__()
            A_ = lambda n, shp, dt: pes.enter_context(nc.sbuf_tensor(n, shp, dt))
            acc = A_("e_acc", [128, 4, D_], F32)
            x1c = A_("e_x1c", [128, 32, 512], BF16)
            gts = A_("e_gt", [128, 4, 65], F32)
            wg_r = Ring(pes, nc, "e_wg", [128, 32, 256], BF16, 2)
            wu_r = Ring(pes, nc, "e_wu", [128, 32, 256], BF16, 2)
            wd_r = Ring(pes, nc, "e_wd", [128, 2, D_], BF16, 2)
            sg_r = Ring(pes, nc, "e_sg", [128, 512], F32, 2)
            h_r = Ring(pes, nc, "e_h", [128, 512], BF16, 4)
            g_r = Ring(pes, nc, "e_g", [128, 512], F32, 1)
            b_r = Ring(pes, nc, "e_b", [128, 512], F32, 1)
            st = A_("e_st", [128, 8, 6], F32)
            mv = A_("e_mv", [128, 2], F32)
            rsd = A_("e_rsd", [128, 1], F32)
            NE = 65
            for tc in range(8):
                for j in range(4):
                    tt = tc * 4 + j
                    tsl = slice(tt * 128, (tt + 1) * 128)
                    op('sp', lambda e, j=j, tsl=tsl: e.dma_start(out=acc[:, j, :], in_=X1[tsl, :]), w=[('e_acc', j)], dma=True)
                    op('act', lambda e, j=j: e.activation(out=acc[:, j, :], in_=acc[:, j, :], func=AF.Copy, scale=ALPHA), r=[('e_acc', j)], w=[('e_acc', j)])
                    op('sp', lambda e, j=j, tt=tt: e.dma_start(out=x1c[:, :, j * 128:(j + 1) * 128], in_=x1T_d[tt].rearrange("p (k t) -> p k t", k=32)), w=[('e_x1c', j)], dma=True)
                    op('sp', lambda e, j=j, tsl=tsl: e.dma_start(out=gts[:, j, :], in_=Gt[tsl, :]), w=[('e_gt', j)], dma=True)
                xkeys = [('e_x1c', j) for j in range(4)]
                steps = [(ex, fh) for ex in range(NE) for fh in range(2)]
                nst = len(steps)
                gu_banks = [(ps_acc.bufs[0], ('psacc', 0)), (ps_acc.bufs[1], ('psacc', 1)), (ps_o.bufs[0], ('pso', 0)), (ps_o.bufs[1], ('pso', 1))]
                dn_banks = [(ps_o.bufs[2], ('pso', 2)), (ps_o.bufs[3], ('pso', 3))]
                wts = {}
                hts_of = {}

                def load_gu(i):
                    ex, fh = steps[i]
                    wg, wgk = wg_r.get()
                    wu, wuk = wu_r.get()
                    op('pool', lambda e, wg=wg, ex=ex, fh=fh: e.dma_start(out=wg[:], in_=eg[ex, fh].rearrange("p (k f) -> p k f", k=32), max_dma_last_dim=4096), w=[wgk], dma=True)
                    op('pool', lambda e, wu=wu, ex=ex, fh=fh: e.dma_start(out=wu[:], in_=eu[ex, fh].rearrange("p (k f) -> p k f", k=32), max_dma_last_dim=4096), w=[wuk], dma=True)
                    wts[('gu', i)] = (wg, wgk, wu, wuk)

                def load_d(i):
                    ex, fh = steps[i]
                    wd, wdk = wd_r.get()
                    op('pool', lambda e, wd=wd, ex=ex, fh=fh: e.dma_start(out=wd[:], in_=ed[ex, fh].rearrange("p (t c) -> p t c", t=2), max_dma_last_dim=4096), w=[wdk], dma=True)
                    wts[('d', i)] = (wd, wdk)

                def gu_group(i, q):
                    wg, wgk, wu, wuk = wts[('gu', i)]
                    for m in range(q * 4, q * 4 + 4):
                        ft, rem = divmod(m, 64)
                        isu, k = divmod(rem, 32)
                        bank, bk = gu_banks[ft * 2 + isu]
                        w_, wk_ = (wu, wuk) if isu else (wg, wgk)
                        op('pe', lambda e, bank=bank, w_=w_, k=k, ft=ft: e.matmul(bank[:, :], lhsT=w_[:, k, ft * 128:(ft + 1) * 128], rhs=x1c[:, k, :], start=(k == 0), stop=(k == 31)),
                           r=[wk_] + xkeys, w=[bk])
                    if q in (15, 31):
                        ft = 0 if q == 15 else 1
                        (pg_, pgk), (pu_, puk) = gu_banks[ft * 2], gu_banks[ft * 2 + 1]
                        sg, sgk = sg_r.get()
                        op('act', lambda e, sg=sg, pg_=pg_: e.activation(out=sg[:], in_=pg_[:, :], func=AF.Silu), r=[pgk], w=[sgk])
                        hT, hk = h_r.get()
                        op('dve', lambda e, hT=hT, sg=sg, pu_=pu_: e.tensor_tensor(out=hT[:], in0=sg[:], in1=pu_[:, :], op=ALU.mult), r=[sgk, puk], w=[hk])
                        hts_of.setdefault(i, []).append((hT, hk))

                def dn_group(i, q):
                    ex, fh = steps[i]
                    wd, wdk = wts[('d', i)]
                    tq, cc = divmod(q, 8)
                    po, pok = dn_banks[q % 2]
                    for ft in range(2):
                        hT, hk = hts_of[i][ft]
                        op('pe', lambda e, po=po, hT=hT, wd=wd, ft=ft, tq=tq, cc=cc: e.matmul(po[:, :], lhsT=hT[:, tq * 128:(tq + 1) * 128], rhs=wd[:, ft, cc * 512:(cc + 1) * 512], start=(ft == 0), stop=(ft == 1)),
                           r=[hk, wdk], w=[pok])
                    op('dve', lambda e, po=po, tq=tq, cc=cc, ex=ex: e.scalar_tensor_tensor(out=acc[:, tq, cc * 512:(cc + 1) * 512], in0=po[:, :], scalar=gts[:, tq, ex:ex + 1], in1=acc[:, tq, cc * 512:(cc + 1) * 512], op0=ALU.mult, op1=ALU.add),
                       r=[pok, ('e_gt', tq), ('e_acc', tq)], w=[('e_acc', tq)])

                load_gu(0)
                load_d(0)
                load_gu(1)
                for q in range(32):
                    gu_group(0, q)
                for i in range(nst):
                    if i + 2 < nst:
                        load_gu(i + 2)
                    if i + 1 < nst:
                        load_d(i + 1)
                    for q in range(32):
                        if i + 1 < nst:
                            gu_group(i + 1, q)
                        dn_group(i, q)
                    hts_of.pop(i, None)
                    wts.pop(('gu', i), None)
                    wts.pop(('d', i), None)
                for j in range(4):
                    tt = tc * 4 + j
                    tsl = slice(tt * 128, (tt + 1) * 128)
                    layer_norm_tile(acc[:, j, :], ('e_acc', j), ln2g, ln2b, g_r, b_r, st, mv, rsd, 'l2')
                    fin.append(op('act', lambda e, j=j, tsl=tsl: e.dma_start(out=out[tsl, :], in_=acc[:, j, :]), r=[('e_acc', j)], w=[('out', tt)], dma=True))
            S.wait_all('act', fin)
            S.flush()
            pes.__exit__(None, None, None)
        else:
            zt = sb("zt", [128, D_], F32)
            op('dve', lambda e: e.memset(zt[:], 0.0), w=['zt'])
            for tt in range(NT):
                fin.append(op('act', lambda e, tt=tt: e.dma_start(out=out[tt * 128:(tt + 1) * 128, :], in_=zt[:]), r=['zt'], w=[('out', tt)], dma=True))
            S.wait_all('act', fin)
            S.flush()
        S.close()
    return nc


def host_constants():
    c = {}
    c["ident"] = np.eye(128, dtype=np.float32)
    inva = (10000.0 ** (-np.arange(32, dtype=np.float32) * 2.0 / 64)).astype(np.float32)
    invb = (10000.0 ** (-np.arange(64, dtype=np.float32) * 2.0 / 128)).astype(np.float32)
    c["inva"] = np.ascontiguousarray(np.broadcast_to(inva, (128, 32)))
    c["invb"] = np.ascontiguousarray(np.broadcast_to(invb, (128, 64)))
    k = np.arange(128)[:, None]
    q = np.arange(512)[None, :]

    def band(r, W):
        d = q - (128 * r + k)
        return ((d >= 0) & (d < W)).astype(np.float32)
    c["m_swa"] = np.stack([band(r, 128) for r in range(-1, 4)])
    c["m_win"] = np.stack([band(r, 512) for r in range(-4, 4)])
    t = np.arange(S_)[:, None]
    blk = np.arange(256)[None, :]
    mc = ((16 * blk + 31 <= t) & (blk < 255)).astype(np.float32)
    c["m_cmp"] = mc
    c["m_cmpT"] = np.ascontiguousarray(mc.T.reshape(2, 128, S_))
    j = np.arange(64)[None, :]
    cur = t // 64
    valid = (j * 64 <= t)
    forced = (j == 0) | (j == cur) | (j == cur - 1)
    c["sl_vb"] = valid.astype(np.float32)
    c["sl_add"] = (1e9 * forced - (~valid)).astype(np.float32)
    ex = np.zeros((64, 32, 128), np.float32)
    for kt in range(32):
        for kk in range(128):
            ex[2 * kt + kk // 64, kt, kk] = 1.0
    c["exmat"] = ex.reshape(64, 32 * 128)
    return c


def lay_gu(w):
    E = w.shape[0]
    return np.ascontiguousarray(w.reshape(E, 32, 128, 2, 256).transpose(0, 3, 2, 1, 4)).reshape(E, 2, 128, 32 * 256)


def lay_d(w):
    E = w.shape[0]
    return np.ascontiguousarray(w.reshape(E, 2, 2, 128, 4096).transpose(0, 1, 3, 2, 4)).reshape(E, 2, 128, 2 * 4096)


def kernel(x, positions, w_in, a_sinks, cmp_k_pos, cmp_k_w1, cmp_k_w2, cmp_v_pos, cmp_v_w1, cmp_v_w2,
           w_o, ln1_g, ln1_b, w_router, router_bias, exp_w_gate, exp_w_up, exp_w_down,
           sh_w_gate, sh_w_up, sh_w_down, ln2_g, ln2_b):
    f = lambda a: np.ascontiguousarray(np.asarray(a))
    bc = lambda v, n: np.ascontiguousarray(np.broadcast_to(np.asarray(v).reshape(1, -1), (128, n)))
    nc = build_nc()
    shared = host_constants()
    shared.update({
        "w_in": f(w_in[0]), "sinks_b": bc(a_sinks[0], 64),
        "kpeT": f(np.asarray(cmp_k_pos[0]).T), "vpeT": f(np.asarray(cmp_v_pos[0]).T),
        "kw1": f(cmp_k_w1[0]), "vw1": f(cmp_v_w1[0]), "kw2": f(cmp_k_w2[0]), "vw2": f(cmp_v_w2[0]),
        "w_o": f(w_o[0]), "ln1g": bc(ln1_g[0], D_), "ln1b": bc(ln1_b[0], D_), "ln2g": bc(ln2_g[0], D_), "ln2b": bc(ln2_b[0], D_),
        "w_r": f(w_router[0]), "rbias": bc(router_bias[0], 64),
        "eg": lay_gu(np.concatenate([np.asarray(exp_w_gate[0]), np.asarray(sh_w_gate[0])[None]], 0)),
        "eu": lay_gu(np.concatenate([np.asarray(exp_w_up[0]), np.asarray(sh_w_up[0])[None]], 0)),
        "ed": lay_d(np.concatenate([np.asarray(exp_w_down[0]), np.asarray(sh_w_down[0])[None]], 0)),
    })
    in_maps = []
    for b in range(NCORES):
        m = dict(shared)
        m["x"] = f(np.asarray(x)[b])
        m["pos"] = f(np.asarray(positions)[b].astype(np.int32).reshape(NT, 128).T)
        in_maps.append({k: v for k, v in m.items() if k in DECLARED})
    res = run_bass_kernel_spmd(nc, in_maps, core_ids=list(range(NCORES)))
    return np.stack([np.asarray(res.results[b]["out"]) for b in range(NCORES)], 0).astype(np.float32)
```

```python
import math
import contextlib
import numpy as np
import concourse.bass as bass
import concourse.mybir as mybir
from concourse.bass_utils import run_bass_kernel_spmd

F32 = mybir.dt.float32
BF16 = mybir.dt.bfloat16
I32 = mybir.dt.int32
AF = mybir.ActivationFunctionType
ALU = mybir.AluOpType
AX = mybir.AxisListType

COMPUTE = ('pe', 'act', 'dve', 'pool')
NCORES = 2
S_ = 4096
D_ = 4096
NT = S_ // 128
ALPHA = 2.0 ** 0.25
EPS = 1e-5
TWO_PI = 2.0 * math.pi
C1 = 6.28125
C2 = TWO_PI - C1


class Sched:
    def __init__(self, nc, n_dma_sems=8):
        self.nc = nc
        self.streams = {e: [] for e in ('pe', 'act', 'dve', 'pool', 'sp')}
        self._sem_ctx = []
        self.sems = {e: self._mksem('c_' + e) for e in COMPUTE}
        self.seq = {e: 0 for e in COMPUTE}
        self.dma_sems, self.dma_val, self.dma_rr = {}, {}, {}
        for q in ('sp', 'act', 'pool'):
            self.dma_sems[q] = [self._mksem('d_%s%d' % (q, i)) for i in range(n_dma_sems)]
            self.dma_rr[q] = 0
            for s in self.dma_sems[q]:
                self.dma_val[id(s)] = 0
        self.observed = {e: {} for e in self.streams}
        self.last_w = {}
        self.readers = {}
        self.n_ins = 0

    def _mksem(self, name):
        ctx = self.nc.semaphore(name)
        h = ctx.__enter__()
        self._sem_ctx.append(ctx)
        return h

    def close(self):
        for c in reversed(self._sem_ctx):
            c.__exit__(None, None, None)

    def _need(self, eng, dep, waits):
        if dep is None:
            return
        sem, val, deng = dep
        if deng == eng and eng == 'pe':
            return
        ob = self.observed[eng]
        if ob.get(id(sem), 0) >= val:
            return
        ob[id(sem)] = val
        for i, (s, v) in enumerate(waits):
            if s is sem:
                waits[i] = (s, max(v, val))
                return
        waits.append((sem, val))

    def op(self, eng, fn, reads=(), writes=(), dma=False):
        waits = []
        for k in reads:
            self._need(eng, self.last_w.get(k), waits)
        for k in writes:
            self._need(eng, self.last_w.get(k), waits)
            for r in list(self.readers.get(k, {}).values()):
                self._need(eng, r, waits)
        if dma:
            lst = self.dma_sems[eng]
            s = lst[self.dma_rr[eng] % len(lst)]
            self.dma_rr[eng] += 1
            old = self.dma_val[id(s)]
            if old > 0:
                self._need(eng, (s, old, 'dma'), waits)
            val = old + 16
            self.dma_val[id(s)] = val
            comp = (s, val, 'dma')
            inc = (s, 16)
        else:
            self.seq[eng] += 1
            comp = (self.sems[eng], self.seq[eng], eng)
            inc = (self.sems[eng], 1)
        for k in reads:
            self.readers.setdefault(k, {})[id(comp[0])] = comp
        for k in writes:
            self.last_w[k] = comp
            self.readers[k] = {}
        self.streams[eng].append((waits, fn, inc))
        self.n_ins += 1
        return comp

    def wait_all(self, eng, comps):
        waits = []
        for c in comps:
            self._need(eng, c, waits)
        self.streams[eng].append((waits, None, None))

    def barrier(self):
        comps = [(self.sems[e], self.seq[e], e) for e in COMPUTE if self.seq[e] > 0]
        for q in self.dma_sems:
            for sm in self.dma_sems[q]:
                if self.dma_val[id(sm)] > 0:
                    comps.append((sm, self.dma_val[id(sm)], 'dma'))
        for e in self.streams:
            waits = []
            for c in comps:
                sem, val, deng = c
                ob = self.observed[e]
                if ob.get(id(sem), 0) >= val:
                    continue
                ob[id(sem)] = val
                waits.append((sem, val))
            self.streams[e].append((waits, None, None))

    def flush(self):
        self.barrier()
        self.emit()
        self.streams = {e: [] for e in self.streams}

    def emit(self):
        engobj = {'pe': 'tensor', 'act': 'scalar', 'dve': 'vector', 'pool': 'gpsimd', 'sp': 'sync'}
        with self.nc.Block() as block:
            for e, attr in engobj.items():
                stream = self.streams[e]

                def body(engine, stream=stream):
                    for waits, fn, inc in stream:
                        for (s, v) in waits:
                            engine.wait_ge(s, v)
                        if fn is not None:
                            fn(engine).then_inc(inc[0], inc[1])
                getattr(block, attr)(body)


class Ring:
    def __init__(self, es, nc, name, shape, dt, n, psum=False):
        mk = nc.psum_tensor if psum else nc.sbuf_tensor
        self.bufs = [es.enter_context(mk("%s%d" % (name, i), shape, dt)) for i in range(n)]
        self.name = name
        self.i = 0

    def get(self):
        j = self.i % len(self.bufs)
        self.i += 1
        return self.bufs[j], (self.name, j)


QA0, KA0, VA0, QB0, KC0, VC0, KS0, VS0, KW0, VW0, GN0, GA0, GB0 = (
    0, 4096, 4608, 5120, 9216, 9728, 10240, 10752, 11264, 11776, 12288, 12384, 16480)
WTOT = 20576


DECLARED = []
DBG_NAMES = ("X1", "Gt", "out")
USED_INPUTS = ("x", "pos", "w_in", "inva", "invb", "ident", "sinks_b", "m_swa", "m_win", "kpeT", "vpeT", "kw1", "vw1", "kw2", "vw2", "m_cmpT", "m_cmp", "sl_vb", "sl_add", "exmat",
               "w_o", "ln1g", "ln1b", "ln2g", "ln2b", "w_r", "rbias", "eg", "eu", "ed")


def build_nc(stages=99, dbg=False):
    del DECLARED[:]
    nc = bass.Bass("TRN2", target_bir_lowering=False)
    S = Sched(nc)

    def din(name, shape, dt=F32):
        return nc.dram_tensor(name, list(shape), dt, kind="ExternalInput").ap()

    DBG_OUT = DBG_NAMES

    def dscr(name, shape, dt=BF16):
        if dbg and name in DBG_OUT:
            return nc.dram_tensor(name, list(shape), dt, kind="ExternalOutput").ap()
        return nc.dram_tensor(name, list(shape), dt).ap()

    _din = din

    def din(name, shape, dt=F32):
        if name not in USED_INPUTS or (name in ("eg", "eu", "ed") and stages < 8):
            return None
        DECLARED.append(name)
        return _din(name, shape, dt)

    x = din("x", [S_, D_])
    pos = din("pos", [128, NT], I32)
    w_in = din("w_in", [D_, WTOT])
    esink_in = din("sinks_b", [128, 64])
    inva = din("inva", [128, 32])
    invb = din("invb", [128, 64])
    ident_in = din("ident", [128, 128])
    kpeT = din("kpeT", [128, 32])
    vpeT = din("vpeT", [128, 32])
    kw1 = din("kw1", [4096, 256])
    vw1 = din("vw1", [4096, 256])
    kw2 = din("kw2", [256, 128])
    vw2 = din("vw2", [256, 128])
    w_o = din("w_o", [D_, D_])
    ln1g = din("ln1g", [128, D_])
    ln1b = din("ln1b", [128, D_])
    ln2g = din("ln2g", [128, D_])
    ln2b = din("ln2b", [128, D_])
    w_r = din("w_r", [D_, 64])
    rbias = din("rbias", [128, 64])
    eg = din("eg", [65, 2, 128, 32 * 256])
    eu = din("eu", [65, 2, 128, 32 * 256])
    ed = din("ed", [65, 2, 128, 2 * D_])
    m_swa = din("m_swa", [5, 128, 512])
    m_win = din("m_win", [8, 128, 512])
    m_cmpT = din("m_cmpT", [2, 128, S_])
    m_cmp = din("m_cmp", [S_, 256])
    sl_vb = din("sl_vb", [S_, 64])
    sl_add = din("sl_add", [S_, 64])
    exmat = din("exmat", [64, 32 * 128])
    out = nc.dram_tensor("out", [S_, D_], F32, kind="ExternalOutput").ap()

    xT_d = dscr("xT_d", [NT, 128, 32 * 128])
    QaT = dscr("QaT", [32, 128, S_])
    KaT2 = dscr("KaT2", [8, 128, S_])
    Va = dscr("Va", [S_, 8 * 65])
    QbT = dscr("QbT", [32, 128, S_])
    kcT = dscr("kcT", [4, 128, S_])
    vcT = dscr("vcT", [4, 128, S_])
    ksT = dscr("ksT", [4, 128, S_])
    kwT = dscr("kwT", [4, 128, S_])
    Vs = dscr("Vs", [S_, 4 * 129])
    Vw = dscr("Vw", [S_, 4 * 129])
    Gn = dscr("Gn", [S_, 96], F32)
    Ga = dscr("Ga", [S_, D_], F32)
    Gb = dscr("Gb", [S_, D_], F32)
    Oa = dscr("Oa", [S_, D_], F32)
    Ocmp = dscr("Ocmp", [S_, D_], F32)
    Oslc = dscr("Oslc", [S_, D_], F32)
    Owin = dscr("Owin", [S_, D_], F32)
    yT_d = dscr("yT_d", [NT, 128, 32 * 128])
    R1 = dscr("R1", [S_, D_], F32)
    X1 = dscr("X1", [S_, D_], F32)
    x1T_d = dscr("x1T_d", [NT, 128, 32 * 128])
    Gt = dscr("Gt", [S_, 65], F32)
    kcmpT_d = dscr("kcmpT_d", [4, 128, S_])
    vcmp_d = dscr("vcmp_d", [S_, 4 * 129])
    selT_d = dscr("selT_d", [4, 64, S_])

    es = contextlib.ExitStack()
    with es:
        es.enter_context(nc.allow_low_precision("bf16 matmul operands, fp32 accumulation"))

        def sb(name, shape, dt):
            return es.enter_context(nc.sbuf_tensor(name, shape, dt))

        def op(eng, fn, r=(), w=(), dma=False):
            return S.op(eng, fn, reads=r, writes=w, dma=dma)

        idf = sb("idf", [128, 128], F32)
        idb = sb("idb", [128, 128], BF16)
        op('sp', lambda e: e.dma_start(out=idf[:], in_=ident_in), w=['idf'], dma=True)
        op('dve', lambda e: e.tensor_copy(out=idb[:], in_=idf[:]), r=['idf'], w=['idb'])

        ps_acc = Ring(es, nc, "psacc", [128, 512], F32, 2, psum=True)
        ps_t = Ring(es, nc, "pst", [128, 8, 128], BF16, 2, psum=True)
        ps_o = Ring(es, nc, "pso", [128, 512], F32, 4, psum=True)

        def transpose_blocks(src_fn, nblk, dst_fn, rkeys, wkey_fn):
            for j0 in range(0, nblk, 8):
                n = min(8, nblk - j0)
                pt, pk = ps_t.get()
                for j in range(n):
                    op('pe', lambda e, j=j, pt=pt, j0=j0: e.transpose(out=pt[:, j, :], in_=src_fn(j0 + j), identity=idb[:]),
                       r=list(rkeys) + ['idb'], w=[pk])
                op('act', lambda e, pt=pt, n=n, j0=j0: e.copy(out=dst_fn(j0, n), in_=pt[:, 0:n, :]), r=[pk], w=[wkey_fn(j0)])

        pes = contextlib.ExitStack()
        pes.__enter__()
        xf_r = Ring(pes, nc, "xf", [128, D_], F32, 2)
        xb_r = Ring(pes, nc, "xb", [128, D_], BF16, 2)
        xt_r = Ring(pes, nc, "xTt", [128, 32, 128], BF16, 2)
        for tt in range(NT):
            xf, kf = xf_r.get()
            xb, kb = xb_r.get()
            xt, kt = xt_r.get()
            op('sp', lambda e, xf=xf, tt=tt: e.dma_start(out=xf[:], in_=x[tt * 128:(tt + 1) * 128, :]), w=[kf], dma=True)
            op('dve', lambda e, xf=xf, xb=xb: e.tensor_copy(out=xb[:], in_=xf[:]), r=[kf], w=[kb])
            transpose_blocks(lambda j, xb=xb: xb[:, j * 128:(j + 1) * 128], 32,
                             lambda j0, n, xt=xt: xt[:, j0:j0 + n, :], [kb], lambda j0, kt=kt: kt)
            op('act', lambda e, xt=xt, tt=tt: e.dma_start(out=xT_d[tt].rearrange("p (k t) -> p k t", k=32), in_=xt[:]),
               r=[kt], w=[('xT_d', tt)], dma=True)

        S.flush()
        pes.__exit__(None, None, None)
        tes = contextlib.ExitStack()
        tes.__enter__()
        sbt = lambda n, shp, dt: tes.enter_context(nc.sbuf_tensor(n, shp, dt))
        cosA = sbt("cosA", [128, NT, 32], F32)
        sinA = sbt("sinA", [128, NT, 32], F32)
        cosB = sbt("cosB", [128, NT, 64], F32)
        sinB = sbt("sinB", [128, NT, 64], F32)
        inv_sb = sbt("inv_sb", [128, 96], F32)
        posi = sbt("posi", [128, NT], I32)
        posf = sbt("posf", [128, NT], F32)
        op('sp', lambda e: e.dma_start(out=inv_sb[:, 0:32], in_=inva), w=['inva'], dma=True)
        op('sp', lambda e: e.dma_start(out=inv_sb[:, 32:96], in_=invb), w=['invb'], dma=True)
        op('sp', lambda e: e.dma_start(out=posi[:], in_=pos), w=['posi'], dma=True)
        op('dve', lambda e: e.tensor_copy(out=posf[:], in_=posi[:]), r=['posi'], w=['posf'])
        ang = sbt("ang", [128, 96], F32)
        kq_i = sbt("kq_i", [128, 96], I32)
        kq = sbt("kq", [128, 96], F32)
        rr = sbt("rr", [128, 96], F32)
        mm = sbt("mm", [128, 96], F32)
        r2 = sbt("r2", [128, 96], F32)

        def wrap(v):
            op('dve', lambda e: e.tensor_single_scalar(out=mm[:], in_=v[:], scalar=math.pi, op=ALU.is_gt), r=['rp'], w=['mm'])
            op('dve', lambda e: e.scalar_tensor_tensor(out=v[:], in0=mm[:], scalar=-TWO_PI, in1=v[:], op0=ALU.mult, op1=ALU.add), r=['mm', 'rp'], w=['rp'])
            op('dve', lambda e: e.tensor_single_scalar(out=mm[:], in_=v[:], scalar=-math.pi, op=ALU.is_lt), r=['rp'], w=['mm'])
            op('dve', lambda e: e.scalar_tensor_tensor(out=v[:], in0=mm[:], scalar=TWO_PI, in1=v[:], op0=ALU.mult, op1=ALU.add), r=['mm', 'rp'], w=['rp'])

        for tt in range(NT):
            op('dve', lambda e, tt=tt: e.tensor_scalar(out=ang[:], in0=inv_sb[:], scalar1=posf[:, tt:tt + 1], scalar2=None, op0=ALU.mult),
               r=['inva', 'invb', 'posf'], w=['rp'])
            op('dve', lambda e: e.tensor_single_scalar(out=kq[:], in_=ang[:], scalar=1.0 / TWO_PI, op=ALU.mult), r=['rp'], w=['kq'])
            op('dve', lambda e: e.tensor_copy(out=kq_i[:], in_=kq[:]), r=['kq'], w=['kqi'])
            op('dve', lambda e: e.tensor_copy(out=kq[:], in_=kq_i[:]), r=['kqi'], w=['kq'])
            op('dve', lambda e: e.scalar_tensor_tensor(out=rr[:], in0=kq[:], scalar=-C1, in1=ang[:], op0=ALU.mult, op1=ALU.add), r=['kq', 'rp'], w=['rp'])
            op('dve', lambda e: e.scalar_tensor_tensor(out=rr[:], in0=kq[:], scalar=-C2, in1=rr[:], op0=ALU.mult, op1=ALU.add), r=['kq', 'rp'], w=['rp'])
            wrap(rr)
            op('act', lambda e, tt=tt: e.activation(out=sinA[:, tt, :], in_=rr[:, 0:32], func=AF.Sin), r=['rp'], w=['sinA'])
            op('act', lambda e, tt=tt: e.activation(out=sinB[:, tt, :], in_=rr[:, 32:96], func=AF.Sin), r=['rp'], w=['sinB'])
            op('dve', lambda e: e.tensor_single_scalar(out=r2[:], in_=rr[:], scalar=math.pi / 2, op=ALU.add), r=['rp'], w=['rp'])
            wrap(r2)
            op('act', lambda e, tt=tt: e.activation(out=cosA[:, tt, :], in_=r2[:, 0:32], func=AF.Sin), r=['rp'], w=['cosA'])
            op('act', lambda e, tt=tt: e.activation(out=cosB[:, tt, :], in_=r2[:, 32:96], func=AF.Sin), r=['rp'], w=['cosB'])

        pes = contextlib.ExitStack()
        pes.__enter__()
        wb_r = Ring(pes, nc, "wb", [128, 32, 512], BF16, 2)
        xl_r = Ring(pes, nc, "xl", [128, 32, 128], BF16, 3)
        ep_f = Ring(pes, nc, "epf", [128, 512], F32, 2)
        ep_b = Ring(pes, nc, "epb", [128, 1024], BF16, 5)
        tr_b = Ring(pes, nc, "trb", [128, 8, 128], BF16, 3)
        t1 = pes.enter_context(nc.sbuf_tensor("t1", [128, 256], F32))
        t2 = pes.enter_context(nc.sbuf_tensor("t2", [128, 256], F32))
        ones_col = pes.enter_context(nc.sbuf_tensor("ones_col", [128, 8], BF16))
        op('dve', lambda e: e.memset(ones_col[:], 1.0), w=['ones_col'])
        w_in_v = w_in.rearrange("(k p) c -> p k c", p=128)

        chunks = []
        for c in range(8):
            chunks.append((QA0 + c * 512, 512, 'ropeA_q', c))
        chunks.append((KA0, 512, 'ropeA_k', 0))
        chunks.append((VA0, 512, 'v_a', 0))
        for c in range(8):
            chunks.append((QB0 + c * 512, 512, 'ropeB_q', c))
        chunks.append((KC0, 512, 'ropeB_k', kcT))
        chunks.append((VC0, 512, 'plainT', vcT))
        chunks.append((KS0, 512, 'ropeB_k', ksT))
        chunks.append((VS0, 512, 'v_b', Vs))
        chunks.append((KW0, 512, 'ropeB_k', kwT))
        chunks.append((VW0, 512, 'v_b', Vw))
        chunks.append((GN0, 96, 'sig', Gn))
        for c in range(8):
            chunks.append((GA0 + c * 512, 512, 'sig', (Ga, c)))
        for c in range(8):
            chunks.append((GB0 + c * 512, 512, 'sig', (Gb, c)))

        def rope(ps, pk, H, hd, cos_t, sin_t, tt, ob, obk):
            v = ps[:, :].rearrange("p (h t d) -> p h t d", h=H, t=2)
            o = ob[:, 0:512].rearrange("p (h t d) -> p h t d", h=H, t=2)
            cb = cos_t[:, tt, :].unsqueeze(1).broadcast_to([128, H, hd])
            sn = sin_t[:, tt, :].unsqueeze(1).broadcast_to([128, H, hd])
            a = t1[:, :].rearrange("p (h d) -> p h d", h=H)
            b = t2[:, :].rearrange("p (h d) -> p h d", h=H)
            tabs = ['cosA', 'sinA', 'cosB', 'sinB']
            op('dve', lambda e: e.tensor_tensor(out=a, in0=v[:, :, 0, :], in1=cb, op=ALU.mult), r=[pk] + tabs, w=['t1'])
            op('dve', lambda e: e.tensor_tensor(out=b, in0=v[:, :, 1, :], in1=sn, op=ALU.mult), r=[pk] + tabs, w=['t2'])
            op('dve', lambda e: e.tensor_tensor(out=o[:, :, 0, :], in0=a, in1=b, op=ALU.subtract), r=['t1', 't2'], w=[obk])
            op('dve', lambda e: e.tensor_tensor(out=a, in0=v[:, :, 1, :], in1=cb, op=ALU.mult), r=[pk] + tabs, w=['t1'])
            op('dve', lambda e: e.tensor_tensor(out=b, in0=v[:, :, 0, :], in1=sn, op=ALU.mult), r=[pk] + tabs, w=['t2'])
            op('dve', lambda e: e.tensor_tensor(out=o[:, :, 1, :], in0=a, in1=b, op=ALU.add), r=['t1', 't2'], w=[obk])

        def to_featmajor(ob, obk, nblk, dst, tt, idx0):
            tb, tk = tr_b.get()
            transpose_blocks(lambda j: ob[:, j * 128:(j + 1) * 128], nblk, lambda j0, n: tb[:, j0:j0 + n, :], [obk], lambda j0: tk)
            op('act', lambda e: e.dma_start(out=dst[idx0:idx0 + nblk, :, tt * 128:(tt + 1) * 128].rearrange("j p t -> p j t"), in_=tb[:, 0:nblk, :]),
               r=[tk], w=[('featT', id(dst), tt)], dma=True)

        pending_tf = []

        def defer_tf(*a):
            pending_tf.append(a)

        def flush_tf():
            while pending_tf:
                to_featmajor(*pending_tf.pop(0))

        for (c0, ncol, kind, arg) in (chunks if stages >= 1 else []):
            wb, wk = wb_r.get()
            for k0 in range(0, 32, 8):
                op('pool', lambda e, wb=wb, k0=k0, c0=c0, ncol=ncol: e.dma_start(out=wb[:, k0:k0 + 8, 0:ncol], in_=w_in_v[:, k0:k0 + 8, c0:c0 + ncol]),
                   w=[(wk, k0)], dma=True)
            for tt in range(NT):
                xl, xk = xl_r.get()
                op('sp', lambda e, xl=xl, tt=tt: e.dma_start(out=xl[:], in_=xT_d[tt].rearrange("p (k t) -> p k t", k=32)),
                   r=[('xT_d', tt)], w=[xk], dma=True)
                ps, pk = ps_acc.get()
                for k in range(32):
                    op('pe', lambda e, ps=ps, xl=xl, wb=wb, k=k, ncol=ncol: e.matmul(ps[:, 0:ncol], lhsT=xl[:, k, :], rhs=wb[:, k, 0:ncol], start=(k == 0), stop=(k == 31)),
                       r=[xk, (wk, (k // 8) * 8)], w=[pk])
                flush_tf()
                tsl = slice(tt * 128, (tt + 1) * 128)
                if kind == 'sig':
                    of, ofk = ep_f.get()
                    op('act', lambda e, of=of, ps=ps, ncol=ncol: e.activation(out=of[:, 0:ncol], in_=ps[:, 0:ncol], func=AF.Sigmoid), r=[pk], w=[ofk])
                    if ncol == 96:
                        op('act', lambda e, of=of, tsl=tsl: e.dma_start(out=Gn[tsl, :], in_=of[:, 0:96]), r=[ofk], w=[('Gn', tt)], dma=True)
                    else:
                        dst, cc = arg
                        op('act', lambda e, of=of, tsl=tsl, dst=dst, cc=cc: e.dma_start(out=dst[tsl, cc * 512:(cc + 1) * 512], in_=of[:]), r=[ofk], w=[('G', id(dst), tt, cc)], dma=True)
                elif kind == 'v_a':
                    ob, obk = ep_b.get()
                    o3 = ob[:, 0:520].rearrange("p (g d) -> p g d", g=8)
                    op('act', lambda e, o3=o3, ps=ps: e.copy(out=o3[:, :, 0:64], in_=ps[:, :].rearrange("p (g d) -> p g d", g=8)), r=[pk], w=[obk])
                    op('dve', lambda e, o3=o3: e.tensor_copy(out=o3[:, :, 64:65], in_=ones_col[:, 0:8].unsqueeze(2)), r=['ones_col'], w=[obk])
                    op('act', lambda e, ob=ob, tsl=tsl: e.dma_start(out=Va[tsl, :], in_=ob[:, 0:520]), r=[obk], w=[('Va', tt)], dma=True)
                elif kind == 'v_b':
                    ob, obk = ep_b.get()
                    o3 = ob[:, 0:516].rearrange("p (g d) -> p g d", g=4)
                    op('act', lambda e, o3=o3, ps=ps: e.copy(out=o3[:, :, 0:128], in_=ps[:, :].rearrange("p (g d) -> p g d", g=4)), r=[pk], w=[obk])
                    op('dve', lambda e, o3=o3: e.tensor_copy(out=o3[:, :, 128:129], in_=ones_col[:, 0:4].unsqueeze(2)), r=['ones_col'], w=[obk])
                    op('act', lambda e, ob=ob, tsl=tsl, arg=arg: e.dma_start(out=arg[tsl, :], in_=ob[:, 0:516]), r=[obk], w=[('Vb', id(arg), tt)], dma=True)
                elif kind == 'plainT':
                    ob, obk = ep_b.get()
                    op('act', lambda e, ob=ob, ps=ps: e.copy(out=ob[:, 0:512], in_=ps[:, :]), r=[pk], w=[obk])
                    defer_tf(ob, obk, 4, arg, tt, 0)
                elif kind == 'ropeB_k':
                    ob, obk = ep_b.get()
                    rope(ps, pk, 4, 64, cosB, sinB, tt, ob, obk)
                    defer_tf(ob, obk, 4, arg, tt, 0)
                elif kind == 'ropeB_q':
                    ob, obk = ep_b.get()
                    rope(ps, pk, 4, 64, cosB, sinB, tt, ob, obk)
                    defer_tf(ob, obk, 4, QbT, tt, arg * 4)
                elif kind == 'ropeA_q':
                    ob, obk = ep_b.get()
                    rope(ps, pk, 8, 32, cosA, sinA, tt, ob, obk)
                    defer_tf(ob, obk, 4, QaT, tt, arg * 4)
                elif kind == 'ropeA_k':
                    ob, obk = ep_b.get()
                    rope(ps, pk, 8, 32, cosA, sinA, tt, ob, obk)
                    ob2, ob2k = ep_b.get()
                    src = ob[:, 0:512].rearrange("p (g d) -> p g d", g=8)
                    d4 = ob2[:, 0:1024].rearrange("p (g t d) -> p g t d", g=8, t=2)
                    op('dve', lambda e, d4=d4, src=src: e.tensor_copy(out=d4[:, :, 0, :], in_=src), r=[obk], w=[ob2k])
                    op('dve', lambda e, d4=d4, src=src: e.tensor_copy(out=d4[:, :, 1, :], in_=src), r=[obk], w=[ob2k])
                    defer_tf(ob2, ob2k, 8, KaT2, tt, 0)
        flush_tf()

        S.flush()
        pes.__exit__(None, None, None)
        tes.__exit__(None, None, None)
        def attention_phase(tag, nheads, hpg, dh, dv, QT_src, KT_src, V_src, vstride, plan, Odst, mask_setup, sink_tab=None):
            pes = contextlib.ExitStack()
            pes.__enter__()
            scale = float(dh) ** -0.5
            pre_g, pre_gc, mask_fn = mask_setup(pes)
            kt_r = Ring(pes, nc, tag + "K", [128, S_], BF16, 2)
            v_r = Ring(pes, nc, tag + "V", [128, NT, dv + 1], BF16, 2)
            nq = hpg if dh == 128 else hpg // 2
            q_r = Ring(pes, nc, tag + "Q", [128, S_], BF16, nq + 1)
            e_r = Ring(pes, nc, tag + "E", [128, 512], BF16, 4)
            os_r = Ring(pes, nc, tag + "O", [128, 2, dv], F32, 3)
            rec_r = Ring(pes, nc, tag + "R", [128, 2], F32, 3)
            G = nheads // hpg
            cnt = [0]
            for g in range(G):
                ksb, kk = kt_r.get()
                vsb, vk = v_r.get()
                op('sp', lambda e, ksb=ksb, g=g: e.dma_start(out=ksb[:], in_=KT_src(g)), w=[kk], dma=True)
                op('sp', lambda e, vsb=vsb, g=g: e.dma_start(out=vsb[:], in_=V_src[:, g * vstride:(g + 1) * vstride].rearrange("(kt p) d -> p kt d", p=128)),
                   w=[vk], dma=True)
                qtiles = {}
                for hh in range(hpg):
                    qsrc, pbase, qid = QT_src(g * hpg + hh)
                    if qid not in qtiles:
                        qsb, qk = q_r.get()
                        op('sp', lambda e, qsb=qsb, qsrc=qsrc: e.dma_start(out=qsb[:], in_=qsrc), w=[qk], dma=True)
                        qtiles[qid] = (qsb, qk)
                if pre_g is not None:
                    pre_g(g)
                for c in range(8):
                    kts = plan(c)
                    if pre_gc is not None:
                        pre_gc(g, c)
                    for hh in range(hpg):
                        h = g * hpg + hh
                        qsrc, pbase, qid = QT_src(h)
                        qsb, qk = qtiles[qid]
                        pr = slice(pbase, pbase + dh)
                        ob = [ps_o.get(), ps_o.get()]
                        first = [True, True]
                        def emit_pv(eb, ek, kt, ki):
                            for qi in range(4):
                                (ot, otk) = ob[qi // 2]
                                j = qi % 2
                                stf = first[qi // 2]
                                first[qi // 2] = False
                                op('pe', lambda e, ot=ot, eb=eb, vsb=vsb, kt=kt, qi=qi, j=j, stf=stf, last=(ki == len(kts) - 1):
                                   e.matmul(ot[:, j * (dv + 1):(j + 1) * (dv + 1)], lhsT=eb[:, qi * 128:(qi + 1) * 128], rhs=vsb[:, kt, :], start=stf, stop=last),
                                   r=[ek, vk], w=[otk])
                        pend = None
                        for ki, kt in enumerate(kts):
                            st, stk = ps_acc.get()
                            op('pe', lambda e, st=st, ksb=ksb, qsb=qsb, pr=pr, kt=kt, c=c: e.matmul(st[:, :], lhsT=ksb[pr, kt * 128:(kt + 1) * 128], rhs=qsb[pr, c * 512:(c + 1) * 512], start=True, stop=True),
                               r=[kk, qk], w=[stk])
                            eb, ek = e_r.get()
                            op('act', lambda e, eb=eb, st=st: e.activation(out=eb[:], in_=st[:, :], func=AF.Exp, scale=scale), r=[stk], w=[ek])
                            mk = mask_fn(g, c, kt)
                            if mk is not None:
                                cnt[0] += 1
                                eng = 'dve' if cnt[0] % 2 == 0 else 'pool'
                                op(eng, lambda e, eb=eb, mk=mk: e.tensor_tensor(out=eb[:], in0=eb[:], in1=mk[0], op=ALU.mult), r=[ek, mk[1]], w=[ek])
                            if pend is not None:
                                emit_pv(*pend)
                            pend = (eb, ek, kt, ki)
                        emit_pv(*pend)
                        for b2 in range(2):
                            ot, otk = ob[b2]
                            o3 = ot[:, 0:2 * (dv + 1)].rearrange("p (j d) -> p j d", j=2)
                            rec, rk = rec_r.get()
                            osb, osk = os_r.get()
                            s1 = sink_tab[:, h:h + 1] if sink_tab is not None else 0.0
                            op('dve', lambda e, rec=rec, o3=o3, s1=s1: e.tensor_scalar(out=rec[:].unsqueeze(2), in0=o3[:, :, dv:dv + 1], scalar1=s1, scalar2=1e-30, op0=ALU.add, op1=ALU.max),
                               r=[otk, 'esink'], w=[rk])
                            op('dve', lambda e, rec=rec: e.reciprocal(out=rec[:], in_=rec[:]), r=[rk], w=[rk])
                            op('dve', lambda e, osb=osb, o3=o3, rec=rec: e.tensor_tensor(out=osb[:], in0=o3[:, :, 0:dv], in1=rec[:].unsqueeze(2).broadcast_to([128, 2, dv]), op=ALU.mult),
                               r=[otk, rk], w=[osk])
                            t0 = c * 512 + b2 * 256
                            op('act', lambda e, osb=osb, t0=t0, h=h: e.dma_start(out=Odst[t0:t0 + 256, h * dv:(h + 1) * dv].rearrange("(j p) d -> p j d", p=128), in_=osb[:]),
                               r=[osk], w=[('O', tag, h, c, b2)], dma=True)
            S.flush()
            pes.__exit__(None, None, None)

        def static_masks(tag, masks_in, idx_fn):
            def setup(pes):
                n = len(masks_in)
                msk = pes.enter_context(nc.sbuf_tensor(tag + "msk", [128, n, 512], BF16))
                for r in range(n):
                    op('pool', lambda e, r=r: e.dma_start(out=msk[:, r, :], in_=masks_in[r]), w=[(tag, 'msk')], dma=True)
                return None, None, (lambda g, c, kt: (msk[:, idx_fn(c, kt), :], (tag, 'msk')))
            return setup

        if stages >= 2:
            esink = sb("esink", [128, 64], F32)
            op('sp', lambda e: e.dma_start(out=esink[:], in_=esink_in), w=['esink'], dma=True)
            op('act', lambda e: e.activation(out=esink[:], in_=esink[:], func=AF.Exp), r=['esink'], w=['esink'])
            attention_phase("swa", 64, 8, 64, 64,
                            lambda h: (QaT[h // 2], 64 * (h % 2), h // 2), lambda g: KaT2[g], Va, 65,
                            lambda c: [kt for kt in range(4 * c - 1, 4 * c + 4) if kt >= 0], Oa,
                            static_masks("swa", [m_swa[r] for r in range(5)], lambda c, kt: kt - 4 * c + 1), sink_tab=esink)
        if stages >= 3:
            attention_phase("win", 32, 8, 128, 128,
                            lambda h: (QbT[h], 0, h), lambda g: kwT[g], Vw, 129,
                            lambda c: [kt for kt in range(4 * c - 4, 4 * c + 4) if kt >= 0], Owin,
                            static_masks("win", [m_win[r] for r in range(8)], lambda c, kt: kt - 4 * c + 4))

        if stages >= 4:
            pes = contextlib.ExitStack()
            pes.__enter__()
            A_ = lambda n, shp, dt: pes.enter_context(nc.sbuf_tensor(n, shp, dt))
            w1k = A_("w1k", [128, 32, 256], BF16)
            w1v = A_("w1v", [128, 32, 256], BF16)
            w2k = A_("w2k", [128, 2, 128], BF16)
            w2v = A_("w2v", [128, 2, 128], BF16)
            pek = A_("pek", [128, 32], F32)
            pev = A_("pev", [128, 32], F32)
            op('pool', lambda e: e.dma_start(out=w1k[:], in_=kw1.rearrange("(l d) h -> d l h", d=128)), w=['w1k'], dma=True)
            op('pool', lambda e: e.dma_start(out=w1v[:], in_=vw1.rearrange("(l d) h -> d l h", d=128)), w=['w1v'], dma=True)
            op('pool', lambda e: e.dma_start(out=w2k[:], in_=kw2.rearrange("(hh p) d -> p hh d", p=128)), w=['w2k'], dma=True)
            op('pool', lambda e: e.dma_start(out=w2v[:], in_=vw2.rearrange("(hh p) d -> p hh d", p=128)), w=['w2v'], dma=True)
            op('sp', lambda e: e.dma_start(out=pek[:], in_=kpeT), w=['pek'], dma=True)
            op('sp', lambda e: e.dma_start(out=pev[:], in_=vpeT), w=['pev'], dma=True)
            kcmp_sb = A_("kcmp_sb", [128, 4, 256], BF16)
            vcmp_sb = A_("vcmp_sb", [128, 2, 4 * 129], BF16)
            op('dve', lambda e: e.memset(kcmp_sb[:], 0.0), w=['kcmp_sb'])
            op('dve', lambda e: e.memset(vcmp_sb[:], 0.0), w=['vcmp_sb'])
            src_r = Ring(pes, nc, "csrc", [128, S_], BF16, 2)
            tmp_r = Ring(pes, nc, "ctmp", [128, 256], BF16, 4)
            xs = A_("cxs", [128, 256], F32)
            g1 = A_("cg1", [128, 256], F32)
            g2 = A_("cg2", [128, 256], F32)
            hg = A_("chg", [128, 2, 256], BF16)
            op('dve', lambda e: e.memset(hg[:], 0.0), w=['chg'])
            for which in ('k', 'v'):
                srcT, w1, w2, pe_, = (kcT, w1k, w2k, pek) if which == 'k' else (vcT, w1v, w2v, pev)
                for g in range(4):
                    sT, sk = src_r.get()
                    op('sp', lambda e, sT=sT, g=g, srcT=srcT: e.dma_start(out=sT[:], in_=srcT[g]), w=[sk], dma=True)
                    pa = [ps_acc.get(), ps_acc.get()]
                    for l in range(32):
                        tm, tk = tmp_r.get()
                        op('dve', lambda e, tm=tm, sT=sT, l=l, pe_=pe_: e.tensor_scalar(out=tm[:, 0:255], in0=sT[:, l:l + 16 * 254 + 1:16], scalar1=pe_[:, l:l + 1], scalar2=None, op0=ALU.add),
                           r=[sk, 'pek', 'pev'], w=[tk])
                        for hh in range(2):
                            op('pe', lambda e, hh=hh, tm=tm, l=l, w1=w1, pa=pa: e.matmul(pa[hh][0][:, 0:255], lhsT=w1[:, l, hh * 128:(hh + 1) * 128], rhs=tm[:, 0:255], start=(l == 0), stop=(l == 31)),
                               r=[tk, 'w1k', 'w1v'], w=[pa[hh][1]])
                    for hh in range(2):
                        pt_, pk_ = pa[hh]
                        op('act', lambda e, pt_=pt_: e.copy(out=xs[:, 0:255], in_=pt_[:, 0:255]), r=[pk_], w=['cxs'])
                        op('dve', lambda e: e.tensor_tensor(out=g1[:, 0:255], in0=xs[:, 0:255], in1=xs[:, 0:255], op=ALU.mult), r=['cxs'], w=['cg1'])
                        op('dve', lambda e: e.tensor_scalar(out=g1[:, 0:255], in0=g1[:, 0:255], scalar1=0.044715, scalar2=1.0, op0=ALU.mult, op1=ALU.add), r=['cg1'], w=['cg1'])
                        op('dve', lambda e: e.tensor_tensor(out=g1[:, 0:255], in0=g1[:, 0:255], in1=xs[:, 0:255], op=ALU.mult), r=['cg1', 'cxs'], w=['cg1'])
                        op('act', lambda e: e.activation(out=g2[:, 0:255], in_=g1[:, 0:255], func=AF.Sigmoid, scale=1.5957691216057308), r=['cg1'], w=['cg2'])
                        op('dve', lambda e, hh=hh: e.tensor_tensor(out=hg[:, hh, 0:255], in0=xs[:, 0:255], in1=g2[:, 0:255], op=ALU.mult), r=['cxs', 'cg2'], w=['chg'])
                    if which == 'k':
                        pt_, pk_ = ps_acc.get()
                        for hh in range(2):
                            op('pe', lambda e, hh=hh, pt_=pt_: e.matmul(pt_[:, 0:256], lhsT=w2k[:, hh, :], rhs=hg[:, hh, :], start=(hh == 0), stop=(hh == 1)), r=['chg', 'w2k'], w=[pk_])
                        op('act', lambda e, pt_=pt_, g=g: e.copy(out=kcmp_sb[:, g, 0:255], in_=pt_[:, 0:255]), r=[pk_], w=['kcmp_sb'])
                    else:
                        for bt in range(2):
                            pt_, pk_ = ps_acc.get()
                            for hh in range(2):
                                op('pe', lambda e, hh=hh, pt_=pt_, bt=bt: e.matmul(pt_[:, 0:128], lhsT=hg[:, hh, bt * 128:(bt + 1) * 128], rhs=w2v[:, hh, :], start=(hh == 0), stop=(hh == 1)), r=['chg', 'w2v'], w=[pk_])
                            op('act', lambda e, pt_=pt_, g=g, bt=bt: e.copy(out=vcmp_sb[:, bt, g * 129:g * 129 + 128], in_=pt_[:, 0:128]), r=[pk_], w=['vcmp_sb'])
                            op('dve', lambda e, g=g, bt=bt: e.memset(vcmp_sb[:, bt, g * 129 + 128:g * 129 + 129], 1.0), r=[], w=['vcmp_sb'])
            for g in range(4):
                op('act', lambda e, g=g: e.dma_start(out=kcmpT_d[g][:, 0:256], in_=kcmp_sb[:, g, :]), r=['kcmp_sb'], w=[('kcmpT_d', g)], dma=True)
            op('act', lambda e: e.dma_start(out=vcmp_d[0:256, :].rearrange("(bt p) d -> p bt d", p=128), in_=vcmp_sb[:]), r=['vcmp_sb'], w=['vcmp_d'], dma=True)

            qg_r = Ring(pes, nc, "pq", [128, S_], BF16, 9)
            mc_r = Ring(pes, nc, "pmc", [128, 256], F32, 2)
            vb_r = Ring(pes, nc, "pvb", [128, 64], F32, 2)
            ad_r = Ring(pes, nc, "pad", [128, 64], F32, 2)
            ee = A_("pee", [128, 256], F32)
            den = A_("pden", [128, 1], F32)
            pg = A_("ppg", [128, 256], F32)
            s3 = A_("ps3", [128, 64], F32)
            psl = A_("ppsl", [128, 64], F32)
            sc = A_("psc", [128, 64], F32)
            sc2 = A_("psc2", [128, 64], F32)
            m8 = A_("pm8", [128, 8], F32)
            selb = A_("pselb", [128, 64], BF16)
            selT_sb = A_("pselT", [64, S_], BF16)
            for g in range(4):
                qs = []
                for hh in range(8):
                    qsb, qk = qg_r.get()
                    op('sp', lambda e, qsb=qsb, h=g * 8 + hh: e.dma_start(out=qsb[:], in_=QbT[h]), w=[qk], dma=True)
                    qs.append((qsb, qk))
                for tt in range(NT):
                    mc, mck = mc_r.get()
                    vb, vbk = vb_r.get()
                    ad, adk = ad_r.get()
                    tsl = slice(tt * 128, (tt + 1) * 128)
                    op('sp', lambda e, mc=mc, tsl=tsl: e.dma_start(out=mc[:], in_=m_cmp[tsl, :]), w=[mck], dma=True)
                    op('sp', lambda e, vb=vb, tsl=tsl: e.dma_start(out=vb[:], in_=sl_vb[tsl, :]), w=[vbk], dma=True)
                    op('sp', lambda e, ad=ad, tsl=tsl: e.dma_start(out=ad[:], in_=sl_add[tsl, :]), w=[adk], dma=True)
                    for hh in range(8):
                        qsb, qk = qs[hh]
                        pt_, pk_ = ps_acc.get()
                        op('pe', lambda e, pt_=pt_, qsb=qsb, tsl=tsl, g=g: e.matmul(pt_[:, 0:256], lhsT=qsb[:, tsl], rhs=kcmp_sb[:, g, :], start=True, stop=True), r=[qk, 'kcmp_sb'], w=[pk_])
                        op('act', lambda e, pt_=pt_: e.activation(out=ee[:], in_=pt_[:, 0:256], func=AF.Exp, scale=128.0 ** -0.5), r=[pk_], w=['pee'])
                        op('dve', lambda e, mc=mc: e.tensor_tensor(out=ee[:], in0=ee[:], in1=mc[:], op=ALU.mult), r=['pee', mck], w=['pee'])
                        op('dve', lambda e: e.reduce_sum(out=den[:], in_=ee[:], axis=AX.X), r=['pee'], w=['pden'])
                        op('dve', lambda e: e.tensor_scalar(out=den[:], in0=den[:], scalar1=1e-30, scalar2=None, op0=ALU.max), r=['pden'], w=['pden'])
                        op('dve', lambda e: e.reciprocal(out=den[:], in_=den[:]), r=['pden'], w=['pden'])
                        if hh == 0:
                            op('dve', lambda e: e.tensor_scalar(out=pg[:], in0=ee[:], scalar1=den[:, 0:1], scalar2=None, op0=ALU.mult), r=['pee', 'pden'], w=['ppg'])
                        else:
                            op('dve', lambda e: e.scalar_tensor_tensor(out=pg[:], in0=ee[:], scalar=den[:, 0:1], in1=pg[:], op0=ALU.mult, op1=ALU.add), r=['pee', 'pden', 'ppg'], w=['ppg'])
                    pgv = pg[:, :].rearrange("p (j m) -> p j m", m=4)
                    op('dve', lambda e, pgv=pgv: e.tensor_tensor(out=s3[:], in0=pgv[:, :, 0], in1=pgv[:, :, 1], op=ALU.add), r=['ppg'], w=['ps3'])
                    op('dve', lambda e, pgv=pgv: e.tensor_tensor(out=s3[:], in0=s3[:], in1=pgv[:, :, 2], op=ALU.add), r=['ppg', 'ps3'], w=['ps3'])
                    op('dve', lambda e, pgv=pgv: e.scalar_tensor_tensor(out=psl[:], in0=s3[:], scalar=2.0, in1=pgv[:, :, 3], op0=ALU.mult, op1=ALU.add), r=['ppg', 'ps3'], w=['ppsl'])
                    op('dve', lambda e, pgv=pgv: e.tensor_tensor(out=psl[:, 1:64], in0=psl[:, 1:64], in1=pgv[:, 0:63, 3], op=ALU.add), r=['ppg', 'ppsl'], w=['ppsl'])
                    op('dve', lambda e, vb=vb: e.tensor_tensor(out=sc[:], in0=psl[:], in1=vb[:], op=ALU.mult), r=['ppsl', vbk], w=['psc'])
                    op('dve', lambda e, ad=ad: e.tensor_tensor(out=sc[:], in0=sc[:], in1=ad[:], op=ALU.add), r=['psc', adk], w=['psc'])
                    op('dve', lambda e: e.max(out=m8[:], in_=sc[:]), r=['psc'], w=['pm8'])
                    op('dve', lambda e: e.match_replace(out=sc2[:], in_to_replace=m8[:], in_values=sc[:], imm_value=-1e30), r=['psc', 'pm8'], w=['psc2'])
                    op('dve', lambda e: e.max(out=m8[:], in_=sc2[:]), r=['psc2'], w=['pm8'])
                    op('dve', lambda e: e.tensor_scalar(out=sc2[:], in0=sc[:], scalar1=m8[:, 7:8], scalar2=None, op0=ALU.is_ge), r=['psc', 'pm8'], w=['psc2'])
                    op('dve', lambda e, vb=vb: e.tensor_tensor(out=selb[:], in0=sc2[:], in1=vb[:], op=ALU.mult), r=['psc2', vbk], w=['pselb'])
                    ptt, ptk = ps_t.get()
                    op('pe', lambda e, ptt=ptt: e.transpose(out=ptt[0:64, 0, :], in_=selb[:, 0:64], identity=idb[:]), r=['pselb', 'idb'], w=[ptk])
                    op('act', lambda e, ptt=ptt, tsl=tsl: e.copy(out=selT_sb[:, tsl], in_=ptt[0:64, 0, :]), r=[ptk], w=['pselT'])
                op('act', lambda e, g=g: e.dma_start(out=selT_d[g], in_=selT_sb[:]), r=['pselT'], w=[('selT_d', g)], dma=True)
            S.flush()
            pes.__exit__(None, None, None)

            def cmp_masks(pes):
                msk = pes.enter_context(nc.sbuf_tensor("cmpmsk", [128, 2, S_], BF16))
                for bt in range(2):
                    op('pool', lambda e, bt=bt: e.dma_start(out=msk[:, bt, :], in_=m_cmpT[bt]), w=[('cmp', 'msk')], dma=True)
                return None, None, (lambda g, c, kt: (msk[:, kt, c * 512:(c + 1) * 512], ('cmp', 'msk')))
            attention_phase("cmp", 32, 8, 128, 128, lambda h: (QbT[h], 0, h), lambda g: kcmpT_d[g], vcmp_d, 129,
                            lambda c: [0, 1], Ocmp, cmp_masks)

        if stages >= 5:
            def slc_masks(pes):
                exm = pes.enter_context(nc.sbuf_tensor("slcex", [64, 32, 128], BF16))
                mwin = pes.enter_context(nc.sbuf_tensor("slcmw", [128, 4, 512], BF16))
                mk_all = pes.enter_context(nc.sbuf_tensor("slcmk", [128, 32, 512], BF16))
                selr = Ring(pes, nc, "slcsel", [64, S_], BF16, 2)
                op('pool', lambda e: e.dma_start(out=exm[:], in_=exmat.rearrange("b (k q) -> b k q", k=32)), w=['slcex'], dma=True)
                for r in range(4):
                    op('pool', lambda e, r=r: e.dma_start(out=mwin[:, r, :], in_=m_win[4 + r]), w=['slcmw'], dma=True)
                cur = {}

                def pre_g(g):
                    sb_, sk_ = selr.get()
                    op('sp', lambda e, sb_=sb_, g=g: e.dma_start(out=sb_[:], in_=selT_d[g]), w=[sk_], dma=True)
                    cur['sel'] = (sb_, sk_)

                def pre_gc(g, c):
                    sb_, sk_ = cur['sel']
                    for kt in range(4 * c + 4):
                        pt_, pk_ = ps_acc.get()
                        op('pe', lambda e, pt_=pt_, kt=kt, sb_=sb_, c=c: e.matmul(pt_[:, :], lhsT=exm[:, kt, :], rhs=sb_[:, c * 512:(c + 1) * 512], start=True, stop=True),
                           r=['slcex', sk_], w=[pk_])
                        if kt >= 4 * c:
                            op('dve', lambda e, pt_=pt_, kt=kt, c=c: e.tensor_tensor(out=mk_all[:, kt, :], in0=pt_[:, :], in1=mwin[:, kt - 4 * c, :], op=ALU.mult),
                               r=[pk_, 'slcmw'], w=[('slcmk', kt)])
                        else:
                            op('act', lambda e, pt_=pt_, kt=kt: e.copy(out=mk_all[:, kt, :], in_=pt_[:, :]), r=[pk_], w=[('slcmk', kt)])
                return pre_g, pre_gc, (lambda g, c, kt: (mk_all[:, kt, :], ('slcmk', kt)))
            attention_phase("slc", 32, 8, 128, 128, lambda h: (QbT[h], 0, h), lambda g: ksT[g], Vs, 129,
                            lambda c: list(range(4 * c + 4)), Oslc, slc_masks)

        fin = []
        if stages >= 6:
            pes = contextlib.ExitStack()
            pes.__enter__()
            HW = 2048
            r_oa = Ring(pes, nc, "m_oa", [128, HW], F32, 2)
            r_oc = Ring(pes, nc, "m_oc", [128, HW], F32, 2)
            r_os = Ring(pes, nc, "m_os", [128, HW], F32, 2)
            r_ow = Ring(pes, nc, "m_ow", [128, HW], F32, 2)
            r_ga = Ring(pes, nc, "m_ga", [128, HW], F32, 2)
            r_gb = Ring(pes, nc, "m_gb", [128, HW], F32, 2)
            r_gn = Ring(pes, nc, "m_gn", [128, 96], F32, 2)
            r_acc = Ring(pes, nc, "m_acc", [128, HW], F32, 2)
            r_tmp = Ring(pes, nc, "m_tmp", [128, HW], F32, 4)
            r_y = Ring(pes, nc, "m_y", [128, D_], BF16, 2)
            r_yT = Ring(pes, nc, "m_yT", [128, 32, 128], BF16, 2)
            for tt in range(NT):
                tsl = slice(tt * 128, (tt + 1) * 128)
                gn, gnk = r_gn.get()
                op('sp', lambda e, gn=gn, tsl=tsl: e.dma_start(out=gn[:], in_=Gn[tsl, :]), w=[gnk], dma=True)
                yb, ybk = r_y.get()
                for hf in range(2):
                    cs = slice(hf * HW, (hf + 1) * HW)
                    tiles = []
                    for ring, src in ((r_oa, Oa), (r_oc, Ocmp), (r_os, Oslc), (r_ow, Owin), (r_ga, Ga), (r_gb, Gb)):
                        t_, k_ = ring.get()
                        op('sp', lambda e, t_=t_, src=src, tsl=tsl, cs=cs: e.dma_start(out=t_[:], in_=src[tsl, cs]), w=[k_], dma=True)
                        tiles.append((t_, k_))
                    (oa, oak), (oc, ock), (os_, osk), (ow, owk), (ga, gak), (gb_, gbk) = tiles
                    acc, ack = r_acc.get()
                    tmp, tmk = r_tmp.get()
                    v3 = lambda t_: t_[:, :].rearrange("p (h d) -> p h d", h=16)
                    gsl_ = [gn[:, j * 32 + hf * 16: j * 32 + hf * 16 + 16].unsqueeze(2).broadcast_to([128, 16, 128]) for j in range(3)]
                    gsl = lambda j, gsl_=gsl_: gsl_[j]
                    tmp2, tm2k = r_tmp.get()
                    op('dve', lambda e, acc=acc, oc=oc, g0=gsl_[0]: e.tensor_tensor(out=v3(acc), in0=v3(oc), in1=g0, op=ALU.mult), r=[ock, gnk], w=[ack])
                    op('dve', lambda e, tmp=tmp, os_=os_, g1_=gsl_[1]: e.tensor_tensor(out=v3(tmp), in0=v3(os_), in1=g1_, op=ALU.mult), r=[osk, gnk], w=[tmk])
                    op('pool', lambda e, acc=acc, tmp=tmp: e.tensor_tensor(out=acc[:], in0=acc[:], in1=tmp[:], op=ALU.add), r=[ack, tmk], w=[ack])
                    op('dve', lambda e, tmp2=tmp2, ow=ow, g2_=gsl_[2]: e.tensor_tensor(out=v3(tmp2), in0=v3(ow), in1=g2_, op=ALU.mult), r=[owk, gnk], w=[tm2k])
                    op('pool', lambda e, acc=acc, tmp2=tmp2: e.tensor_tensor(out=acc[:], in0=acc[:], in1=tmp2[:], op=ALU.add), r=[ack, tm2k], w=[ack])
                    op('pool', lambda e, acc=acc, gb_=gb_: e.tensor_tensor(out=acc[:], in0=acc[:], in1=gb_[:], op=ALU.mult), r=[ack, gbk], w=[ack])
                    op('pool', lambda e, tmp=tmp, oa=oa, ga=ga: e.tensor_tensor(out=tmp[:], in0=oa[:], in1=ga[:], op=ALU.mult), r=[oak, gak], w=[tmk])
                    op('dve', lambda e, acc=acc, tmp=tmp, yb=yb, cs=cs: e.tensor_tensor(out=yb[:, cs], in0=acc[:], in1=tmp[:], op=ALU.add), r=[ack, tmk], w=[ybk])
                yT, yTk = r_yT.get()
                transpose_blocks(lambda j, yb=yb: yb[:, j * 128:(j + 1) * 128], 32, lambda j0, n, yT=yT: yT[:, j0:j0 + n, :], [ybk], lambda j0, yTk=yTk: yTk)
                op('act', lambda e, yT=yT, tt=tt: e.dma_start(out=yT_d[tt].rearrange("p (k t) -> p k t", k=32), in_=yT[:]), r=[yTk], w=[('yT_d', tt)], dma=True)
            S.flush()
            pes.__exit__(None, None, None)

            pes = contextlib.ExitStack()
            pes.__enter__()
            wb_r = Ring(pes, nc, "wob", [128, 32, 512], BF16, 2)
            xl_r = Ring(pes, nc, "oyl", [128, 32, 128], BF16, 3)
            xs_r = Ring(pes, nc, "oxs", [128, 512], F32, 3)
            rs_r = Ring(pes, nc, "ors", [128, 512], F32, 3)
            w_o_v = w_o.rearrange("(k p) c -> p k c", p=128)
            for cc in range(8):
                wb, wk = wb_r.get()
                for k0 in range(0, 32, 8):
                    op('pool', lambda e, wb=wb, k0=k0, cc=cc: e.dma_start(out=wb[:, k0:k0 + 8, :], in_=w_o_v[:, k0:k0 + 8, cc * 512:(cc + 1) * 512]), w=[(wk, k0)], dma=True)
                for tt in range(NT):
                    tsl = slice(tt * 128, (tt + 1) * 128)
                    xl, xk = xl_r.get()
                    op('sp', lambda e, xl=xl, tt=tt: e.dma_start(out=xl[:], in_=yT_d[tt].rearrange("p (k t) -> p k t", k=32)), w=[xk], dma=True)
                    xs, xsk = xs_r.get()
                    op('sp', lambda e, xs=xs, tsl=tsl, cc=cc: e.dma_start(out=xs[:], in_=x[tsl, cc * 512:(cc + 1) * 512]), w=[xsk], dma=True)
                    ps, pk = ps_acc.get()
                    for k in range(32):
                        op('pe', lambda e, ps=ps, xl=xl, wb=wb, k=k: e.matmul(ps[:, :], lhsT=xl[:, k, :], rhs=wb[:, k, :], start=(k == 0), stop=(k == 31)),
                           r=[xk, (wk, (k // 8) * 8)], w=[pk])
                    rs, rsk = rs_r.get()
                    op('dve', lambda e, rs=rs, xs=xs, ps=ps: e.scalar_tensor_tensor(out=rs[:], in0=xs[:], scalar=ALPHA, in1=ps[:, :], op0=ALU.mult, op1=ALU.add), r=[xsk, pk], w=[rsk])
                    op('act', lambda e, rs=rs, tsl=tsl, cc=cc: e.dma_start(out=R1[tsl, cc * 512:(cc + 1) * 512], in_=rs[:]), r=[rsk], w=[('R1', tt, cc)], dma=True)
            S.flush()
            pes.__exit__(None, None, None)

        def layer_norm_tile(rt, rtk, gsrc, bsrc, g_r, b_r, st, mv, rsd, tagk):
            for c8 in range(8):
                op('dve', lambda e, c8=c8: e.bn_stats(out=st[:, c8, :], in_=rt[:, c8 * 512:(c8 + 1) * 512]), r=[rtk], w=[tagk + 'st'])
            op('dve', lambda e: e.bn_aggr(out=mv[:], in_=st[:].rearrange("p a b -> p (a b)")), r=[tagk + 'st'], w=[tagk + 'mv'])
            op('dve', lambda e: e.tensor_scalar(out=rsd[:], in0=mv[:, 1:2], scalar1=EPS, scalar2=None, op0=ALU.add), r=[tagk + 'mv'], w=[tagk + 'rs'])
            op('act', lambda e: e.activation(out=rsd[:], in_=rsd[:], func=AF.Sqrt), r=[tagk + 'rs'], w=[tagk + 'rs'])
            op('dve', lambda e: e.reciprocal(out=rsd[:], in_=rsd[:]), r=[tagk + 'rs'], w=[tagk + 'rs'])
            op('dve', lambda e: e.tensor_scalar(out=rt, in0=rt, scalar1=mv[:, 0:1], scalar2=rsd[:, 0:1], op0=ALU.subtract, op1=ALU.mult), r=[rtk, tagk + 'mv', tagk + 'rs'], w=[rtk])
            for c8 in range(8):
                cs = slice(c8 * 512, (c8 + 1) * 512)
                gt_, gk_ = g_r.get()
                bt_, bk_ = b_r.get()
                op('sp', lambda e, gt_=gt_, cs=cs: e.dma_start(out=gt_[:], in_=gsrc[:, cs]), w=[gk_], dma=True)
                op('sp', lambda e, bt_=bt_, cs=cs: e.dma_start(out=bt_[:], in_=bsrc[:, cs]), w=[bk_], dma=True)
                op('dve', lambda e, gt_=gt_, cs=cs: e.tensor_tensor(out=rt[:, cs], in0=rt[:, cs], in1=gt_[:], op=ALU.mult), r=[rtk, gk_], w=[rtk])
                op('pool', lambda e, bt_=bt_, cs=cs: e.tensor_tensor(out=rt[:, cs], in0=rt[:, cs], in1=bt_[:], op=ALU.add), r=[rtk, bk_], w=[rtk])

        if stages >= 7:
            pes = contextlib.ExitStack()
            pes.__enter__()
            A_ = lambda n, shp, dt: pes.enter_context(nc.sbuf_tensor(n, shp, dt))
            r_r = Ring(pes, nc, "l_r", [128, D_], F32, 2)
            g_r = Ring(pes, nc, "l_g", [128, 512], F32, 3)
            b_r = Ring(pes, nc, "l_b", [128, 512], F32, 3)
            st = A_("l_st", [128, 8, 6], F32)
            mv = A_("l_mv", [128, 2], F32)
            rsd = A_("l_rsd", [128, 1], F32)
            xb_r2 = Ring(pes, nc, "l_xb", [128, D_], BF16, 2)
            xT_r2 = Ring(pes, nc, "l_xT", [128, 32, 128], BF16, 2)
            xTf = A_("l_xTf", [128, 32, 128], F32)
            wr_sb = A_("l_wr", [128, 32, 64], F32)
            rb_sb = A_("l_rb", [128, 64], F32)
            scs = A_("l_sc", [128, 64], F32)
            sel = A_("l_sel", [128, 64], F32)
            m8r = A_("l_m8", [128, 8], F32)
            ssum = A_("l_ss", [128, 1], F32)
            gt_r = Ring(pes, nc, "l_gt", [128, 65], F32, 2)
            op('sp', lambda e: e.dma_start(out=wr_sb[:], in_=w_r.rearrange("(k p) c -> p k c", p=128)), w=['l_wr'], dma=True)
            op('sp', lambda e: e.dma_start(out=rb_sb[:], in_=rbias), w=['l_rb'], dma=True)
            for tt in range(NT):
                tsl = slice(tt * 128, (tt + 1) * 128)
                rt, rtk = r_r.get()
                op('sp', lambda e, rt=rt, tsl=tsl: e.dma_start(out=rt[:], in_=R1[tsl, :]), w=[rtk], dma=True)
                layer_norm_tile(rt[:, :], rtk, ln1g, ln1b, g_r, b_r, st, mv, rsd, 'l1')
                op('act', lambda e, rt=rt, tsl=tsl: e.dma_start(out=X1[tsl, :], in_=rt[:]), r=[rtk], w=[('X1', tt)], dma=True)
                xb, xbk = xb_r2.get()
                op('act', lambda e, xb=xb, rt=rt: e.copy(out=xb[:], in_=rt[:]), r=[rtk], w=[xbk])
                xT, xTk = xT_r2.get()
                transpose_blocks(lambda j, xb=xb: xb[:, j * 128:(j + 1) * 128], 32, lambda j0, n, xT=xT: xT[:, j0:j0 + n, :], [xbk], lambda j0, xTk=xTk: xTk)
                op('act', lambda e, xT=xT, tt=tt: e.dma_start(out=x1T_d[tt].rearrange("p (k t) -> p k t", k=32), in_=xT[:]), r=[xTk], w=[('x1T_d', tt)], dma=True)
                for j0 in range(0, 32, 4):
                    pf, pfk = ps_o.get()
                    for j in range(4):
                        op('pe', lambda e, pf=pf, j=j, j0=j0, rt=rt: e.transpose(out=pf[:, j * 128:(j + 1) * 128], in_=rt[:, (j0 + j) * 128:(j0 + j + 1) * 128], identity=idf[:]),
                           r=[rtk, 'idf'], w=[pfk])
                    op('act', lambda e, pf=pf, j0=j0: e.copy(out=xTf[:, j0:j0 + 4, :], in_=pf[:, :].rearrange("p (j t) -> p j t", j=4)), r=[pfk], w=[('l_xTf', j0)])
                pr_, prk = ps_acc.get()
                for k in range(32):
                    op('pe', lambda e, pr_=pr_, k=k: e.matmul(pr_[:, 0:64], lhsT=xTf[:, k, :], rhs=wr_sb[:, k, :], start=(k == 0), stop=(k == 31)),
                       r=[('l_xTf', (k // 4) * 4), 'l_wr'], w=[prk])
                gt, gtk = gt_r.get()
                op('act', lambda e, pr_=pr_: e.activation(out=scs[:], in_=pr_[:, 0:64], func=AF.Sigmoid), r=[prk], w=['l_sc'])
                op('dve', lambda e: e.tensor_tensor(out=sel[:], in0=scs[:], in1=rb_sb[:], op=ALU.add), r=['l_sc', 'l_rb'], w=['l_sel'])
                op('dve', lambda e: e.max(out=m8r[:], in_=sel[:]), r=['l_sel'], w=['l_m8'])
                op('dve', lambda e: e.tensor_scalar(out=sel[:], in0=sel[:], scalar1=m8r[:, 7:8], scalar2=None, op0=ALU.is_ge), r=['l_sel', 'l_m8'], w=['l_sel'])
                op('dve', lambda e: e.tensor_tensor(out=sel[:], in0=sel[:], in1=scs[:], op=ALU.mult), r=['l_sel', 'l_sc'], w=['l_sel'])
                op('dve', lambda e: e.reduce_sum(out=ssum[:], in_=sel[:], axis=AX.X), r=['l_sel'], w=['l_ss'])
                op('dve', lambda e: e.reciprocal(out=ssum[:], in_=ssum[:]), r=['l_ss'], w=['l_ss'])
                op('dve', lambda e, gt=gt: e.tensor_scalar(out=gt[:, 0:64], in0=sel[:], scalar1=ssum[:, 0:1], scalar2=2.5, op0=ALU.mult, op1=ALU.mult), r=['l_sel', 'l_ss'], w=[gtk])
                op('dve', lambda e, gt=gt: e.memset(gt[:, 64:65], 1.0), w=[gtk])
                op('act', lambda e, gt=gt, tsl=tsl: e.dma_start(out=Gt[tsl, :], in_=gt[:]), r=[gtk], w=[('Gt', tt)], dma=True)
            S.flush()
            pes.__exit__(None, None, None)

        if stages >= 8:
            pes = contextlib.ExitStack()
            pes.__enter__()
            A_ = lambda n, shp, dt: pes.enter_context(nc.sbuf_tensor(n, shp, dt))
            acc = A_("e_acc", [128, 4, D_], F32)
            x1c = A_("e_x1c", [128, 32, 512], BF16)
            gts = A_("e_gt", [128, 4, 65], F32)
            wg_r = Ring(pes, nc, "e_wg", [128, 32, 256], BF16, 2)
            wu_r = Ring(pes, nc, "e_wu", [128, 32, 256], BF16, 2)
            wd_r = Ring(pes, nc, "e_wd", [128, 2, D_], BF16, 2)
            sg_r = Ring(pes, nc, "e_sg", [128, 512], F32, 2)
            h_r = Ring(pes, nc, "e_h", [128, 512], BF16, 4)
            g_r = Ring(pes, nc, "e_g", [128, 512], F32, 1)
            b_r = Ring(pes, nc, "e_b", [128, 512], F32, 1)
            st = A_("e_st", [128, 8, 6], F32)
            mv = A_("e_mv", [128, 2], F32)
            rsd = A_("e_rsd", [128, 1], F32)
            NE = 65
            for tc in range(8):
                for j in range(4):
                    tt = tc * 4 + j
                    tsl = slice(tt * 128, (tt + 1) * 128)
                    op('sp', lambda e, j=j, tsl=tsl: e.dma_start(out=acc[:, j, :], in_=X1[tsl, :]), w=[('e_acc', j)], dma=True)
                    op('act', lambda e, j=j: e.activation(out=acc[:, j, :], in_=acc[:, j, :], func=AF.Copy, scale=ALPHA), r=[('e_acc', j)], w=[('e_acc', j)])
                    op('sp', lambda e, j=j, tt=tt: e.dma_start(out=x1c[:, :, j * 128:(j + 1) * 128], in_=x1T_d[tt].rearrange("p (k t) -> p k t", k=32)), w=[('e_x1c', j)], dma=True)
                    op('sp', lambda e, j=j, tsl=tsl: e.dma_start(out=gts[:, j, :], in_=Gt[tsl, :]), w=[('e_gt', j)], dma=True)
                xkeys = [('e_x1c', j) for j in range(4)]
                steps = [(ex, fh) for ex in range(NE) for fh in range(2)]
                nst = len(steps)
                gu_banks = [(ps_acc.bufs[0], ('psacc', 0)), (ps_acc.bufs[1], ('psacc', 1)), (ps_o.bufs[0], ('pso', 0)), (ps_o.bufs[1], ('pso', 1))]
                dn_banks = [(ps_o.bufs[2], ('pso', 2)), (ps_o.bufs[3], ('pso', 3))]
                wts = {}
                hts_of = {}

                def load_gu(i):
                    ex, fh = steps[i]
                    wg, wgk = wg_r.get()
                    wu, wuk = wu_r.get()
                    op('pool', lambda e, wg=wg, ex=ex, fh=fh: e.dma_start(out=wg[:], in_=eg[ex, fh].rearrange("p (k f) -> p k f", k=32), max_dma_last_dim=4096), w=[wgk], dma=True)
                    op('pool', lambda e, wu=wu, ex=ex, fh=fh: e.dma_start(out=wu[:], in_=eu[ex, fh].rearrange("p (k f) -> p k f", k=32), max_dma_last_dim=4096), w=[wuk], dma=True)
                    wts[('gu', i)] = (wg, wgk, wu, wuk)

                def load_d(i):
                    ex, fh = steps[i]
                    wd, wdk = wd_r.get()
                    op('pool', lambda e, wd=wd, ex=ex, fh=fh: e.dma_start(out=wd[:], in_=ed[ex, fh].rearrange("p (t c) -> p t c", t=2), max_dma_last_dim=4096), w=[wdk], dma=True)
                    wts[('d', i)] = (wd, wdk)

                def gu_group(i, q):
                    wg, wgk, wu, wuk = wts[('gu', i)]
                    for m in range(q * 4, q * 4 + 4):
                        ft, rem = divmod(m, 64)
                        isu, k = divmod(rem, 32)
                        bank, bk = gu_banks[ft * 2 + isu]
                        w_, wk_ = (wu, wuk) if isu else (wg, wgk)
                        op('pe', lambda e, bank=bank, w_=w_, k=k, ft=ft: e.matmul(bank[:, :], lhsT=w_[:, k, ft * 128:(ft + 1) * 128], rhs=x1c[:, k, :], start=(k == 0), stop=(k == 31)),
                           r=[wk_] + xkeys, w=[bk])
                    if q in (15, 31):
                        ft = 0 if q == 15 else 1
                        (pg_, pgk), (pu_, puk) = gu_banks[ft * 2], gu_banks[ft * 2 + 1]
                        sg, sgk = sg_r.get()
                        op('act', lambda e, sg=sg, pg_=pg_: e.activation(out=sg[:], in_=pg_[:, :], func=AF.Silu), r=[pgk], w=[sgk])
                        hT, hk = h_r.get()
                        op('dve', lambda e, hT=hT, sg=sg, pu_=pu_: e.tensor_tensor(out=hT[:], in0=sg[:], in1=pu_[:, :], op=ALU.mult), r=[sgk, puk], w=[hk])
                        hts_of.setdefault(i, []).append((hT, hk))

                def dn_group(i, q):
                    ex, fh = steps[i]
                    wd, wdk = wts[('d', i)]
                    tq, cc = divmod(q, 8)
                    po, pok = dn_banks[q % 2]
                    for ft in range(2):
                        hT, hk = hts_of[i][ft]
                        op('pe', lambda e, po=po, hT=hT, wd=wd, ft=ft, tq=tq, cc=cc: e.matmul(po[:, :], lhsT=hT[:, tq * 128:(tq + 1) * 128], rhs=wd[:, ft, cc * 512:(cc + 1) * 512], start=(ft == 0), stop=(ft == 1)),
                           r=[hk, wdk], w=[pok])
                    op('dve', lambda e, po=po, tq=tq, cc=cc, ex=ex: e.scalar_tensor_tensor(out=acc[:, tq, cc * 512:(cc + 1) * 512], in0=po[:, :], scalar=gts[:, tq, ex:ex + 1], in1=acc[:, tq, cc * 512:(cc + 1) * 512], op0=ALU.mult, op1=ALU.add),
                       r=[pok, ('e_gt', tq), ('e_acc', tq)], w=[('e_acc', tq)])

                load_gu(0)
                load_d(0)
                load_gu(1)
                for q in range(32):
                    gu_group(0, q)
                for i in range(nst):
                    if i + 2 < nst:
                        load_gu(i + 2)
                    if i + 1 < nst:
                        load_d(i + 1)
                    for q in range(32):
                        if i + 1 < nst:
                            gu_group(i + 1, q)
                        dn_group(i, q)
                    hts_of.pop(i, None)
                    wts.pop(('gu', i), None)
                    wts.pop(('d', i), None)
                for j in range(4):
                    tt = tc * 4 + j
                    tsl = slice(tt * 128, (tt + 1) * 128)
                    layer_norm_tile(acc[:, j, :], ('e_acc', j), ln2g, ln2b, g_r, b_r, st, mv, rsd, 'l2')
                    fin.append(op('act', lambda e, j=j, tsl=tsl: e.dma_start(out=out[tsl, :], in_=acc[:, j, :]), r=[('e_acc', j)], w=[('out', tt)], dma=True))
            S.wait_all('act', fin)
            S.flush()
            pes.__exit__(None, None, None)
        else:
            zt = sb("zt", [128, D_], F32)
            op('dve', lambda e: e.memset(zt[:], 0.0), w=['zt'])
            for tt in range(NT):
                fin.append(op('act', lambda e, tt=tt: e.dma_start(out=out[tt * 128:(tt + 1) * 128, :], in_=zt[:]), r=['zt'], w=[('out', tt)], dma=True))
            S.wait_all('act', fin)
            S.flush()
        S.close()
    return nc


def host_constants():
    c = {}
    c["ident"] = np.eye(128, dtype=np.float32)
    inva = (10000.0 ** (-np.arange(32, dtype=np.float32) * 2.0 / 64)).astype(np.float32)
    invb = (10000.0 ** (-np.arange(64, dtype=np.float32) * 2.0 / 128)).astype(np.float32)
    c["inva"] = np.ascontiguousarray(np.broadcast_to(inva, (128, 32)))
    c["invb"] = np.ascontiguousarray(np.broadcast_to(invb, (128, 64)))
    k = np.arange(128)[:, None]
    q = np.arange(512)[None, :]

    def band(r, W):
        d = q - (128 * r + k)
        return ((d >= 0) & (d < W)).astype(np.float32)
    c["m_swa"] = np.stack([band(r, 128) for r in range(-1, 4)])
    c["m_win"] = np.stack([band(r, 512) for r in range(-4, 4)])
    t = np.arange(S_)[:, None]
    blk = np.arange(256)[None, :]
    mc = ((16 * blk + 31 <= t) & (blk < 255)).astype(np.float32)
    c["m_cmp"] = mc
    c["m_cmpT"] = np.ascontiguousarray(mc.T.reshape(2, 128, S_))
    j = np.arange(64)[None, :]
    cur = t // 64
    valid = (j * 64 <= t)
    forced = (j == 0) | (j == cur) | (j == cur - 1)
    c["sl_vb"] = valid.astype(np.float32)
    c["sl_add"] = (1e9 * forced - (~valid)).astype(np.float32)
    ex = np.zeros((64, 32, 128), np.float32)
    for kt in range(32):
        for kk in range(128):
            ex[2 * kt + kk // 64, kt, kk] = 1.0
    c["exmat"] = ex.reshape(64, 32 * 128)
    return c


def lay_gu(w):
    E = w.shape[0]
    return np.ascontiguousarray(w.reshape(E, 32, 128, 2, 256).transpose(0, 3, 2, 1, 4)).reshape(E, 2, 128, 32 * 256)


def lay_d(w):
    E = w.shape[0]
    return np.ascontiguousarray(w.reshape(E, 2, 2, 128, 4096).transpose(0, 1, 3, 2, 4)).reshape(E, 2, 128, 2 * 4096)


def kernel(x, positions, w_in, a_sinks, cmp_k_pos, cmp_k_w1, cmp_k_w2, cmp_v_pos, cmp_v_w1, cmp_v_w2,
           w_o, ln1_g, ln1_b, w_router, router_bias, exp_w_gate, exp_w_up, exp_w_down,
           sh_w_gate, sh_w_up, sh_w_down, ln2_g, ln2_b):
    f = lambda a: np.ascontiguousarray(np.asarray(a))
    bc = lambda v, n: np.ascontiguousarray(np.broadcast_to(np.asarray(v).reshape(1, -1), (128, n)))
    nc = build_nc()
    shared = host_constants()
    shared.update({
        "w_in": f(w_in[0]), "sinks_b": bc(a_sinks[0], 64),
        "kpeT": f(np.asarray(cmp_k_pos[0]).T), "vpeT": f(np.asarray(cmp_v_pos[0]).T),
        "kw1": f(cmp_k_w1[0]), "vw1": f(cmp_v_w1[0]), "kw2": f(cmp_k_w2[0]), "vw2": f(cmp_v_w2[0]),
        "w_o": f(w_o[0]), "ln1g": bc(ln1_g[0], D_), "ln1b": bc(ln1_b[0], D_), "ln2g": bc(ln2_g[0], D_), "ln2b": bc(ln2_b[0], D_),
        "w_r": f(w_router[0]), "rbias": bc(router_bias[0], 64),
        "eg": lay_gu(np.concatenate([np.asarray(exp_w_gate[0]), np.asarray(sh_w_gate[0])[None]], 0)),
        "eu": lay_gu(np.concatenate([np.asarray(exp_w_up[0]), np.asarray(sh_w_up[0])[None]], 0)),
        "ed": lay_d(np.concatenate([np.asarray(exp_w_down[0]), np.asarray(sh_w_down[0])[None]], 0)),
    })
    in_maps = []
    for b in range(NCORES):
        m = dict(shared)
        m["x"] = f(np.asarray(x)[b])
        m["pos"] = f(np.asarray(positions)[b].astype(np.int32).reshape(NT, 128).T)
        in_maps.append({k: v for k, v in m.items() if k in DECLARED})
    res = run_bass_kernel_spmd(nc, in_maps, core_ids=list(range(NCORES)))
    return np.stack([np.asarray(res.results[b]["out"]) for b in range(NCORES)], 0).astype(np.float32)
```
